# Optimizing a Trainium2 kernel written in Bass

```python
import math
import jax, jax.numpy as jnp
from jax import lax
import numpy as np

D_MODEL = 1024
BATCH = 8
SEQ = 2048
DEPTH = 2

GRID_W = 64
CTX_LEN = 256
F32 = jnp.float32
RMS_EPS = 1e-6
HEAD_DIM = 64
ATTN_HEADS = 8
ATTN_KV_HEADS = 2
Q_BLOCK = 128
ROPE_THETA = 10000.0
SSD_HEADS = 8
SSD_HEAD_DIM = 64
SSD_GROUPS = 2
SSD_STATE = 64
SSD_CONV = 3
SSD_CHUNK = 128
S5_GROUPS = 24
S5_GROUP_DIM = 16
S5_STATE = 64
SC_WIDTH = 384
SC_CONV = 3
N_EXPERTS = 16
EXPERT_FF = 1024
EC_CAPACITY = 2
N_BRANCHES = 4

ATTN_Q_DIM = ATTN_HEADS * HEAD_DIM
ATTN_KV_DIM = ATTN_KV_HEADS * HEAD_DIM
SSD_INNER = SSD_HEADS * SSD_HEAD_DIM
SSD_BC_DIM = SSD_GROUPS * SSD_STATE
SSD_CONV_DIM = SSD_INNER + 2 * SSD_BC_DIM
S5_WIDTH = S5_GROUPS * S5_GROUP_DIM
IN_SPLITS = (ATTN_Q_DIM, ATTN_KV_DIM, ATTN_KV_DIM, SSD_INNER, SSD_CONV_DIM, SSD_HEADS,
             S5_WIDTH, SC_WIDTH, SC_WIDTH, SC_WIDTH, N_BRANCHES * D_MODEL)
N_IN = sum(IN_SPLITS)

kernel_name = "hybrid_parallel_dit_block"


def rmsnorm(x, g):
    xf = x.astype(F32)
    y = xf * lax.rsqrt(jnp.mean(xf * xf, axis=-1, keepdims=True) + RMS_EPS)
    return (y * g.astype(F32)).astype(x.dtype)


def modulate(h, shift, scale):
    return h * (1.0 + scale) + shift


def split_cols(z, sizes):
    return jnp.split(z, np.cumsum(sizes)[:-1].tolist(), axis=-1)


def dwconv_centred(x, w):
    k = w.shape[0]
    pad = k // 2
    T = x.shape[1]
    xp = jnp.pad(x, ((0, 0), (pad, pad), (0, 0)))
    return sum(w[i] * xp[:, i:i + T] for i in range(k))


def axial_angles(T):
    rows = T // GRID_W
    row = jnp.repeat(jnp.arange(rows, dtype=F32), GRID_W)
    col = jnp.tile(jnp.arange(GRID_W, dtype=F32), rows)
    half = HEAD_DIM // 2
    inv = ROPE_THETA ** (-jnp.arange(0, half, 2, dtype=F32) / half)
    return row[:, None] * inv, col[:, None] * inv


def rope_1d(x, ang):
    x1, x2 = jnp.split(x, 2, axis=-1)
    cos = jnp.cos(ang)[None, :, None, :].astype(x.dtype)
    sin = jnp.sin(ang)[None, :, None, :].astype(x.dtype)
    return jnp.concatenate([x1 * cos - x2 * sin, x2 * cos + x1 * sin], axis=-1)


def axial_rope(x, row_ang, col_ang):
    half = HEAD_DIM // 2
    return jnp.concatenate([rope_1d(x[..., :half], row_ang), rope_1d(x[..., half:], col_ang)], axis=-1)


def gqa_attend(q, k, v):
    b, T, H, d = q.shape
    G = k.shape[2]
    qg = q.reshape(b, T, G, H // G, d)
    s = jnp.einsum('btgrd,bsgd->bgrts', qg, k).astype(F32) * (d ** -0.5)
    p = jax.nn.softmax(s, axis=-1).astype(v.dtype)
    o = jnp.einsum('bgrts,bsgd->btgrd', p, v)
    return o.reshape(b, T, H * d)


def attention_mixer(q_l, k_l, v_l, q_c, k_c, v_c, q_norm_g, k_norm_g, ctx_out):
    b, T = q_l.shape[:2]

    def heads(t, h):
        return t.reshape(t.shape[0], t.shape[1], h, HEAD_DIM)

    row_ang, col_ang = axial_angles(T)
    ql = axial_rope(rmsnorm(heads(q_l, ATTN_HEADS), q_norm_g), row_ang, col_ang)
    kl = axial_rope(rmsnorm(heads(k_l, ATTN_KV_HEADS), k_norm_g), row_ang, col_ang)
    vl = heads(v_l, ATTN_KV_HEADS)
    kc = rmsnorm(heads(k_c, ATTN_KV_HEADS), k_norm_g)
    vc = heads(v_c, ATTN_KV_HEADS)
    k_all = jnp.concatenate([kc, kl], axis=1)
    v_all = jnp.concatenate([vc, vl], axis=1)
    nb = T // Q_BLOCK
    qb = jnp.moveaxis(ql.reshape(b, nb, Q_BLOCK, ATTN_HEADS, HEAD_DIM), 1, 0)
    ob = lax.map(lambda qq: gqa_attend(qq, k_all, v_all), qb)
    y_l = jnp.moveaxis(ob, 0, 1).reshape(b, T, ATTN_Q_DIM)
    if not ctx_out:
        return y_l, None
    y_c = gqa_attend(rmsnorm(heads(q_c, ATTN_HEADS), q_norm_g), kc, vc)
    return y_l, y_c


def segsum(a):
    T = a.shape[-1]
    x = jnp.broadcast_to(a[..., :, None], a.shape + (T,))
    x = jnp.where(jnp.tril(jnp.ones((T, T), bool), -1), x, 0.0)
    cs = jnp.cumsum(x, axis=-2)
    return jnp.where(jnp.tril(jnp.ones((T, T), bool)), cs, -jnp.inf)


def ssd_scan(x, dt, a, bm, cm, h0):
    b, T, H, P = x.shape
    N = bm.shape[-1]
    l = SSD_CHUNK
    nc = T // l
    xd = (x.astype(F32) * dt[..., None]).reshape(b, nc, l, H, P)
    bc = bm.astype(F32).reshape(b, nc, l, H, N)
    cc = cm.astype(F32).reshape(b, nc, l, H, N)
    adt = jnp.moveaxis((dt * a).reshape(b, nc, l, H), -1, 1)
    a_cs = jnp.cumsum(adt, axis=-1)
    scores = jnp.einsum('bclhn,bcshn->bhcls', cc, bc) * jnp.exp(segsum(adt))
    y_diag = jnp.einsum('bhcls,bcshp->bclhp', scores, xd)
    decay_to_end = jnp.exp(a_cs[..., -1:] - a_cs)
    chunk_states = jnp.einsum('bclhn,bhcl,bclhp->bchpn', bc, decay_to_end, xd)
    states = jnp.concatenate([h0[:, None].astype(F32), chunk_states], axis=1)
    decay_chunk = jnp.exp(segsum(jnp.pad(a_cs[..., -1], ((0, 0), (0, 0), (1, 0)))))
    states = jnp.einsum('bhzc,bchpn->bzhpn', decay_chunk, states)
    y_off = jnp.einsum('bclhn,bchpn,bhcl->bclhp', cc, states[:, :-1], jnp.exp(a_cs))
    return (y_diag + y_off).reshape(b, T, H, P), states[:, -1]


def ssd_split(xbc):
    xs, bs, cs = split_cols(xbc, (SSD_INNER, SSD_BC_DIM, SSD_BC_DIM))
    b, T = xs.shape[:2]
    rep = SSD_HEADS // SSD_GROUPS
    xs = xs.reshape(b, T, SSD_HEADS, SSD_HEAD_DIM)
    bs = jnp.repeat(bs.reshape(b, T, SSD_GROUPS, SSD_STATE), rep, axis=2)
    cs = jnp.repeat(cs.reshape(b, T, SSD_GROUPS, SSD_STATE), rep, axis=2)
    return xs, bs, cs


def ssd_direction(x, dt, bm, cm, a, h0, rev):
    if rev:
        x, dt, bm, cm = (jnp.flip(t, axis=1) for t in (x, dt, bm, cm))
    y, h = ssd_scan(x, dt, a, bm, cm, h0)
    return (jnp.flip(y, axis=1) if rev else y), h


def ssd_mixer(z_l, xbc_l, dt_l, z_c, xbc_c, dt_c, conv_w, conv_b, dt_bias, a_log, d_skip, norm_g, ctx_out):
    xl, bl, cl = ssd_split(jax.nn.silu(dwconv_centred(xbc_l, conv_w) + conv_b))
    xc, bc, cc = ssd_split(jax.nn.silu(dwconv_centred(xbc_c, conv_w) + conv_b))
    h0 = jnp.zeros((xl.shape[0], SSD_HEADS, SSD_HEAD_DIM, SSD_STATE), F32)
    y_l = d_skip.astype(F32)[:, None] * xl.astype(F32)
    y_c = d_skip.astype(F32)[:, None] * xc.astype(F32)
    for d in range(2):
        a = -jnp.exp(a_log[d].astype(F32))
        dtl = jax.nn.softplus((dt_l + dt_bias[d]).astype(F32))
        dtc = jax.nn.softplus((dt_c + dt_bias[d]).astype(F32))
        yc_d, hc = ssd_direction(xc, dtc, bc, cc, a, h0, d == 1)
        yl_d, _ = ssd_direction(xl, dtl, bl, cl, a, hc, d == 1)
        y_l = y_l + yl_d
        y_c = y_c + yc_d

    def finish(y, z):
        y = y.reshape(z.shape).astype(z.dtype) * jax.nn.silu(z)
        return rmsnorm(y, norm_g)

    return finish(y_l, z_l), (finish(y_c, z_c) if ctx_out else None)


def s5_discretise(lam_re, lam_im, log_step, b_re, b_im):
    step = jnp.exp(log_step.astype(F32))[:, None]
    lr, li = lam_re.astype(F32), lam_im.astype(F32)
    mag = jnp.exp(lr * step)
    ab_re, ab_im = mag * jnp.cos(li * step), mag * jnp.sin(li * step)
    den = lr * lr + li * li
    nr, ni = ab_re - 1.0, ab_im
    coef_re = (nr * lr + ni * li) / den
    coef_im = (ni * lr - nr * li) / den
    br, bi = b_re.astype(F32), b_im.astype(F32)
    bb_re = coef_re[..., None] * br - coef_im[..., None] * bi
    bb_im = coef_re[..., None] * bi + coef_im[..., None] * br
    return ab_re, ab_im, bb_re, bb_im


def complex_linear_scan(ab_re, ab_im, bu_re, bu_im, h0_re, h0_im):
    a_re = jnp.broadcast_to(ab_re, bu_re.shape)
    a_im = jnp.broadcast_to(ab_im, bu_re.shape)

    def combine(e1, e2):
        a1r, a1i, b1r, b1i = e1
        a2r, a2i, b2r, b2i = e2
        return (a2r * a1r - a2i * a1i, a2r * a1i + a2i * a1r,
                a2r * b1r - a2i * b1i + b2r, a2r * b1i + a2i * b1r + b2i)

    pr, pi, hr, hi = lax.associative_scan(combine, (a_re, a_im, bu_re, bu_im), axis=1)
    h0r, h0i = h0_re[:, None], h0_im[:, None]
    return hr + pr * h0r - pi * h0i, hi + pr * h0i + pi * h0r


def s5_drive(u, bb):
    return jnp.einsum('btgi,gpi->btgp', u.astype(F32), bb)


def s5_readout(u, s_re, s_im, c_re, c_im, d_skip, w_glu, b_glu):
    b, T = u.shape[:2]
    y = (jnp.einsum('btgp,gip->btgi', s_re, c_re.astype(F32))
         - jnp.einsum('btgp,gip->btgi', s_im, c_im.astype(F32))
         + d_skip.reshape(S5_GROUPS, S5_GROUP_DIM).astype(F32) * u.astype(F32))
    y = jax.nn.gelu(y.reshape(b, T, S5_WIDTH)).astype(u.dtype)
    return y * jax.nn.sigmoid(y @ w_glu + b_glu)


def s5_mixer(u_l, u_c, lam_re, lam_im, log_step, b_re, b_im, c_re, c_im, d_skip, w_glu, b_glu, ctx_out):
    def groups(u):
        return u.reshape(u.shape[0], u.shape[1], S5_GROUPS, S5_GROUP_DIM)

    ul, uc = groups(u_l), groups(u_c)
    zero = jnp.zeros((ul.shape[0], S5_GROUPS, S5_STATE), F32)
    sl_re = sl_im = sc_re = sc_im = 0.0
    for d in range(2):
        ab_re, ab_im, bb_re, bb_im = s5_discretise(lam_re[d], lam_im[d], log_step[d], b_re, b_im)
        uc_d = jnp.flip(uc, axis=1) if d else uc
        ul_d = jnp.flip(ul, axis=1) if d else ul
        hc_re, hc_im = complex_linear_scan(ab_re, ab_im, s5_drive(uc_d, bb_re), s5_drive(uc_d, bb_im), zero, zero)
        hl_re, hl_im = complex_linear_scan(ab_re, ab_im, s5_drive(ul_d, bb_re), s5_drive(ul_d, bb_im),
                                           hc_re[:, -1], hc_im[:, -1])
        if d:
            hc_re, hc_im, hl_re, hl_im = (jnp.flip(t, axis=1) for t in (hc_re, hc_im, hl_re, hl_im))
        sl_re, sl_im = sl_re + hl_re, sl_im + hl_im
        sc_re, sc_im = sc_re + hc_re, sc_im + hc_im
    y_l = s5_readout(ul, sl_re, sl_im, c_re, c_im, d_skip, w_glu, b_glu)
    y_c = s5_readout(uc, sc_re, sc_im, c_re, c_im, d_skip, w_glu, b_glu) if ctx_out else None
    return y_l, y_c


def shortconv_mixer(bg, cg, hh, conv_w):
    return bg * dwconv_centred(cg * hh, conv_w)


def hybrid_mixer(h_l, h_c, p, ctx_out):
    proj_l = h_l @ p['w_in']
    proj_c = h_c @ p['w_in']
    q_l, k_l, v_l, z_l, xbc_l, dt_l, u_l, sb_l, sg_l, sh_l, g_l = split_cols(proj_l, IN_SPLITS)
    q_c, k_c, v_c, z_c, xbc_c, dt_c, u_c, sb_c, sg_c, sh_c, g_c = split_cols(proj_c, IN_SPLITS)
    ya_l, ya_c = attention_mixer(q_l, k_l, v_l, q_c, k_c, v_c, p['q_norm_g'], p['k_norm_g'], ctx_out)
    yb_l, yb_c = ssd_mixer(z_l, xbc_l, dt_l, z_c, xbc_c, dt_c, p['ssd_conv_w'], p['ssd_conv_b'],
                           p['ssd_dt_bias'], p['ssd_a_log'], p['ssd_d'], p['ssd_norm_g'], ctx_out)
    yc_l, yc_c = s5_mixer(u_l, u_c, p['s5_lambda_re'], p['s5_lambda_im'], p['s5_log_step'],
                          p['s5_b_re'], p['s5_b_im'], p['s5_c_re'], p['s5_c_im'], p['s5_d'],
                          p['s5_w_glu'], p['s5_b_glu'], ctx_out)
    yd_l = shortconv_mixer(sb_l, sg_l, sh_l, p['sc_conv_w'])

    def merge(ya, yb, yc, yd, g):
        gates = jnp.split(jax.nn.sigmoid(g), N_BRANCHES, axis=-1)
        merged = (gates[0] * (ya @ p['w_br_attn']) + gates[1] * (yb @ p['w_br_ssd'])
                  + gates[2] * (yc @ p['w_br_s5']) + gates[3] * (yd @ p['w_br_sc']))
        return merged @ p['w_out']

    y_l = merge(ya_l, yb_l, yc_l, yd_l, g_l)
    if not ctx_out:
        return y_l, None
    yd_c = shortconv_mixer(sb_c, sg_c, sh_c, p['sc_conv_w'])
    return y_l, merge(ya_c, yb_c, yc_c, yd_c, g_c)


def expert_choice_ffn(h, w_router, w_gate, w_up, w_down):
    b, T, _ = h.shape
    cap = EC_CAPACITY * T // N_EXPERTS
    aff = jax.nn.softmax((h @ w_router).astype(F32), axis=-1)
    gval, idx = lax.top_k(jnp.swapaxes(aff, 1, 2), cap)
    bidx = jnp.arange(b)[:, None, None]
    xg = h[bidx, idx]
    hid = jax.nn.silu(jnp.einsum('becd,edf->becf', xg, w_gate)) * jnp.einsum('becd,edf->becf', xg, w_up)
    ye = jnp.einsum('becf,efd->becd', hid, w_down) * gval[..., None].astype(h.dtype)
    return jnp.zeros_like(h).at[bidx, idx].add(ye)


def setup_inputs(seed: int = 0) -> dict:
    key = jax.random.key(seed)
    ks = iter(jax.random.split(key, 48))

    def nrm(shape, scale=1.0):
        return scale * jax.random.normal(next(ks), shape, F32)

    def unif(shape, lo, hi):
        return jax.random.uniform(next(ks), shape, F32, lo, hi)

    L, D = DEPTH, D_MODEL
    dt0 = jnp.exp(unif((L, 2, SSD_HEADS), math.log(1e-3), math.log(1e-1)))
    return {
        'x': nrm((BATCH, SEQ, D)),
        'c': nrm((BATCH, D)),
        'ctx': nrm((BATCH, CTX_LEN, D)),
        'c_ctx': nrm((D,)),
        'w_mod': nrm((L, D, 6 * D), 0.5 * D ** -0.5),
        'b_mod': nrm((L, 6 * D), 0.01),
        'norm_mix_g': 1.0 + nrm((L, D), 0.01),
        'norm_ffn_g': 1.0 + nrm((L, D), 0.01),
        'w_in': nrm((L, D, N_IN), D ** -0.5),
        'q_norm_g': 1.0 + nrm((L, HEAD_DIM), 0.01),
        'k_norm_g': 1.0 + nrm((L, HEAD_DIM), 0.01),
        'ssd_conv_w': nrm((L, SSD_CONV, SSD_CONV_DIM), SSD_CONV ** -0.5),
        'ssd_conv_b': nrm((L, SSD_CONV_DIM), 0.01),
        'ssd_dt_bias': dt0 + jnp.log(-jnp.expm1(-dt0)),
        'ssd_a_log': jnp.log(unif((L, 2, SSD_HEADS), 1.0, 16.0)),
        'ssd_d': 1.0 + nrm((L, SSD_HEADS), 0.01),
        'ssd_norm_g': 1.0 + nrm((L, SSD_INNER), 0.01),
        's5_lambda_re': -0.5 + nrm((L, 2, S5_GROUPS, S5_STATE), 0.01),
        's5_lambda_im': jnp.pi * jnp.arange(S5_STATE, dtype=F32) + nrm((L, 2, S5_GROUPS, S5_STATE), 0.01),
        's5_log_step': unif((L, 2, S5_GROUPS), math.log(1e-3), math.log(1e-1)),
        's5_b_re': nrm((L, S5_GROUPS, S5_STATE, S5_GROUP_DIM), S5_GROUP_DIM ** -0.5),
        's5_b_im': nrm((L, S5_GROUPS, S5_STATE, S5_GROUP_DIM), S5_GROUP_DIM ** -0.5),
        's5_c_re': nrm((L, S5_GROUPS, S5_GROUP_DIM, S5_STATE), S5_STATE ** -0.5),
        's5_c_im': nrm((L, S5_GROUPS, S5_GROUP_DIM, S5_STATE), S5_STATE ** -0.5),
        's5_d': nrm((L, S5_WIDTH)),
        's5_w_glu': nrm((L, S5_WIDTH, S5_WIDTH), S5_WIDTH ** -0.5),
        's5_b_glu': nrm((L, S5_WIDTH), 0.01),
        'sc_conv_w': nrm((L, SC_CONV, SC_WIDTH), SC_CONV ** -0.5),
        'w_br_attn': nrm((L, ATTN_Q_DIM, D), ATTN_Q_DIM ** -0.5),
        'w_br_ssd': nrm((L, SSD_INNER, D), SSD_INNER ** -0.5),
        'w_br_s5': nrm((L, S5_WIDTH, D), S5_WIDTH ** -0.5),
        'w_br_sc': nrm((L, SC_WIDTH, D), SC_WIDTH ** -0.5),
        'w_out': nrm((L, D, D), D ** -0.5),
        'w_router': nrm((L, D, N_EXPERTS), D ** -0.5),
        'w_exp_gate': nrm((L, N_EXPERTS, D, EXPERT_FF), D ** -0.5),
        'w_exp_up': nrm((L, N_EXPERTS, D, EXPERT_FF), D ** -0.5),
        'w_exp_down': nrm((L, N_EXPERTS, EXPERT_FF, D), EXPERT_FF ** -0.5),
        'final_norm_g': 1.0 + nrm((D,), 0.01),
    }


def reference(x, c, ctx, c_ctx, w_mod, b_mod, norm_mix_g, norm_ffn_g, w_in, q_norm_g, k_norm_g,
              ssd_conv_w, ssd_conv_b, ssd_dt_bias, ssd_a_log, ssd_d, ssd_norm_g,
              s5_lambda_re, s5_lambda_im, s5_log_step, s5_b_re, s5_b_im, s5_c_re, s5_c_im,
              s5_d, s5_w_glu, s5_b_glu, sc_conv_w, w_br_attn, w_br_ssd, w_br_s5, w_br_sc, w_out,
              w_router, w_exp_gate, w_exp_up, w_exp_down, final_norm_g):
    silu_c = jax.nn.silu(c)
    silu_cc = jax.nn.silu(c_ctx)
    for i in range(DEPTH):
        ctx_out = i < DEPTH - 1
        p = {
            'w_in': w_in[i], 'q_norm_g': q_norm_g[i], 'k_norm_g': k_norm_g[i],
            'ssd_conv_w': ssd_conv_w[i], 'ssd_conv_b': ssd_conv_b[i], 'ssd_dt_bias': ssd_dt_bias[i],
            'ssd_a_log': ssd_a_log[i], 'ssd_d': ssd_d[i], 'ssd_norm_g': ssd_norm_g[i],
            's5_lambda_re': s5_lambda_re[i], 's5_lambda_im': s5_lambda_im[i], 's5_log_step': s5_log_step[i],
            's5_b_re': s5_b_re[i], 's5_b_im': s5_b_im[i], 's5_c_re': s5_c_re[i], 's5_c_im': s5_c_im[i],
            's5_d': s5_d[i], 's5_w_glu': s5_w_glu[i], 's5_b_glu': s5_b_glu[i], 'sc_conv_w': sc_conv_w[i],
            'w_br_attn': w_br_attn[i], 'w_br_ssd': w_br_ssd[i], 'w_br_s5': w_br_s5[i], 'w_br_sc': w_br_sc[i],
            'w_out': w_out[i],
        }
        mod_l = jnp.split((silu_c @ w_mod[i] + b_mod[i])[:, None, :], 6, axis=-1)
        mod_c = jnp.split(silu_cc @ w_mod[i] + b_mod[i], 6, axis=-1)
        h_l = modulate(rmsnorm(x, norm_mix_g[i]), mod_l[0], mod_l[1])
        h_c = modulate(rmsnorm(ctx, norm_mix_g[i]), mod_c[0], mod_c[1])
        y_l, y_c = hybrid_mixer(h_l, h_c, p, ctx_out)
        x = x + mod_l[2] * y_l
        x = x + mod_l[5] * expert_choice_ffn(modulate(rmsnorm(x, norm_ffn_g[i]), mod_l[3], mod_l[4]),
                                             w_router[i], w_exp_gate[i], w_exp_up[i], w_exp_down[i])
        if ctx_out:
            ctx = ctx + mod_c[2] * y_c
            ctx = ctx + mod_c[5] * expert_choice_ffn(modulate(rmsnorm(ctx, norm_ffn_g[i]), mod_c[3], mod_c[4]),
                                                     w_router[i], w_exp_gate[i], w_exp_up[i], w_exp_down[i])
    return rmsnorm(x, final_norm_g)
```

```python
import os
import numpy as np
from contextlib import ExitStack
import ml_dtypes
import concourse.bass as bass
import concourse.mybir as mybir
from concourse.bass_utils import run_bass_kernel_spmd

F32 = mybir.dt.float32
BF16 = mybir.dt.bfloat16
AF = mybir.ActivationFunctionType
ALU = mybir.AluOpType
AX = mybir.AxisListType

D = 1024
TL = 2048
TC = 256
T = TL + TC
NT = T // 128
N_IN = 7688
DEPTH = 2
NE = 16
SAME_ENG_SYNC = os.environ.get("KSES", "1") == "1"
TOKCH = [(0, 256), (256, 512), (768, 512), (1280, 512), (1792, 512)]

PARAM_SHAPES = {
    'w_mod': (D, 6 * D), 'b_mod': (6 * D,), 'norm_mix_g': (D,), 'norm_ffn_g': (D,), 'w_in': (D, N_IN),
    'q_norm_g': (64,), 'k_norm_g': (64,), 'ssd_conv_w': (3, 768), 'ssd_conv_b': (768,), 'ssd_dt_bias': (2, 8),
    'ssd_a_log': (2, 8), 'ssd_d': (8,), 'ssd_norm_g': (512,), 's5_lambda_re': (2, 24, 64), 's5_lambda_im': (2, 24, 64),
    's5_log_step': (2, 24), 's5_b_re': (24, 64, 16), 's5_b_im': (24, 64, 16), 's5_c_re': (24, 16, 64), 's5_c_im': (24, 16, 64),
    's5_d': (384,), 's5_w_glu': (384, 384), 's5_b_glu': (384,), 'sc_conv_w': (3, 384), 'w_br_attn': (512, D), 'w_br_ssd': (512, D),
    'w_br_s5': (384, D), 'w_br_sc': (384, D), 'w_out': (D, D), 'w_router': (D, NE), 'w_exp_gate': (NE, D, D), 'w_exp_up': (NE, D, D),
    'w_exp_down': (NE, D, D),
}
SCRATCH = {
    'x_d': ((T, D), F32), 'modv_d': ((128, 128), F32), 'modrow_d': ((2, 48, 128), F32),
    'projT_d': ((T, 1288), F32), 'projF_d': ((18, 128, T), F32), 'hT_d': ((8, 128, T), BF16), 'yT_d': ((14, 128, T), BF16),
}


class Sched:
    def __init__(self, nc, es):
        self.nc = nc
        self.eng = {'pe': nc.tensor, 'act': nc.scalar, 'dve': nc.vector, 'pool': nc.gpsimd, 'sp': nc.sync}
        self.sem = {}
        self.cnt = {}
        for e in self.eng:
            self.sem[e] = es.enter_context(nc.semaphore("s_" + e))
            self.cnt[e] = 0
        self.es = es
        self.waited = {e: {} for e in self.eng}
        self.last_w = {}
        self.readers = {}
        self.n = 0
        self.limit = int(os.environ.get("KLIMIT", "1000000000"))

    def dsem(self, name):
        k = ('dma', name)
        if k not in self.sem:
            self.sem[k] = self.es.enter_context(self.nc.semaphore("d_" + name))
            self.cnt[k] = 0
        return k

    def _deps(self, reads, writes):
        deps = {}

        def add(d):
            if d is None:
                return
            k, v = d
            if deps.get(k, 0) < v:
                deps[k] = v
        for r in reads:
            add(self.last_w.get(r))
            if isinstance(r, tuple) and r[0] in ('psf', 'psb', 'psfO'):
                for d in self.readers.get(r, ()):
                    add(d)
        for w in writes:
            add(self.last_w.get(w))
            for d in self.readers.get(w, ()):
                add(d)
        return deps

    def _wait(self, e, deps):
        for k, v in deps.items():
            if k == e and (e == 'pe' or not SAME_ENG_SYNC):
                continue
            if self.waited[e].get(k, 0) >= v:
                continue
            self.eng[e].wait_ge(self.sem[k], v)
            self.waited[e][k] = v

    def _record(self, tag, reads, writes):
        for w in writes:
            self.last_w[w] = tag
            self.readers[w] = []
        for r in reads:
            if r in writes:
                continue
            self.readers.setdefault(r, []).append(tag)

    def op(self, e, fn, reads=(), writes=()):
        self.n += 1
        if self.n > self.limit:
            return None
        reads = list(reads); writes = list(writes)
        self._wait(e, self._deps(reads, writes))
        ins = fn(self.eng[e])
        self.cnt[e] += 1
        ins.then_inc(self.sem[e], 1)
        self._record((e, self.cnt[e]), reads, writes)
        return ins

    def dma(self, q, out, in_, reads=(), writes=(), sem='g', force=False, **kw):
        self.n += 1
        if self.n > self.limit and not force:
            return None
        reads = list(reads); writes = list(writes)
        k = self.dsem(sem)
        self._wait(q, self._deps(reads, writes))
        ins = self.eng[q].dma_start(out=out, in_=in_, **kw)
        self.cnt[k] += 16
        ins.then_inc(self.sem[k], 16)
        self._record((k, self.cnt[k]), reads, writes)
        return ins

    def barrier(self):
        snap = dict(self.cnt)
        for e in self.eng:
            for k, v in snap.items():
                if v > 0 and k != e and self.waited[e].get(k, 0) < v:
                    self.eng[e].wait_ge(self.sem[k], v)
                    self.waited[e][k] = v


class Ctx:
    pass


def dram(K, name, shape=None, dt=F32):
    if name not in K.di:
        if shape is None:
            if name in SCRATCH:
                shape, dt = SCRATCH[name]
            else:
                base, lay = name.rsplit('_', 1)
                shape = PARAM_SHAPES[base]
        kind = K.kinds.get(name, "Internal" if name in SCRATCH else "ExternalInput")
        K.di[name] = K.nc.dram_tensor(name, list(shape), dt, kind=kind).ap()
        K.declared[name] = kind
    return K.di[name]


def dump(K, name, ap, reads, dt=F32):
    shape = list(ap.shape)
    t = K.nc.dram_tensor("dbg_" + name, shape, dt, kind="ExternalOutput").ap()
    K.S.dma('sp', t, ap, reads=reads, writes=['dbgout'], sem='dbg', force=True)


def phase_init(K):
    S, nc = K.S, K.nc
    x_d = dram(K, 'x_d'); xin = dram(K, 'x', (TL, D)); cin = dram(K, 'ctx', (TC, D))
    with ExitStack() as pes:
        bufs = [pes.enter_context(nc.sbuf_tensor(f"init_b{j}", [128, D], F32)) for j in range(2)]
        for t in range(NT):
            j = t % 2
            src = cin[t * 128:(t + 1) * 128, :] if t < 2 else xin[(t - 2) * 128:(t - 1) * 128, :]
            S.dma('sp', bufs[j][:], src, writes=[('ib', j)], sem=f'ib{j}')
            S.dma('sp', x_d[t * 128:(t + 1) * 128, :], bufs[j][:], reads=[('ib', j)], writes=[('x_d', t)], sem=f'ib{j}')
        S.barrier()


def phase_p0(K, layer):
    S, nc, psf = K.S, K.nc, K.psf
    identf = K.identf
    modv_d = dram(K, 'modv_d'); modrow_d = dram(K, 'modrow_d')
    with ExitStack() as pes:
        sb = lambda name, shape, dt=F32: pes.enter_context(nc.sbuf_tensor(f"L{layer}_p0_{name}", list(shape), dt))
        modv = sb("modv", (128, 128))
        modFM = modv[:, 0:96].rearrange("p (w j) -> p w j", w=2)
        G1 = modv[:, 96:112].rearrange("p (w f) -> p w f", w=2); G2 = modv[:, 112:128].rearrange("p (w f) -> p w f", w=2)
        vecs = sb("vecs", (128, 64))
        cct = sb("cct", (2, 1024)); scs = sb("scs", (2, 1024)); scT = sb("scT", (128, 8, 2))
        vstage = sb("vstage", (64, 128))
        wm = [sb(f"wm{j}", (128, 8, 512)) for j in range(2)]
        mrow = sb("mrow", (48, 2, 128))
        S.dma('sp', cct[:], dram(K, 'cc', (2, D))[:, :], writes=['cct'])
        S.dma('sp', vstage[0:48, :], dram(K, f'b_mod_{layer}').rearrange("(j p) -> j p", p=128), writes=['vstage'])
        S.dma('sp', vstage[48:56, :], dram(K, f'norm_mix_g_{layer}').rearrange("(j p) -> j p", p=128), writes=['vstage'])
        S.dma('sp', vstage[56:64, :], dram(K, f'norm_ffn_g_{layer}').rearrange("(j p) -> j p", p=128), writes=['vstage'])
        S.op('act', lambda e: e.activation(out=scs[:], in_=cct[:], func=AF.Silu), reads=['cct'], writes=['scs'])

        def tr_sc(e):
            for k in range(8):
                ins = e.transpose(out=psf[:, 0, k * 2:(k + 1) * 2], in_=scs[:, k * 128:(k + 1) * 128], identity=identf[0:2, 0:2])
            return ins
        S.op('pe', tr_sc, reads=['scs', 'identf'], writes=[('psf', 0)])
        S.op('dve', lambda e: e.tensor_copy(out=scT[:].rearrange("p k w -> p (k w)"), in_=psf[:, 0, 0:16]), reads=[('psf', 0)], writes=['scT'])
        S.op('pe', lambda e: e.transpose(out=psf[:, 1, 0:64], in_=vstage[:, :], identity=identf[0:64, 0:64]), reads=['vstage', 'identf'], writes=[('psf', 1)])
        S.op('dve', lambda e: e.tensor_copy(out=vecs[:], in_=psf[:, 1, 0:64]), reads=[('psf', 1)], writes=['vecs'])
        wmv = dram(K, f'w_mod_{layer}').rearrange("(k p) n -> p k n", p=128)
        for ch in range(12):
            wt = wm[ch % 2]
            S.dma('sp', wt[:], wmv[:, :, ch * 512:(ch + 1) * 512], writes=[('wm', ch % 2)], sem=f'wm{ch % 2}')

            def mm(e, ch=ch, wt=wt):
                for jt in range(4):
                    j = ch * 4 + jt
                    for k in range(8):
                        ins = e.matmul(psf[:, 2, j * 2:(j + 1) * 2], lhsT=wt[:, k, jt * 128:(jt + 1) * 128], rhs=scT[:, k, :], start=(k == 0), stop=(k == 7))
                return ins
            S.op('pe', mm, reads=[('wm', ch % 2), 'scT'], writes=[('psf', 2)])
        for w in range(2):
            S.op('dve', lambda e, w=w: e.tensor_tensor(out=modFM[:, w, :], in0=psf[:, 2, 0:96].rearrange("p (j w) -> p j w", w=2)[:, :, w], in1=vecs[:, 0:48], op=ALU.add),
                 reads=[('psf', 2), 'vecs'], writes=['modv'])
        for w in range(2):
            S.op('dve', lambda e, w=w: e.scalar_tensor_tensor(out=G1[:, w, :], in0=modFM[:, w, 8:16], scalar=1.0, in1=vecs[:, 48:56], op0=ALU.add, op1=ALU.mult),
                 reads=['modv', 'vecs'], writes=['modv'])
            S.op('dve', lambda e, w=w: e.scalar_tensor_tensor(out=G2[:, w, :], in0=modFM[:, w, 32:40], scalar=1.0, in1=vecs[:, 56:64], op0=ALU.add, op1=ALU.mult),
                 reads=['modv', 'vecs'], writes=['modv'])
        S.dma('sp', modv_d[:, :], modv[:], reads=['modv'], writes=['modv_d'], sem='p0o')
        for w in range(2):
            S.op('pe', lambda e, w=w: e.transpose(out=psf[0:48, 3, w * 128:(w + 1) * 128], in_=modv[:, w * 48:(w + 1) * 48], identity=identf[:]), reads=['modv', 'identf'], writes=[('psf', 3)])
        S.op('dve', lambda e: e.tensor_copy(out=mrow[:].rearrange("j w p -> j (w p)"), in_=psf[0:48, 3, 0:256]), reads=[('psf', 3)], writes=['mrow'])
        for w in range(2):
            S.dma('sp', modrow_d[w], mrow[:, w, :], reads=['mrow'], writes=['modrow_d'], sem='p0o')
        S.barrier()


def load_modv(K, sb):
    modv = sb("modv", (128, 128))
    K.S.dma('sp', modv[:], dram(K, 'modv_d')[:, :], reads=['modv_d'], writes=['modv'])
    return modv


def phase_p1(K, layer):
    S, nc, psf, psb = K.S, K.nc, K.psf, K.psb
    identb, eps_t = K.identb, K.eps_t
    x_d = dram(K, 'x_d'); projT_d = dram(K, 'projT_d'); projF_d = dram(K, 'projF_d'); hT_d = dram(K, 'hT_d')
    with ExitStack() as pes:
        sb = lambda name, shape, dt=F32: pes.enter_context(nc.sbuf_tensor(f"L{layer}_p1_{name}", list(shape), dt))
        modv = load_modv(K, sb)
        modFM = modv[:, 0:96].rearrange("p (w j) -> p w j", w=2)
        G1 = modv[:, 96:112].rearrange("p (w f) -> p w f", w=2)
        hFM = sb("hFM", (128, 8, T), BF16)
        xt = [sb(f"xt{j}", (128, 1024)) for j in range(2)]
        sq = sb("sq", (128, 1024))
        xn = [sb(f"xn{j}", (128, 1024), BF16) for j in range(2)]
        ss = sb("ss", (128, 2)); rstd = sb("rstd", (128, 2))
        for t in range(NT):
            w = 1 if t < 2 else 0
            j = t % 2
            S.dma('sp', xt[j][:], x_d[t * 128:(t + 1) * 128, :], reads=[('x_d', t)], writes=[('xt', j)], sem=f'xt{j}')
            S.op('act', lambda e, j=j: e.activation(out=sq[:], in_=xt[j][:], func=AF.Square, accum_out=ss[:, j:j + 1]), reads=[('xt', j)], writes=['sq', ('ss', j)])
            S.op('act', lambda e, j=j: e.activation(out=rstd[:, j:j + 1], in_=ss[:, j:j + 1], func=AF.Sqrt, scale=1.0 / D, bias=eps_t[:]), reads=[('ss', j), 'eps'], writes=[('rstd', j)])
            S.op('dve', lambda e, j=j: e.reciprocal(out=rstd[:, j:j + 1], in_=rstd[:, j:j + 1]), reads=[('rstd', j)], writes=[('rstd', j)])
            S.op('dve', lambda e, j=j: e.tensor_scalar(out=xn[j][:], in0=xt[j][:], scalar1=rstd[:, j:j + 1], scalar2=None, op0=ALU.mult), reads=[('xt', j), ('rstd', j)], writes=[('xn', j)])

            def trx(e, j=j):
                for f in range(8):
                    ins = e.transpose(out=psb[:, j, f * 128:(f + 1) * 128], in_=xn[j][:, f * 128:(f + 1) * 128], identity=identb[:])
                return ins
            S.op('pe', trx, reads=[('xn', j), 'identb'], writes=[('psb', j)])
            for f in range(8):
                if f % 2 == 0:
                    S.op('act', lambda e, f=f, j=j, t=t, w=w: e.activation(out=hFM[:, f, t * 128:(t + 1) * 128], in_=psb[:, j, f * 128:(f + 1) * 128], func=AF.Identity,
                                                                          scale=G1[:, w, f:f + 1], bias=modFM[:, w, f:f + 1]),
                         reads=[('psb', j), 'modv'], writes=[('hFM', t)])
                else:
                    S.op('dve', lambda e, f=f, j=j, t=t, w=w: e.tensor_scalar(out=hFM[:, f, t * 128:(t + 1) * 128], in0=psb[:, j, f * 128:(f + 1) * 128],
                                                                             scalar1=G1[:, w, f:f + 1], scalar2=modFM[:, w, f:f + 1], op0=ALU.mult, op1=ALU.add),
                         reads=[('psb', j), 'modv'], writes=[('hFM', t)])
        for f in range(8):
            S.dma('sp', hT_d[f], hFM[:, f, :], reads=[('hFM', t) for t in range(NT)], writes=[('hT_d', f)], sem='spill')
        wv = dram(K, f'w_in_{layer}').rearrange("(k p) n -> p k n", p=128)
        wch = [sb(f"wch{j}", (128, 8, 512), BF16) for j in range(2)]
        stg = [sb(f"stg{j}", (128, 512)) for j in range(4)]
        wi = 0; si = 0; pbank = 0
        for (c0, wd, d0) in [(0, 512, 0), (512, 512, 512), (1024, 256, 1024), (2048, 8, 1280)]:
            wj = wi % 2; wi += 1
            S.dma('pool', wch[wj][:, :, 0:wd], wv[:, :, c0:c0 + wd], writes=[('wch', wj)], sem=f'wch{wj}')
            for t in range(NT):
                pb = pbank % 4; pbank += 1
                sj = si % 4; si += 1

                def mm(e, t=t, wj=wj, wd=wd, pb=pb):
                    for k in range(8):
                        ins = e.matmul(psf[:, pb, 0:wd], lhsT=hFM[:, k, t * 128:(t + 1) * 128], rhs=wch[wj][:, k, 0:wd], start=(k == 0), stop=(k == 7))
                    return ins
                S.op('pe', mm, reads=[('hFM', t), ('wch', wj)], writes=[('psf', pb)])
                if sj % 2 == 0:
                    S.op('act', lambda e, sj=sj, pb=pb, wd=wd: e.activation(out=stg[sj][:, 0:wd], in_=psf[:, pb, 0:wd], func=AF.Copy), reads=[('psf', pb)], writes=[('stg', sj)])
                else:
                    S.op('dve', lambda e, sj=sj, pb=pb, wd=wd: e.tensor_copy(out=stg[sj][:, 0:wd], in_=psf[:, pb, 0:wd]), reads=[('psf', pb)], writes=[('stg', sj)])
                S.dma('sp', projT_d[t * 128:(t + 1) * 128, d0:d0 + wd], stg[sj][:, 0:wd], reads=[('stg', sj)], writes=[('projT_d', t)], sem=f'st{sj}')
        srow = [sb(f"srow{j}", (128, T)) for j in range(2)]
        fm_tiles = [1280 + 128 * i for i in range(6)] + [2056 + 128 * i for i in range(3)] + [2440 + 128 * i for i in range(9)]
        tokch = [(0, 512), (512, 512), (1024, 512), (1536, 512), (2048, 256)]
        for grp in range(0, 18, 4):
            tiles = fm_tiles[grp:grp + 4]
            wj = wi % 2; wi += 1
            for ti, c0 in enumerate(tiles):
                S.dma('pool', wch[wj][:, :, ti * 128:(ti + 1) * 128], wv[:, :, c0:c0 + 128], writes=[('wch', wj)], sem=f'wch{wj}')
            for ti, c0 in enumerate(tiles):
                ft = grp + ti
                rj = ft % 2
                for (n0, nw) in tokch:
                    pb = pbank % 4; pbank += 1

                    def mm(e, ti=ti, wj=wj, n0=n0, nw=nw, pb=pb):
                        for k in range(8):
                            ins = e.matmul(psf[:, pb, 0:nw], lhsT=wch[wj][:, k, ti * 128:(ti + 1) * 128], rhs=hFM[:, k, n0:n0 + nw], start=(k == 0), stop=(k == 7))
                        return ins
                    S.op('pe', mm, reads=[('hFM', t) for t in range(n0 // 128, (n0 + nw) // 128)] + [('wch', wj)], writes=[('psf', pb)])
                    if pb % 2 == 0:
                        S.op('act', lambda e, rj=rj, pb=pb, n0=n0, nw=nw: e.activation(out=srow[rj][:, n0:n0 + nw], in_=psf[:, pb, 0:nw], func=AF.Copy), reads=[('psf', pb)], writes=[('srow', rj)])
                    else:
                        S.op('dve', lambda e, rj=rj, pb=pb, n0=n0, nw=nw: e.tensor_copy(out=srow[rj][:, n0:n0 + nw], in_=psf[:, pb, 0:nw]), reads=[('psf', pb)], writes=[('srow', rj)])
                S.dma('sp', projF_d[ft], srow[rj][:], reads=[('srow', rj)], writes=[('projF_d', ft)], sem=f'sr{rj}')
        S.barrier()


def phase_attn(K, layer):
    S, nc, psf, psb = K.S, K.nc, K.psf, K.psb
    identb, eps_t = K.identb, K.eps_t
    projT_d = dram(K, 'projT_d'); yT_d = dram(K, 'yT_d')
    with ExitStack() as pes:
        sb = lambda name, shape, dt=F32: pes.enter_context(nc.sbuf_tensor(f"L{layer}_p2_{name}", list(shape), dt))
        qT = sb("qT", (128, 4, T), BF16)
        kTp = [sb(f"kTp{g}", (128, T), BF16) for g in range(2)]
        Vp = sb("Vp", (128, NT, 2, 66), BF16)
        ropeC = sb("ropeC", (128, NT, 64)); ropeS = sb("ropeS", (128, NT, 64))
        gfull = sb("gfull", (128, 10, 64))
        negB = sb("negB", (128, 1))
        qkv = [sb(f"qkv{j}", (128, 768)) for j in range(2)]
        sq2 = sb("sq2", (128, 640)); ssq = sb("ssq", (128, 10)); rs = sb("rs", (128, 10))
        qn = sb("qn", (128, 640)); t1 = sb("t1", (128, 640)); t2 = sb("t2", (128, 640))
        qr = sb("qr", (128, 512), BF16)
        kz = [sb(f"kz{g}", (128, 128), BF16) for g in range(2)]
        PT = [sb(f"PT{j}", (128, 512), BF16) for j in range(2)]
        ya = sb("ya", (128, 4, 512), BF16)
        rc = sb("rc", (128, 4))
        yaTs = sb("yaTs", (128, 4, 512), BF16)
        S.dma('sp', ropeC[:], dram(K, 'ropeC', (T, 64)).rearrange("(t p) d -> p t d", p=128), writes=['ropeC'])
        S.dma('sp', ropeS[:], dram(K, 'ropeS', (T, 64)).rearrange("(t p) d -> p t d", p=128), writes=['ropeS'])
        qg = dram(K, f'q_norm_g_{layer}').rearrange("(o d) -> o d", o=1); kg = dram(K, f'k_norm_g_{layer}').rearrange("(o d) -> o d", o=1)
        for h in range(10):
            S.dma('sp', gfull[:, h, :], (qg if h < 8 else kg).partition_broadcast(128), writes=['gfull'])
        S.op('dve', lambda e: e.memset(negB[:], -12.0), writes=['negB'])
        S.op('dve', lambda e: e.memset(kz[0][:], 0.0), writes=['kz'])
        S.op('dve', lambda e: e.memset(kz[1][:], 0.0), writes=['kz'])
        S.op('dve', lambda e: e.memset(Vp[:], 1.0), writes=['Vp'])
        v3 = lambda ap: ap.rearrange("p (h d) -> p h d", d=64)
        for t in range(NT):
            j = t % 2
            S.dma('sp', qkv[j][:], projT_d[t * 128:(t + 1) * 128, 0:768], reads=[('projT_d', t)], writes=[('qkv', j)], sem=f'qkv{j}')
            S.op('dve', lambda e, j=j: e.tensor_tensor(out=sq2[:], in0=qkv[j][:, 0:640], in1=qkv[j][:, 0:640], op=ALU.mult), reads=[('qkv', j)], writes=['sq2'])
            S.op('dve', lambda e: e.tensor_reduce(out=ssq[:], in_=v3(sq2[:]), axis=AX.X, op=ALU.add), reads=['sq2'], writes=['ssq'])
            S.op('act', lambda e: e.activation(out=rs[:], in_=ssq[:], func=AF.Sqrt, scale=1.0 / 64, bias=eps_t[:]), reads=['ssq', 'eps'], writes=['rs'])
            S.op('dve', lambda e: e.reciprocal(out=rs[:], in_=rs[:]), reads=['rs'], writes=['rs'])
            S.op('dve', lambda e, j=j: e.tensor_tensor(out=v3(qn[:]), in0=v3(qkv[j][:, 0:640]), in1=rs[:].unsqueeze(2).broadcast_to([128, 10, 64]), op=ALU.mult), reads=[('qkv', j), 'rs'], writes=['qn'])
            S.op('dve', lambda e: e.tensor_tensor(out=qn[:], in0=qn[:], in1=gfull[:].rearrange("p h d -> p (h d)"), op=ALU.mult), reads=['qn', 'gfull'], writes=['qn'])
            S.op('dve', lambda e, t=t: e.tensor_tensor(out=v3(t1[:]), in0=v3(qn[:]), in1=ropeC[:, t, :].unsqueeze(1).broadcast_to([128, 10, 64]), op=ALU.mult), reads=['qn', 'ropeC'], writes=['t1'])
            v5 = lambda ap: ap.rearrange("p (h b f j) -> p h b f j", h=10, b=2, f=2)
            for hf in range(2):
                S.op('dve', lambda e, t=t, hf=hf: e.tensor_tensor(
                    out=v5(t2[:])[:, :, :, hf, :], in0=v5(qn[:])[:, :, :, 1 - hf, :],
                    in1=ropeS[:, t, :].rearrange("p (b f j) -> p b f j", b=2, f=2)[:, :, hf, :].unsqueeze(1).broadcast_to([128, 10, 2, 16]), op=ALU.mult),
                    reads=['qn', 'ropeS'], writes=['t2'])
            pr = lambda ap: ap.rearrange("p (g j d) -> p j g d", g=2, j=4)
            S.op('dve', lambda e: e.tensor_tensor(out=qr[:, 0:512].rearrange("p (j g d) -> p j g d", j=4, g=2), in0=pr(t1[:, 0:512]), in1=pr(t2[:, 0:512]), op=ALU.add), reads=['t1', 't2'], writes=['qr'])
            for g in range(2):
                S.op('dve', lambda e, g=g: e.tensor_tensor(out=kz[g][:, g * 64:(g + 1) * 64], in0=t1[:, 512 + g * 64:576 + g * 64], in1=t2[:, 512 + g * 64:576 + g * 64], op=ALU.add), reads=['t1', 't2', 'kz'], writes=['kz'])
            if K.dbg and t == 2:
                dump(K, "qkv", qkv[j][:], [('qkv', j)]); dump(K, "qn", qn[:], ['qn']); dump(K, "qr", qr[:], ['qr'], BF16)

            def trq(e):
                for jj in range(4):
                    e.transpose(out=psb[:, 0, jj * 128:(jj + 1) * 128], in_=qr[:, jj * 128:(jj + 1) * 128], identity=identb[:])
                e.transpose(out=psb[:, 0, 512:640], in_=kz[0][:], identity=identb[:])
                return e.transpose(out=psb[:, 0, 640:768], in_=kz[1][:], identity=identb[:])
            S.op('pe', trq, reads=['qr', 'kz', 'identb'], writes=[('psb', 0)])
            S.op('act', lambda e, t=t: e.activation(out=qT[:, :, t * 128:(t + 1) * 128], in_=psb[:, 0, 0:512].rearrange("p (j q) -> p j q", j=4), func=AF.Copy), reads=[('psb', 0)], writes=[('qT', t)])
            S.op('dve', lambda e, t=t: e.tensor_scalar(out=kTp[0][:, t * 128:(t + 1) * 128], in0=psb[:, 0, 512:640], scalar1=1.0, scalar2=None, op0=ALU.mult), reads=[('psb', 0)], writes=[('kT', t)])
            S.op('dve', lambda e, t=t: e.tensor_scalar(out=kTp[1][:, t * 128:(t + 1) * 128], in0=psb[:, 0, 640:768], scalar1=1.0, scalar2=None, op0=ALU.mult), reads=[('psb', 0)], writes=[('kT', t)])
            S.op('dve', lambda e, t=t, j=j: e.tensor_copy(out=Vp[:, t, :, 0:64], in_=qkv[j][:, 640:768].rearrange("p (g d) -> p g d", g=2)), reads=[('qkv', j), 'Vp'], writes=[('Vp', t)])
        chunks = [(0, 2)] + [(2 + 4 * i, 4) for i in range(4)]
        if os.environ.get("KCUT") == "1":
            dump(K, "qT", qT[:, 0, :], [('qT', t) for t in range(NT)], BF16); dump(K, "kT0", kTp[0][:], [('kT', t) for t in range(NT)], BF16)
            chunks = []
        it = 0
        for (qt0, nq) in chunks:
            kts = list(range(2)) if qt0 == 0 else list(range(NT))
            nw = nq * 128
            for h in range(8):
                g, jj = h // 4, h % 4
                for ki, kt in enumerate(kts):
                    sbk = it % 2; it += 1
                    S.op('pe', lambda e, sbk=sbk, g=g, jj=jj, kt=kt, qt0=qt0, nw=nw: e.matmul(psf[:, sbk, 0:nw], lhsT=kTp[g][:, kt * 128:(kt + 1) * 128], rhs=qT[:, jj, qt0 * 128:qt0 * 128 + nw], start=True, stop=True),
                         reads=[('kT', kt)] + [('qT', qt0 + i) for i in range(nq)], writes=[('psf', sbk)])
                    S.op('act', lambda e, sbk=sbk, nw=nw: e.activation(out=PT[sbk][:, 0:nw], in_=psf[:, sbk, 0:nw], func=AF.Exp, scale=0.125, bias=negB[:]), reads=[('psf', sbk), 'negB'], writes=[('PT', sbk)])

                    def pv(e, sbk=sbk, nq=nq, kt=kt, g=g, ki=ki, last=(ki == len(kts) - 1)):
                        for i in range(nq):
                            ins = e.matmul(psf[:, 2 + i, 0:66], lhsT=PT[sbk][:, i * 128:(i + 1) * 128], rhs=Vp[:, kt, g, :], start=(ki == 0), stop=last)
                        return ins
                    S.op('pe', pv, reads=[('PT', sbk), ('Vp', kt), 'Vp'], writes=[('psfO', 0)])
                    if K.dbg and qt0 == 2 and h == 0 and ki == 0:
                        dump(K, "PT", PT[sbk][:], [('PT', sbk)], BF16)
                S.op('dve', lambda e, nq=nq: e.reciprocal(out=rc[:, 0:nq], in_=psf[:, 2:2 + nq, 64]), reads=[('psfO', 0)], writes=['rc'])
                S.op('dve', lambda e, nq=nq, h=h: e.tensor_tensor(out=ya[:, 0:nq, h * 64:(h + 1) * 64], in0=psf[:, 2:2 + nq, 0:64],
                                                                in1=rc[:, 0:nq].unsqueeze(2).broadcast_to([128, nq, 64]), op=ALU.mult), reads=[('psfO', 0), 'rc'], writes=['ya'])
            if K.dbg and qt0 == 2:
                dump(K, "ya", ya[:], ['ya'], BF16); dump(K, "rc", rc[:], ['rc'])
            for i in range(nq):
                def trya(e, i=i):
                    for c in range(4):
                        ins = e.transpose(out=psb[:, 1, c * 128:(c + 1) * 128], in_=ya[:, i, c * 128:(c + 1) * 128], identity=identb[:])
                    return ins
                S.op('pe', trya, reads=['ya', 'identb'], writes=[('psb', 1)])
                S.op('act', lambda e, i=i: e.activation(out=yaTs[:, :, i * 128:(i + 1) * 128], in_=psb[:, 1, 0:512].rearrange("p (c q) -> p c q", c=4), func=AF.Copy), reads=[('psb', 1)], writes=['yaTs'])
            for c in range(4):
                S.dma('sp', yT_d[c, :, qt0 * 128:qt0 * 128 + nw], yaTs[:, c, 0:nw], reads=['yaTs'], writes=[('yT_d', c)], sem='yTd')
        S.barrier()


def phase_ssd(K, layer):
    S, nc, psf, psb = K.S, K.nc, K.psf, K.psb
    identb, identf, eps_t, ones_f = K.identb, K.identf, K.eps_t, K.ones_f
    projT_d = dram(K, 'projT_d'); projF_d = dram(K, 'projF_d'); yT_d = dram(K, 'yT_d')
    with ExitStack() as pes:
        sb = lambda name, shape, dt=F32: pes.enter_context(nc.sbuf_tensor(f"L{layer}_p3_{name}", list(shape), dt))
        tri = [sb(f"tri{d}", (128, 128)) for d in range(2)]
        negm = [sb(f"negm{d}", (128, 128)) for d in range(2)]
        gmask = sb("gmask", (128, 2)); bmask = sb("bmask", (128, 512))
        cst = sb("cst", (32, 128)); cwT = sb("cwT", (128, 32))
        xin = [sb(f"xin{j}", (128, T)) for j in range(2)]
        acc = sb("acc", (128, T))
        xcF = sb("xcF", (128, T), BF16)
        BTp = [sb(f"BTp{g}", (128, T), BF16) for g in range(2)]
        CT = sb("CT", (128, T), BF16)
        xTM = sb("xTM", (128, NT, 512), BF16)
        BTM = sb("BTM", (128, NT, 128), BF16)
        dtw = sb("dtw", (128, NT, 72)); dtd = sb("dtd", (128, NT, 8)); adt = sb("adt", (128, NT, 8))
        prow = sb("prow", (128, 40)); arow = sb("arow", (128, 8)); grow = sb("grow", (128, 512))
        xd = sb("xd", (128, NT, 512), BF16)
        yacc = sb("yacc", (128, NT, 512))
        rhsA = sb("rhsA", (128, 8, 128)); acol = sb("acol", (128, 8))
        diff = sb("diff", (128, 8, 128)); LT = sb("LT", (128, 8, 128)); MT = sb("MT", (128, 8, 128), BF16)
        Erow = sb("Erow", (128, 8, 128)); CTs = sb("CTs", (128, 8, 128), BF16)
        dte = sb("dte", (128, 8)); atot = sb("atot", (128, 8)); xdd = sb("xdd", (128, 512), BF16)
        ST = sb("ST", (128, 512)); STm = sb("STm", (128, 512), BF16); cst2 = sb("cst2", (128, 512))
        zt = sb("zt", (128, 512)); yz = sb("yz", (128, 512)); ysq = sb("ysq", (128, 512)); yss = sb("yss", (128, 1))
        ybn = sb("ybn", (128, 512), BF16); ybTs = sb("ybTs", (128, 4, 128), BF16)
        for d, nm in enumerate(("triF", "triB")):
            S.dma('sp', tri[d][:], dram(K, nm, (128, 128))[:, :], writes=['tri'])
        for d, nm in enumerate(("negF", "negBm")):
            S.dma('sp', negm[d][:], dram(K, nm, (128, 128))[:, :], writes=['negm'])
        S.dma('sp', bmask[:], dram(K, 'bmask', (128, 512))[:, :], writes=['bmask'])
        S.op('dve', lambda e: e.tensor_copy(out=gmask[:, 0:1], in_=bmask[:, 0:1]), reads=['bmask'], writes=['gmask'])
        S.op('dve', lambda e: e.tensor_copy(out=gmask[:, 1:2], in_=bmask[:, 256:257]), reads=['bmask', 'gmask'], writes=['gmask'])
        S.op('dve', lambda e: e.memset(cst[:], 0.0), writes=['cst'])
        S.dma('sp', cst[0:18, :], dram(K, f'ssd_conv_w_{layer}').rearrange("k (c p) -> (k c) p", p=128), reads=['cst'], writes=['cst'])
        S.dma('sp', cst[18:24, :], dram(K, f'ssd_conv_b_{layer}').rearrange("(c p) -> c p", p=128), reads=['cst'], writes=['cst'])
        S.op('pe', lambda e: e.transpose(out=psf[:, 0, 0:32], in_=cst[:, :], identity=identf[0:32, 0:32]), reads=['cst', 'identf'], writes=[('psf', 0)])
        S.op('dve', lambda e: e.tensor_copy(out=cwT[:], in_=psf[:, 0, 0:32]), reads=[('psf', 0)], writes=['cwT'])
        S.dma('sp', prow[:], dram(K, f'ssdp_{layer}', (1, 40)).partition_broadcast(128), writes=['prow'])
        S.dma('sp', grow[:], dram(K, f'ssd_norm_g_{layer}').rearrange("(o n) -> o n", o=1).partition_broadcast(128), writes=['grow'])
        S.dma('sp', dtw[:], projT_d[:, 1216:1288].rearrange("(t p) h -> p t h", p=128), reads=[('projT_d', t) for t in range(NT)], writes=['dtw'])
        segs = [(0, TC), (TC, T)]
        for c in range(6):
            j = c % 2
            S.dma('sp', xin[j][:], projF_d[c], reads=[('projF_d', c)], writes=[('xin', j)], sem=f'xin{j}')
            w0 = cwT[:, c:c + 1]; w1 = cwT[:, 6 + c:7 + c]; w2 = cwT[:, 12 + c:13 + c]; bb = cwT[:, 18 + c:19 + c]
            S.op('dve', lambda e, j=j, w1=w1: e.tensor_scalar(out=acc[:], in0=xin[j][:], scalar1=w1, scalar2=None, op0=ALU.mult), reads=[('xin', j), 'cwT'], writes=['acc'])
            for (s0, s1) in segs:
                S.op('dve', lambda e, j=j, w0=w0, s0=s0, s1=s1: e.scalar_tensor_tensor(out=acc[:, s0 + 1:s1], in0=xin[j][:, s0:s1 - 1], scalar=w0, in1=acc[:, s0 + 1:s1], op0=ALU.mult, op1=ALU.add),
                     reads=[('xin', j), 'acc'], writes=['acc'])
                S.op('dve', lambda e, j=j, w2=w2, s0=s0, s1=s1: e.scalar_tensor_tensor(out=acc[:, s0:s1 - 1], in0=xin[j][:, s0 + 1:s1], scalar=w2, in1=acc[:, s0:s1 - 1], op0=ALU.mult, op1=ALU.add),
                     reads=[('xin', j), 'acc'], writes=['acc'])
            if c < 5:
                S.op('act', lambda e, bb=bb: e.activation(out=xcF[:], in_=acc[:], func=AF.Silu, bias=bb), reads=['acc', 'cwT'], writes=['xcF'])
                if c == 4:
                    for g in range(2):
                        S.op('dve', lambda e, g=g: e.tensor_scalar(out=BTp[g][:], in0=xcF[:], scalar1=gmask[:, g:g + 1], scalar2=None, op0=ALU.mult), reads=['xcF', 'gmask'], writes=['BTp'])
                for t in range(NT):
                    S.op('pe', lambda e, t=t: e.transpose(out=psb[:, t % 2, 0:128], in_=xcF[:, t * 128:(t + 1) * 128], identity=identb[:]), reads=['xcF', 'identb'], writes=[('psb', t % 2)])
                    dst = xTM[:, t, c * 128:(c + 1) * 128] if c < 4 else BTM[:, t, :]
                    if t % 2:
                        S.op('dve', lambda e, t=t, dst=dst: e.tensor_scalar(out=dst, in0=psb[:, t % 2, 0:128], scalar1=1.0, scalar2=None, op0=ALU.mult), reads=[('psb', t % 2)], writes=['xTM'])
                    else:
                        S.op('act', lambda e, t=t, dst=dst: e.activation(out=dst, in_=psb[:, t % 2, 0:128], func=AF.Copy), reads=[('psb', t % 2)], writes=['xTM'])
            else:
                S.op('act', lambda e, bb=bb: e.activation(out=CT[:], in_=acc[:], func=AF.Silu, bias=bb), reads=['acc', 'cwT'], writes=['CT'])
        h3 = lambda ap: ap.rearrange("p (h q) -> p h q", h=8)
        for t in range(NT):
            S.op('dve', lambda e, t=t: e.tensor_tensor(out=h3(yacc[:, t, :]), in0=h3(xTM[:, t, :]), in1=prow[:, 32:40].unsqueeze(2).broadcast_to([128, 8, 64]), op=ALU.mult), reads=['xTM', 'prow'], writes=[('yacc', t)])
        for d in range(2):
            S.op('act', lambda e, d=d: e.activation(out=arow[:], in_=prow[:, 16 + 8 * d:24 + 8 * d], func=AF.Exp), reads=['prow'], writes=['arow'])
            S.op('dve', lambda e, d=d: e.tensor_tensor(out=dtd[:], in0=dtw[:, :, 64:72], in1=prow[:, 8 * d:8 * d + 8].unsqueeze(1).broadcast_to([128, NT, 8]), op=ALU.add), reads=['dtw', 'prow'], writes=['dtd'])
            S.op('act', lambda e: e.activation(out=dtd[:], in_=dtd[:], func=AF.Exp), reads=['dtd'], writes=['dtd'])
            S.op('act', lambda e: e.activation(out=dtd[:], in_=dtd[:], func=AF.Ln, bias=1.0), reads=['dtd'], writes=['dtd'])
            S.op('dve', lambda e: e.scalar_tensor_tensor(out=adt[:], in0=dtd[:], scalar=-1.0, in1=arow[:].unsqueeze(1).broadcast_to([128, NT, 8]), op0=ALU.mult, op1=ALU.mult), reads=['dtd', 'arow'], writes=['adt'])
            for t in range(NT):
                S.op('dve', lambda e, t=t: e.tensor_tensor(out=h3(xd[:, t, :]), in0=h3(xTM[:, t, :]), in1=dtd[:, t, :].unsqueeze(2).broadcast_to([128, 8, 64]), op=ALU.mult), reads=['xTM', 'dtd'], writes=['xd'])
            S.op('dve', lambda e: e.memset(ST[:], 0.0), writes=['ST'])
            S.op('dve', lambda e: e.memset(STm[:], 0.0), writes=['STm'])
            order = list(range(NT)) if d == 0 else [1, 0] + list(range(NT - 1, 1, -1))
            last = 127 if d == 0 else 0
            for c in order:
                cs = slice(c * 128, (c + 1) * 128)
                S.op('dve', lambda e, c=c, d=d: e.tensor_tensor(out=rhsA[:], in0=tri[d][:].unsqueeze(1).broadcast_to([128, 8, 128]), in1=adt[:, c, :].unsqueeze(2).broadcast_to([128, 8, 128]), op=ALU.mult),
                     reads=['tri', 'adt'], writes=['rhsA'])

                def mmA(e, c=c, d=d):
                    e.matmul(psf[:, 0, :], lhsT=ones_f[:], rhs=rhsA[:, 0:4, :].rearrange("p h l -> p (h l)"), start=True, stop=True)
                    e.matmul(psf[:, 1, :], lhsT=ones_f[:], rhs=rhsA[:, 4:8, :].rearrange("p h l -> p (h l)"), start=True, stop=True)
                    e.matmul(psf[:, 2, 0:8], lhsT=tri[d][:], rhs=adt[:, c, :], start=True, stop=True)
                    for g in range(2):
                        ins = e.matmul(psf[:, 3, g * 128:(g + 1) * 128], lhsT=BTp[g][:, c * 128:(c + 1) * 128], rhs=CT[:, c * 128:(c + 1) * 128], start=True, stop=True)
                    return ins
                S.op('pe', mmA, reads=['rhsA', 'ones_f', 'tri', 'adt', 'BTp', 'CT'], writes=[('psf', 0), ('psf', 1), ('psf', 2), ('psf', 3)])
                arow_ps = psf[:, 0:2, :].rearrange("p a (h l) -> p (a h) l", h=4)
                P01 = [('psf', 0), ('psf', 1)]
                S.op('dve', lambda e: e.tensor_copy(out=acol[:], in_=psf[:, 2, 0:8]), reads=[('psf', 2)], writes=['acol'])
                S.op('dve', lambda e, arow_ps=arow_ps: e.tensor_tensor(out=diff[:], in0=arow_ps, in1=acol[:].unsqueeze(2).broadcast_to([128, 8, 128]), op=ALU.subtract), reads=P01 + ['acol'], writes=['diff'])
                S.op('dve', lambda e, arow_ps=arow_ps, last=last: e.tensor_tensor(out=dte[:], in0=arow_ps[:, :, last], in1=acol[:], op=ALU.subtract), reads=P01 + ['acol'], writes=['dte'])
                S.op('dve', lambda e, d=d: e.tensor_tensor(out=diff[:], in0=diff[:], in1=negm[d][:].unsqueeze(1).broadcast_to([128, 8, 128]), op=ALU.add), reads=['diff', 'negm'], writes=['diff'])
                S.op('act', lambda e, arow_ps=arow_ps: e.activation(out=Erow[:], in_=arow_ps, func=AF.Exp), reads=P01, writes=['Erow'])
                S.op('act', lambda e, arow_ps=arow_ps, last=last: e.activation(out=atot[:], in_=arow_ps[:, :, last], func=AF.Exp), reads=P01, writes=['atot'])
                S.op('act', lambda e: e.activation(out=LT[:], in_=diff[:], func=AF.Exp), reads=['diff'], writes=['LT'])
                S.op('act', lambda e: e.activation(out=dte[:], in_=dte[:], func=AF.Exp), reads=['dte'], writes=['dte'])
                S.op('dve', lambda e: e.tensor_tensor(out=MT[:].rearrange("p (g r) l -> p g r l", g=2), in0=LT[:].rearrange("p (g r) l -> p g r l", g=2),
                                                      in1=psf[:, 3, 0:256].rearrange("p (g l) -> p g l", g=2).unsqueeze(2).broadcast_to([128, 2, 4, 128]), op=ALU.mult), reads=['LT', ('psf', 3)], writes=['MT'])
                S.op('dve', lambda e, cs=cs: e.tensor_tensor(out=CTs[:], in0=Erow[:], in1=CT[:, cs].unsqueeze(1).broadcast_to([128, 8, 128]), op=ALU.mult), reads=['Erow', 'CT'], writes=['CTs'])
                S.op('dve', lambda e, c=c: e.tensor_tensor(out=h3(xdd[:]), in0=h3(xd[:, c, :]), in1=dte[:].unsqueeze(2).broadcast_to([128, 8, 64]), op=ALU.mult),
                     reads=['xd', 'dte'], writes=['xdd'])

                def mmY(e, c=c):
                    for h in range(8):
                        e.matmul(psf[:, 4, h * 64:(h + 1) * 64], lhsT=MT[:, h, :], rhs=xd[:, c, h * 64:(h + 1) * 64], start=True, stop=False)
                        e.matmul(psf[:, 4, h * 64:(h + 1) * 64], lhsT=CTs[:, h, :], rhs=STm[:, h * 64:(h + 1) * 64], start=False, stop=True)
                    return e.matmul(psf[:, 5, :], lhsT=BTM[:, c, :], rhs=xdd[:], start=True, stop=True)
                S.op('pe', mmY, reads=['MT', 'xd', 'CTs', 'STm', 'xTM', 'xdd'], writes=[('psf', 4), ('psf', 5)])
                S.op('dve', lambda e, c=c: e.tensor_tensor(out=yacc[:, c, :], in0=yacc[:, c, :], in1=psf[:, 4, :], op=ALU.add), reads=[('yacc', c), ('psf', 4)], writes=[('yacc', c)])
                S.op('dve', lambda e: e.tensor_tensor(out=cst2[:], in0=psf[:, 5, :], in1=bmask[:], op=ALU.mult), reads=[('psf', 5), 'bmask'], writes=['cst2'])
                S.op('dve', lambda e: e.tensor_tensor(out=h3(ST[:]), in0=h3(ST[:]), in1=atot[:].unsqueeze(2).broadcast_to([128, 8, 64]), op=ALU.mult), reads=['ST', 'atot'], writes=['ST'])
                S.op('dve', lambda e: e.tensor_tensor(out=ST[:], in0=ST[:], in1=cst2[:], op=ALU.add), reads=['ST', 'cst2'], writes=['ST'])
                S.op('dve', lambda e: e.tensor_copy(out=STm[:], in_=ST[:]), reads=['ST'], writes=['STm'])
        for t in range(NT):
            S.dma('sp', zt[:], projT_d[t * 128:(t + 1) * 128, 768:1280], reads=[('projT_d', t)], writes=['zt'], sem='zt')
            S.op('act', lambda e: e.activation(out=zt[:], in_=zt[:], func=AF.Silu), reads=['zt'], writes=['zt'])
            S.op('dve', lambda e, t=t: e.tensor_tensor(out=yz[:], in0=yacc[:, t, :], in1=zt[:], op=ALU.mult), reads=[('yacc', t), 'zt'], writes=['yz'])
            S.op('act', lambda e: e.activation(out=ysq[:], in_=yz[:], func=AF.Square, accum_out=yss[:]), reads=['yz'], writes=['ysq', 'yss'])
            S.op('act', lambda e: e.activation(out=yss[:], in_=yss[:], func=AF.Sqrt, scale=1.0 / 512, bias=eps_t[:]), reads=['yss', 'eps'], writes=['yss'])
            S.op('dve', lambda e: e.reciprocal(out=yss[:], in_=yss[:]), reads=['yss'], writes=['yss'])
            S.op('dve', lambda e: e.scalar_tensor_tensor(out=ybn[:], in0=yz[:], scalar=yss[:, 0:1], in1=grow[:], op0=ALU.mult, op1=ALU.mult), reads=['yz', 'yss', 'grow'], writes=['ybn'])

            def tryb(e):
                for c in range(4):
                    ins = e.transpose(out=psb[:, 0, c * 128:(c + 1) * 128], in_=ybn[:, c * 128:(c + 1) * 128], identity=identb[:])
                return ins
            S.op('pe', tryb, reads=['ybn', 'identb'], writes=[('psb', 0)])
            S.op('act', lambda e: e.activation(out=ybTs[:], in_=psb[:, 0, 0:512].rearrange("p (c q) -> p c q", c=4), func=AF.Copy), reads=[('psb', 0)], writes=['ybTs'])
            for c in range(4):
                S.dma('sp', yT_d[4 + c, :, t * 128:(t + 1) * 128], ybTs[:, c, :], reads=['ybTs'], writes=[('yT_d', 4 + c)], sem='yTd')
        S.barrier()


def phase_sc(K, layer):
    S, nc, psf = K.S, K.nc, K.psf
    identf = K.identf
    projF_d = dram(K, 'projF_d'); yT_d = dram(K, 'yT_d')
    with ExitStack() as pes:
        sb = lambda name, shape, dt=F32: pes.enter_context(nc.sbuf_tensor(f"L{layer}_p5_{name}", list(shape), dt))
        cst = sb("cst", (32, 128)); cwT = sb("cwT", (128, 32))
        tb = [sb(f"tb{j}", (128, T)) for j in range(2)]
        tg = [sb(f"tg{j}", (128, T)) for j in range(2)]
        th = [sb(f"th{j}", (128, T)) for j in range(2)]
        pr = sb("pr", (128, T)); acc = sb("acc", (128, T)); yd = sb("yd", (128, T), BF16)
        S.op('dve', lambda e: e.memset(cst[:], 0.0), writes=['cst'])
        S.dma('sp', cst[0:9, :], dram(K, f'sc_conv_w_{layer}').rearrange("k (c p) -> (k c) p", p=128), reads=['cst'], writes=['cst'])
        S.op('pe', lambda e: e.transpose(out=psf[:, 0, 0:32], in_=cst[:, :], identity=identf[0:32, 0:32]), reads=['cst', 'identf'], writes=[('psf', 0)])
        S.op('dve', lambda e: e.tensor_copy(out=cwT[:], in_=psf[:, 0, 0:32]), reads=[('psf', 0)], writes=['cwT'])
        segs = [(0, TC), (TC, T)]
        for c in range(3):
            j = c % 2
            S.dma('sp', tb[j][:], projF_d[9 + c], reads=[('projF_d', 9 + c)], writes=[('tb', j)], sem=f'scb{j}')
            S.dma('sp', tg[j][:], projF_d[12 + c], reads=[('projF_d', 12 + c)], writes=[('tg', j)], sem=f'scb{j}')
            S.dma('sp', th[j][:], projF_d[15 + c], reads=[('projF_d', 15 + c)], writes=[('th', j)], sem=f'scb{j}')
            w0 = cwT[:, c:c + 1]; w1 = cwT[:, 3 + c:4 + c]; w2 = cwT[:, 6 + c:7 + c]
            S.op('dve', lambda e, j=j: e.tensor_tensor(out=pr[:], in0=tg[j][:], in1=th[j][:], op=ALU.mult), reads=[('tg', j), ('th', j)], writes=['pr'])
            S.op('dve', lambda e, w1=w1: e.tensor_scalar(out=acc[:], in0=pr[:], scalar1=w1, scalar2=None, op0=ALU.mult), reads=['pr', 'cwT'], writes=['acc'])
            for (s0, s1) in segs:
                S.op('dve', lambda e, w0=w0, s0=s0, s1=s1: e.scalar_tensor_tensor(out=acc[:, s0 + 1:s1], in0=pr[:, s0:s1 - 1], scalar=w0, in1=acc[:, s0 + 1:s1], op0=ALU.mult, op1=ALU.add), reads=['pr', 'acc'], writes=['acc'])
                S.op('dve', lambda e, w2=w2, s0=s0, s1=s1: e.scalar_tensor_tensor(out=acc[:, s0:s1 - 1], in0=pr[:, s0 + 1:s1], scalar=w2, in1=acc[:, s0:s1 - 1], op0=ALU.mult, op1=ALU.add), reads=['pr', 'acc'], writes=['acc'])
            S.op('dve', lambda e, j=j: e.tensor_tensor(out=yd[:], in0=acc[:], in1=tb[j][:], op=ALU.mult), reads=['acc', ('tb', j)], writes=['yd'])
            S.dma('sp', yT_d[11 + c], yd[:], reads=['yd'], writes=[('yT_d', 11 + c)], sem='yTd')
        S.barrier()


def phase_merge(K, layer):
    S, nc, psf = K.S, K.nc, K.psf
    hT_d = dram(K, 'hT_d'); yT_d = dram(K, 'yT_d'); modrow_d = dram(K, 'modrow_d'); x_d = dram(K, 'x_d')
    xo_d = dram(K, 'xo_d', (T, D)) if K.kinds.get('xo_d') else x_d
    with ExitStack() as pes:
        sb = lambda name, shape, dt=F32: pes.enter_context(nc.sbuf_tensor(f"L{layer}_p6_{name}", list(shape), dt))
        wg = sb("wg", (128, 8, 4096), BF16)
        wbr = sb("wbr", (128, 14, 1024), BF16)
        wout = sb("wout", (128, 8, 1024), BF16)
        grow = sb("grow", (128, 2, 1024))
        hT = sb("hT", (128, 8, 512), BF16); yT = sb("yT", (128, 14, 512), BF16)
        mT = sb("mT", (128, 8, 512), BF16)
        gs = [sb(f"gs{j}", (128, 512)) for j in range(2)]
        macc = sb("macc", (128, 512)); mtmp = sb("mtmp", (128, 512))
        xt = [sb(f"xt{j}", (128, 1024)) for j in range(2)]
        ytmp = sb("ytmp", (128, 1024))
        wv = dram(K, f'w_in_{layer}').rearrange("(k p) n -> p k n", p=128)
        for i in range(8):
            S.dma('pool', wg[:, :, i * 512:(i + 1) * 512], wv[:, :, 3592 + i * 512:3592 + (i + 1) * 512], writes=['wg'], sem='mw')
        k0 = 0
        for nm, nk in (('w_br_attn', 4), ('w_br_ssd', 4), ('w_br_s5', 3), ('w_br_sc', 3)):
            S.dma('pool', wbr[:, k0:k0 + nk, :], dram(K, f'{nm}_{layer}').rearrange("(k p) n -> p k n", p=128), writes=['wbr'], sem='mw')
            k0 += nk
        S.dma('pool', wout[:], dram(K, f'w_out_{layer}').rearrange("(k p) n -> p k n", p=128), writes=['wout'], sem='mw')
        for w in range(2):
            S.dma('sp', grow[:, w, :], modrow_d[w:w + 1, 16:24, :].rearrange("o j p -> o (j p)").partition_broadcast(128), reads=['modrow_d'], writes=['grow'])
        kr = [(0, 4), (4, 8), (8, 11), (11, 14)]
        pi = 0
        for (n0, nw) in TOKCH:
            S.dma('sp', hT[:, :, 0:nw], hT_d[:, :, n0:n0 + nw].rearrange("k p n -> p k n"), reads=[('hT_d', f) for f in range(8)], writes=['hT'], sem='mh')
            S.dma('sp', yT[:, :, 0:nw], yT_d[:, :, n0:n0 + nw].rearrange("k p n -> p k n"), reads=[('yT_d', f) for f in range(14)], writes=['yT'], sem='mh')
            for f in range(8):
                for b in range(4):
                    pg = pi % 2; pi += 1

                    def mmg(e, f=f, b=b, pg=pg, nw=nw):
                        for k in range(8):
                            ins = e.matmul(psf[:, pg, 0:nw], lhsT=wg[:, k, b * 1024 + f * 128:b * 1024 + (f + 1) * 128], rhs=hT[:, k, 0:nw], start=(k == 0), stop=(k == 7))
                        return ins
                    S.op('pe', mmg, reads=['wg', 'hT'], writes=[('psf', pg)])
                    S.op('act', lambda e, pg=pg, nw=nw: e.activation(out=gs[pg][:, 0:nw], in_=psf[:, pg, 0:nw], func=AF.Sigmoid), reads=[('psf', pg)], writes=[('gs', pg)])

                    def mmb(e, f=f, b=b, pg=pg, nw=nw):
                        ks = list(range(*kr[b]))
                        for k in ks:
                            ins = e.matmul(psf[:, 2 + pg, 0:nw], lhsT=wbr[:, k, f * 128:(f + 1) * 128], rhs=yT[:, k, 0:nw], start=(k == ks[0]), stop=(k == ks[-1]))
                        return ins
                    S.op('pe', mmb, reads=['wbr', 'yT'], writes=[('psf', 2 + pg)])
                    if b == 0:
                        S.op('dve', lambda e, pg=pg, nw=nw: e.tensor_tensor(out=macc[:, 0:nw], in0=psf[:, 2 + pg, 0:nw], in1=gs[pg][:, 0:nw], op=ALU.mult), reads=[('psf', 2 + pg), ('gs', pg)], writes=['macc'])
                    else:
                        S.op('dve', lambda e, pg=pg, nw=nw: e.tensor_tensor(out=mtmp[:, 0:nw], in0=psf[:, 2 + pg, 0:nw], in1=gs[pg][:, 0:nw], op=ALU.mult), reads=[('psf', 2 + pg), ('gs', pg)], writes=['mtmp'])
                        if b < 3:
                            S.op('dve', lambda e, nw=nw: e.tensor_tensor(out=macc[:, 0:nw], in0=macc[:, 0:nw], in1=mtmp[:, 0:nw], op=ALU.add), reads=['macc', 'mtmp'], writes=['macc'])
                        else:
                            S.op('dve', lambda e, nw=nw, f=f: e.tensor_tensor(out=mT[:, f, 0:nw], in0=macc[:, 0:nw], in1=mtmp[:, 0:nw], op=ALU.add), reads=['macc', 'mtmp'], writes=['mT'])
            for i in range(nw // 128):
                t = n0 // 128 + i
                w = 1 if t < 2 else 0
                j = t % 2
                S.dma('sp', xt[j][:], x_d[t * 128:(t + 1) * 128, :], reads=[('x_d', t)], writes=[('xt', j)], sem=f'mx{j}')

                def mmo(e, i=i):
                    for hh in range(2):
                        for k in range(8):
                            ins = e.matmul(psf[:, 4 + hh, :], lhsT=mT[:, k, i * 128:(i + 1) * 128], rhs=wout[:, k, hh * 512:(hh + 1) * 512], start=(k == 0), stop=(k == 7))
                    return ins
                S.op('pe', mmo, reads=['mT', 'wout'], writes=[('psf', 4), ('psf', 5)])
                S.op('dve', lambda e, w=w: e.tensor_tensor(out=ytmp[:], in0=psf[:, 4:6, :].rearrange("p a b -> p (a b)"), in1=grow[:, w, :], op=ALU.mult), reads=[('psf', 4), ('psf', 5), 'grow'], writes=['ytmp'])
                S.op('dve', lambda e, j=j: e.tensor_tensor(out=xt[j][:], in0=xt[j][:], in1=ytmp[:], op=ALU.add), reads=[('xt', j), 'ytmp'], writes=[('xt', j)])
                S.dma('sp', xo_d[t * 128:(t + 1) * 128, :], xt[j][:], reads=[('xt', j)], writes=[('x_d', t), ('xo_d', t)], sem=f'mx{j}')
        S.barrier()


def phase_moe(K, layer):
    S, nc, psf, psb = K.S, K.nc, K.psf, K.psb
    identf, identb, eps_t, ones_f = K.identf, K.identb, K.eps_t, K.ones_f
    NER = int(os.environ.get("KNE", str(NE)))
    x_d = dram(K, 'x_d'); modrow_d = dram(K, 'modrow_d')
    xo_d = dram(K, 'xo_d', (T, D)) if K.kinds.get('xo_d') else x_d
    wgd = dram(K, f'w_exp_gate_{layer}', (NER, D, D)); wud = dram(K, f'w_exp_up_{layer}', (NER, D, D)); wdd = dram(K, f'w_exp_down_{layer}', (NER, D, D))
    with ExitStack() as pes:
        sb = lambda name, shape, dt=F32: pes.enter_context(nc.sbuf_tensor(f"L{layer}_p7_{name}", list(shape), dt))
        modv = load_modv(K, sb)
        S2 = lambda w, f: modv[:, w * 48 + 24 + f:w * 48 + 25 + f]
        G2 = lambda w, f: modv[:, 112 + w * 8 + f:113 + w * 8 + f]
        xnb = sb("xnb", (128, NT, 1024), BF16)
        macc = sb("macc", (128, NT, 1024))
        aff = sb("aff", (128, NT, 16)); mask = sb("mask", (128, NT, 16)); affm = sb("affm", (128, NT, 16)); pos = sb("pos", (128, NT, 16))
        iota = sb("iota", (128, 256)); triS = sb("triS", (128, 128)); wr = sb("wr", (128, 8, 16))
        S.dma('sp', iota[:], dram(K, 'iota', (128, 256))[:, :], writes=['iota'])
        S.dma('sp', triS[:], dram(K, 'triS', (128, 128))[:, :], writes=['triS'])
        S.dma('sp', wr[:], dram(K, f'w_router_{layer}').rearrange("(k p) e -> p k e", p=128), writes=['wr'])
        S.op('pool', lambda e: e.memset(macc[:], 0.0), writes=['macc'])
        mats = []
        for ei in range(NER):
            mats += [(wgd, ei), (wud, ei), (wdd, ei)]
        wstate = {'next': 0}

        def issue_weights(upto):
            while wstate['next'] < min(upto, len(mats)):
                mi = wstate['next']; wstate['next'] += 1
                src, ei = mats[mi]
                v = src[ei].rearrange("(k p) n -> p k n", p=128)
                for hh in range(2):
                    S.dma('pool', Wb[mi % 4][:, hh * 4:(hh + 1) * 4, :], v[:, hh * 4:(hh + 1) * 4, :], writes=[('Wb', mi % 4)], sem=f'wb{mi % 4}')
        with ExitStack() as aes:
            sa = lambda name, shape, dt=F32: aes.enter_context(nc.sbuf_tensor(f"L{layer}_p7a_{name}", list(shape), dt))
            xt = [sa(f"xt{j}", (128, 1024)) for j in range(2)]
            sq = sa("sq", (128, 1024)); xnf = sa("xnf", (128, 1024))
            ss = sa("ss", (128, 1)); rstd = sa("rstd", (128, 1))
            hT2 = sa("hT2", (128, 8, 128))
            mx = sa("mx", (128, 1)); ssum = sa("ssum", (128, 1))
            affT = sa("affT", (16, T)); wk = sa("wk", (16, TL)); m8 = sa("m8", (16, 8))
            thr = sa("thr", (16, 2)); diag16 = sa("diag16", (16, 16)); throw = sa("throw", (128, 2, 16))
            for t in range(NT):
                w = 1 if t < 2 else 0
                j = t % 2
                S.dma('sp', xt[j][:], x_d[t * 128:(t + 1) * 128, :], reads=[('x_d', t)], writes=[('xt', j)], sem=f'mxt{j}')
                S.op('act', lambda e, j=j: e.activation(out=sq[:], in_=xt[j][:], func=AF.Square, accum_out=ss[:]), reads=[('xt', j)], writes=['sq', 'ss'])
                S.op('act', lambda e: e.activation(out=rstd[:], in_=ss[:], func=AF.Sqrt, scale=1.0 / D, bias=eps_t[:]), reads=['ss', 'eps'], writes=['rstd'])
                S.op('dve', lambda e: e.reciprocal(out=rstd[:], in_=rstd[:]), reads=['rstd'], writes=['rstd'])
                S.op('dve', lambda e, j=j: e.tensor_scalar(out=xnf[:], in0=xt[j][:], scalar1=rstd[:, 0:1], scalar2=None, op0=ALU.mult), reads=[('xt', j), 'rstd'], writes=['xnf'])
                S.op('act', lambda e, t=t: e.activation(out=xnb[:, t, :], in_=xnf[:], func=AF.Copy), reads=['xnf'], writes=[('xnb', t)])

                def trr(e):
                    for f in range(8):
                        ins = e.transpose(out=psf[:, f // 4, (f % 4) * 128:(f % 4 + 1) * 128], in_=xnf[:, f * 128:(f + 1) * 128], identity=identf[:])
                    return ins
                S.op('pe', trr, reads=['xnf', 'identf'], writes=[('psf', 0), ('psf', 1)])
                for f in range(8):
                    S.op('dve' if f % 2 else 'act',
                         (lambda e, f=f, w=w: e.tensor_scalar(out=hT2[:, f, :], in0=psf[:, f // 4, (f % 4) * 128:(f % 4 + 1) * 128], scalar1=G2(w, f), scalar2=S2(w, f), op0=ALU.mult, op1=ALU.add)) if f % 2 else
                         (lambda e, f=f, w=w: e.activation(out=hT2[:, f, :], in_=psf[:, f // 4, (f % 4) * 128:(f % 4 + 1) * 128], func=AF.Identity, scale=G2(w, f), bias=S2(w, f))),
                         reads=[('psf', f // 4), 'modv'], writes=['hT2'])

                def mmr(e):
                    for k in range(8):
                        ins = e.matmul(psf[:, 2, 0:16], lhsT=hT2[:, k, :], rhs=wr[:, k, :], start=(k == 0), stop=(k == 7))
                    return ins
                S.op('pe', mmr, reads=['hT2', 'wr'], writes=[('psf', 2)])
                S.op('dve', lambda e: e.tensor_reduce(out=mx[:], in_=psf[:, 2, 0:16], axis=AX.X, op=ALU.max), reads=[('psf', 2)], writes=['mx'])
                S.op('dve', lambda e: e.tensor_scalar(out=mx[:], in0=mx[:], scalar1=-1.0, scalar2=None, op0=ALU.mult), reads=['mx'], writes=['mx'])
                S.op('act', lambda e, t=t: e.activation(out=aff[:, t, :], in_=psf[:, 2, 0:16], func=AF.Exp, bias=mx[:], accum_out=ssum[:]), reads=[('psf', 2), 'mx'], writes=[('aff', t), 'ssum'])
                S.op('dve', lambda e: e.reciprocal(out=ssum[:], in_=ssum[:]), reads=['ssum'], writes=['ssum'])
                S.op('dve', lambda e, t=t: e.tensor_scalar(out=aff[:, t, :], in0=aff[:, t, :], scalar1=ssum[:, 0:1], scalar2=None, op0=ALU.mult), reads=[('aff', t), 'ssum'], writes=[('aff', t)])
            for g0 in range(0, NT, 4):
                tl = list(range(g0, min(g0 + 4, NT)))

                def tra(e, tl=tl):
                    for i, t in enumerate(tl):
                        ins = e.transpose(out=psf[0:16, 3, i * 128:(i + 1) * 128], in_=aff[:, t, :], identity=identf[:])
                    return ins
                S.op('pe', tra, reads=[('aff', t) for t in tl] + ['identf'], writes=[('psf', 3)])
                S.op('dve', lambda e, g0=g0, n=len(tl): e.tensor_copy(out=affT[:, g0 * 128:(g0 + n) * 128], in_=psf[0:16, 3, 0:n * 128]), reads=[('psf', 3)], writes=['affT'])
            for seg, (c0, c1, nit) in enumerate([(TC, T, 32), (0, TC, 4)]):
                n = c1 - c0
                S.op('dve', lambda e, c0=c0, c1=c1, n=n: e.tensor_copy(out=wk[:, 0:n], in_=affT[:, c0:c1]), reads=['affT', 'wk'], writes=['wk'])
                for it in range(nit):
                    S.op('dve', lambda e, n=n: e.max(out=m8[:], in_=wk[:, 0:n]), reads=['wk'], writes=['m8'])
                    if it < nit - 1:
                        S.op('dve', lambda e, n=n: e.match_replace(out=wk[:, 0:n], in_to_replace=m8[:], in_values=wk[:, 0:n], imm_value=-1.0), reads=['wk', 'm8'], writes=['wk'])
                S.op('dve', lambda e, seg=seg: e.tensor_copy(out=thr[:, seg:seg + 1], in_=m8[:, 7:8]), reads=['m8'], writes=['thr'])
                S.op('dve', lambda e, seg=seg: e.tensor_scalar(out=diag16[:], in0=identf[0:16, 0:16], scalar1=thr[:, seg:seg + 1], scalar2=None, op0=ALU.mult), reads=['thr', 'identf'], writes=['diag16'])
                S.op('pe', lambda e: e.matmul(psf[:, 3, 0:16], lhsT=ones_f[0:16, :], rhs=diag16[:], start=True, stop=True), reads=['diag16', 'ones_f'], writes=[('psf', 3)])
                S.op('dve', lambda e, seg=seg: e.tensor_copy(out=throw[:, seg, :], in_=psf[:, 3, 0:16]), reads=[('psf', 3)], writes=['throw'])
            for t in range(NT):
                seg = 1 if t < 2 else 0
                S.op('dve', lambda e, t=t, seg=seg: e.tensor_tensor(out=mask[:, t, :], in0=aff[:, t, :], in1=throw[:, seg, :], op=ALU.is_ge), reads=[('aff', t), 'throw'], writes=[('mask', t)])
                S.op('dve', lambda e, t=t: e.tensor_tensor(out=affm[:, t, :], in0=aff[:, t, :], in1=mask[:, t, :], op=ALU.mult), reads=[('aff', t), ('mask', t)], writes=[('affm', t)])
            for t in range(NT):
                prev = list(range(0, t)) if t < 2 else list(range(2, t))

                def mmp(e, t=t, prev=prev):
                    ins = e.matmul(psf[:, t % 2, 0:16], lhsT=triS[:], rhs=mask[:, t, :], start=True, stop=(len(prev) == 0))
                    for i, tp in enumerate(prev):
                        ins = e.matmul(psf[:, t % 2, 0:16], lhsT=ones_f[:], rhs=mask[:, tp, :], start=False, stop=(i == len(prev) - 1))
                    return ins
                S.op('pe', mmp, reads=[('mask', tp) for tp in prev + [t]] + ['triS', 'ones_f'], writes=[('psf', t % 2)])
                S.op('dve', lambda e, t=t: e.tensor_copy(out=pos[:, t, :], in_=psf[:, t % 2, 0:16]), reads=[('psf', t % 2)], writes=[('pos', t)])
            S.barrier()
        des = ExitStack()
        sb_outer = sb
        sb = lambda name, shape, dt=F32: des.enter_context(nc.sbuf_tensor(f"L{layer}_p7d_{name}", list(shape), dt))
        Wb = [sb(f"Wb{j}", (128, 8, 1024), BF16) for j in range(4)]
        issue_weights(4)
        Soh = sb("Soh", (128, NT, 256), BF16)
        SWb = [sb(f"SW{q}", (128, 256), BF16) for q in range(2)]; SWTb = [sb(f"SWT{q}", (128, 2, 128), BF16) for q in range(2)]
        xgT = sb("xgT", (128, 8, 288), BF16); hidT = sb("hidT", (128, 8, 288), BF16)
        sg = sb("sg", (128, 288))
        ye = [sb(f"ye{j}", (128, 1024), BF16) for j in range(3)]
        pcnt = 0
        for ei in range(NER):
            Wg, Wu, Wd = Wb[(ei * 3) % 4], Wb[(ei * 3 + 1) % 4], Wb[(ei * 3 + 2) % 4]
            kWg, kWu, kWd = ('Wb', (ei * 3) % 4), ('Wb', (ei * 3 + 1) % 4), ('Wb', (ei * 3 + 2) % 4)
            for t in range(NT):
                ns = 32 if t < 2 else 256
                S.op('dve' if t % 2 else 'pool', lambda e, t=t, ns=ns, ei=ei: e.tensor_scalar(out=Soh[:, t, 0:ns], in0=iota[:, 0:ns], scalar1=pos[:, t, ei:ei + 1], scalar2=mask[:, t, ei:ei + 1], op0=ALU.is_equal, op1=ALU.mult),
                     reads=['iota', ('pos', t), ('mask', t)], writes=[('Soh', t)])
            for f in range(8):
                pb = pcnt % 2; pcnt += 1

                def mmg(e, f=f, pb=pb):
                    for i, t in enumerate(range(2, NT)):
                        ins = e.matmul(psf[:, pb, 0:256], lhsT=xnb[:, t, f * 128:(f + 1) * 128], rhs=Soh[:, t, :], start=(i == 0), stop=(i == NT - 3))
                    for i, t in enumerate(range(2)):
                        ins = e.matmul(psf[:, pb, 256:288], lhsT=xnb[:, t, f * 128:(f + 1) * 128], rhs=Soh[:, t, 0:32], start=(i == 0), stop=(i == 1))
                    return ins
                S.op('pe', mmg, reads=[('xnb', t) for t in range(NT)] + [('Soh', t) for t in range(NT)], writes=[('psf', pb)])
                S.op('act', lambda e, f=f, pb=pb: e.activation(out=xgT[:, f, 0:256], in_=psf[:, pb, 0:256], func=AF.Identity, scale=G2(0, f), bias=S2(0, f)), reads=[('psf', pb), 'modv'], writes=['xgT'])
                S.op('act', lambda e, f=f, pb=pb: e.activation(out=xgT[:, f, 256:288], in_=psf[:, pb, 256:288], func=AF.Identity, scale=G2(1, f), bias=S2(1, f)), reads=[('psf', pb), 'modv'], writes=['xgT'])
            for m in range(8):
                pb = 2 * (m % 2)

                def mmf(e, m=m, pb=pb):
                    for k in range(8):
                        e.matmul(psf[:, pb, 0:288], lhsT=Wg[:, k, m * 128:(m + 1) * 128], rhs=xgT[:, k, :], start=(k == 0), stop=(k == 7))
                    for k in range(8):
                        ins = e.matmul(psf[:, pb + 1, 0:288], lhsT=Wu[:, k, m * 128:(m + 1) * 128], rhs=xgT[:, k, :], start=(k == 0), stop=(k == 7))
                    return ins
                S.op('pe', mmf, reads=[kWg, kWu, 'xgT'], writes=[('psf', pb), ('psf', pb + 1)])
                S.op('act', lambda e, pb=pb: e.activation(out=sg[:], in_=psf[:, pb, 0:288], func=AF.Silu), reads=[('psf', pb)], writes=['sg'])
                S.op('dve', lambda e, m=m, pb=pb: e.tensor_tensor(out=hidT[:, m, :], in0=psf[:, pb + 1, 0:288], in1=sg[:], op=ALU.mult), reads=[('psf', pb + 1), 'sg'], writes=['hidT'])
            issue_weights(ei * 3 + 6)
            for sidx, (s0, sn) in enumerate([(0, 128), (128, 128), (256, 32)]):
                def mmd(e, s0=s0, sn=sn):
                    for hh in range(2):
                        for m in range(8):
                            ins = e.matmul(psf[0:sn, 4 + hh, :], lhsT=hidT[:, m, s0:s0 + sn], rhs=Wd[:, m, hh * 512:(hh + 1) * 512], start=(m == 0), stop=(m == 7))
                    return ins
                S.op('pe', mmd, reads=['hidT', kWd], writes=[('psf', 4), ('psf', 5)])
                S.op('act', lambda e, sidx=sidx, sn=sn: e.activation(out=ye[sidx][0:sn, :], in_=psf[0:sn, 4:6, :].rearrange("p a b -> p (a b)"), func=AF.Copy), reads=[('psf', 4), ('psf', 5)], writes=[('ye', sidx)])
            issue_weights(ei * 3 + 7)
            for t in range(NT):
                ns = 32 if t < 2 else 256
                j = t % 2
                pb = 2 * (t % 2)
                SW = SWb[j]; SWT = SWTb[j]
                S.op('pool', lambda e, t=t, ns=ns, ei=ei, SW=SW: e.tensor_scalar(out=SW[:, 0:ns], in0=iota[:, 0:ns], scalar1=pos[:, t, ei:ei + 1], scalar2=affm[:, t, ei:ei + 1], op0=ALU.is_equal, op1=ALU.mult),
                     reads=['iota', ('pos', t), ('affm', t)], writes=[('SW', j)])
                if t >= 2:
                    def trs(e, j=j, SW=SW):
                        e.transpose(out=psb[:, j, 0:128], in_=SW[:, 0:128], identity=identb[:])
                        return e.transpose(out=psb[:, j, 128:256], in_=SW[:, 128:256], identity=identb[:])
                    S.op('pe', trs, reads=[('SW', j), 'identb'], writes=[('psb', j)])
                    S.op('act', lambda e, j=j, SWT=SWT: e.activation(out=SWT[:].rearrange("p s q -> p (s q)"), in_=psb[:, j, 0:256], func=AF.Copy), reads=[('psb', j)], writes=[('SWT', j)])

                    def mms(e, pb=pb, SWT=SWT):
                        for hh in range(2):
                            for s_ in range(2):
                                ins = e.matmul(psf[:, pb + hh, :], lhsT=SWT[:, s_, :], rhs=ye[s_][:, hh * 512:(hh + 1) * 512], start=(s_ == 0), stop=(s_ == 1))
                        return ins
                    S.op('pe', mms, reads=[('SWT', j), ('ye', 0), ('ye', 1)], writes=[('psf', pb), ('psf', pb + 1)])
                else:
                    S.op('pe', lambda e, j=j, SW=SW: e.transpose(out=psb[0:32, j, 0:128], in_=SW[:, 0:32], identity=identb[:]), reads=[('SW', j), 'identb'], writes=[('psb', j)])
                    S.op('act', lambda e, j=j, SWT=SWT: e.activation(out=SWT[0:32, 0, :], in_=psb[0:32, j, 0:128], func=AF.Copy), reads=[('psb', j)], writes=[('SWT', j)])

                    def mms(e, pb=pb, SWT=SWT):
                        for hh in range(2):
                            ins = e.matmul(psf[:, pb + hh, :], lhsT=SWT[0:32, 0, :], rhs=ye[2][0:32, hh * 512:(hh + 1) * 512], start=True, stop=True)
                        return ins
                    S.op('pe', mms, reads=[('SWT', j), ('ye', 2)], writes=[('psf', pb), ('psf', pb + 1)])
                S.op('dve', lambda e, t=t, pb=pb: e.tensor_tensor(out=macc[:, t, :], in0=macc[:, t, :], in1=psf[:, pb:pb + 2, :].rearrange("p a b -> p (a b)"), op=ALU.add), reads=[('psf', pb), ('psf', pb + 1), 'macc'], writes=['macc'])
        S.barrier()
        des.close()
        sb = sb_outer
        grow = sb("grow", (128, 2, 1024))
        for w in range(2):
            S.dma('sp', grow[:, w, :], modrow_d[w:w + 1, 40:48, :].rearrange("o j p -> o (j p)").partition_broadcast(128), reads=['modrow_d'], writes=['grow'])
        xr = [sb(f"xr{j}", (128, 1024)) for j in range(2)]
        for t in range(NT):
            w = 1 if t < 2 else 0
            j = t % 2
            S.dma('sp', xr[j][:], x_d[t * 128:(t + 1) * 128, :], reads=[('x_d', t)], writes=[('xr', j)], sem=f'mxr{j}')
            S.op('dve', lambda e, t=t, w=w: e.tensor_tensor(out=macc[:, t, :], in0=macc[:, t, :], in1=grow[:, w, :], op=ALU.mult), reads=['macc', 'grow'], writes=['macc'])
            S.op('dve', lambda e, t=t, j=j: e.tensor_tensor(out=xr[j][:], in0=xr[j][:], in1=macc[:, t, :], op=ALU.add), reads=['macc', ('xr', j)], writes=[('xr', j)])
            S.dma('sp', xo_d[t * 128:(t + 1) * 128, :], xr[j][:], reads=[('xr', j)], writes=[('x_d', t), ('xo_d', t)], sem=f'mxr{j}')
        S.barrier()


def phase_final(K, layer):
    S, nc = K.S, K.nc
    eps_t = K.eps_t
    x_d = dram(K, 'x_d'); out = dram(K, 'out', (TL, D))
    with ExitStack() as pes:
        sb = lambda name, shape, dt=F32: pes.enter_context(nc.sbuf_tensor(f"fin_{name}", list(shape), dt))
        grow = sb("grow", (128, 1024))
        xt = [sb(f"xt{j}", (128, 1024)) for j in range(2)]
        sq = sb("sq", (128, 1024)); ss = sb("ss", (128, 1)); rstd = sb("rstd", (128, 1))
        S.dma('sp', grow[:], dram(K, 'final_norm_g', (D,)).rearrange("(o n) -> o n", o=1).partition_broadcast(128), writes=['grow'])
        for t in range(2, NT):
            j = t % 2
            S.dma('sp', xt[j][:], x_d[t * 128:(t + 1) * 128, :], reads=[('x_d', t)], writes=[('xt', j)], sem=f'fx{j}')
            S.op('act', lambda e, j=j: e.activation(out=sq[:], in_=xt[j][:], func=AF.Square, accum_out=ss[:]), reads=[('xt', j)], writes=['sq', 'ss'])
            S.op('act', lambda e: e.activation(out=rstd[:], in_=ss[:], func=AF.Sqrt, scale=1.0 / D, bias=eps_t[:]), reads=['ss', 'eps'], writes=['rstd'])
            S.op('dve', lambda e: e.reciprocal(out=rstd[:], in_=rstd[:]), reads=['rstd'], writes=['rstd'])
            S.op('dve', lambda e, j=j: e.scalar_tensor_tensor(out=xt[j][:], in0=xt[j][:], scalar=rstd[:, 0:1], in1=grow[:], op0=ALU.mult, op1=ALU.mult), reads=[('xt', j), 'rstd', 'grow'], writes=[('xt', j)])
            S.dma('sp', out[(t - 2) * 128:(t - 1) * 128, :], xt[j][:], reads=[('xt', j)], writes=[('out', t)], sem=f'fx{j}')
        S.barrier()


def phase_s5(K, layer):
    S, nc, psf = K.S, K.nc, K.psf
    identf = K.identf
    projF_d = dram(K, 'projF_d'); yT_d = dram(K, 'yT_d')
    NJ = int(os.environ.get("KNJ", "12"))
    with ExitStack() as pes:
        sb = lambda name, shape, dt=F32: pes.enter_context(nc.sbuf_tensor(f"L{layer}_p4_{name}", list(shape), dt))
        prm = sb("prm", (128, 846)); bmask = sb("bmask", (128, 512)); halfpi = sb("halfpi", (128, 1))
        uF = sb("uF", (128, 3, T)); yS = sb("yS", (128, 3, T))
        S.dma('sp', prm[:], dram(K, f's5p_{layer}', (128, 846))[:, :], writes=['prm'])
        S.dma('sp', bmask[:], dram(K, 'bmask', (128, 512))[:, :], writes=['bmask'])
        for ct in range(3):
            S.dma('sp', uF[:, ct, :], projF_d[6 + ct], reads=[('projF_d', 6 + ct)], writes=['uF'])
        S.op('dve', lambda e: e.memset(halfpi[:], float(np.pi / 2)), writes=['halfpi'])
        gm = [bmask[:, 0:1], bmask[:, 256:257]]
        BRE = prm[:, 72:264].rearrange("p (j i) -> p j i", j=12); BIM = prm[:, 264:456].rearrange("p (j i) -> p j i", j=12)
        CRE = prm[:, 456:648].rearrange("p (j i) -> p j i", j=12); CIM = prm[:, 648:840].rearrange("p (j i) -> p j i", j=12)
        Bb = [[sb(f"Bb{d}{c}", (128, 12, 16)) for c in range(2)] for d in range(2)]
        PR = [sb(f"PR{d}", (128, 12, 12)) for d in range(2)]; PI = [sb(f"PI{d}", (128, 12, 12)) for d in range(2)]; NPI = [sb(f"NPI{d}", (128, 12, 12)) for d in range(2)]
        tt = {n: sb("t_" + n, (128, 12)) for n in ("step", "a", "mag", "ang", "s", "c", "t1", "t2", "abr", "abi", "den", "nr", "cr", "ci", "u1", "u2")}
        tb1 = sb("tb1", (128, 12, 16)); tb2 = sb("tb2", (128, 12, 16))
        V = lambda e, f, r, w: S.op('dve', f, reads=r, writes=w)
        for d in range(2):
            LR = prm[:, 12 * d:12 * d + 12]; LI = prm[:, 24 + 12 * d:36 + 12 * d]; LS = prm[:, 48 + 12 * d:60 + 12 * d]
            S.op('act', lambda e, LS=LS: e.activation(out=tt["step"][:], in_=LS, func=AF.Exp), reads=['prm'], writes=['t_step'])
            V(0, lambda e, LR=LR: e.tensor_tensor(out=tt["a"][:], in0=LR, in1=tt["step"][:], op=ALU.mult), ['prm', 't_step'], ['t_a'])
            V(0, lambda e, LI=LI: e.tensor_tensor(out=tt["ang"][:], in0=LI, in1=tt["step"][:], op=ALU.mult), ['prm', 't_step'], ['t_ang'])
            S.op('act', lambda e: e.activation(out=tt["mag"][:], in_=tt["a"][:], func=AF.Exp), reads=['t_a'], writes=['t_mag'])
            S.op('act', lambda e: e.activation(out=tt["s"][:], in_=tt["ang"][:], func=AF.Sin, scale=1.0 / 32), reads=['t_ang'], writes=['t_s'])
            S.op('act', lambda e: e.activation(out=tt["c"][:], in_=tt["ang"][:], func=AF.Sin, scale=1.0 / 32, bias=halfpi[:]), reads=['t_ang', 'halfpi'], writes=['t_c'])
            for it in range(5):
                V(0, lambda e: e.tensor_tensor(out=tt["t1"][:], in0=tt["c"][:], in1=tt["c"][:], op=ALU.mult), ['t_c'], ['t_t1'])
                V(0, lambda e: e.tensor_tensor(out=tt["t2"][:], in0=tt["s"][:], in1=tt["s"][:], op=ALU.mult), ['t_s'], ['t_t2'])
                V(0, lambda e: e.scalar_tensor_tensor(out=tt["s"][:], in0=tt["c"][:], scalar=2.0, in1=tt["s"][:], op0=ALU.mult, op1=ALU.mult), ['t_c', 't_s'], ['t_s'])
                V(0, lambda e: e.tensor_tensor(out=tt["c"][:], in0=tt["t1"][:], in1=tt["t2"][:], op=ALU.subtract), ['t_t1', 't_t2', 't_s'], ['t_c'])
            V(0, lambda e: e.tensor_tensor(out=tt["abr"][:], in0=tt["mag"][:], in1=tt["c"][:], op=ALU.mult), ['t_mag', 't_c'], ['t_abr'])
            V(0, lambda e: e.tensor_tensor(out=tt["abi"][:], in0=tt["mag"][:], in1=tt["s"][:], op=ALU.mult), ['t_mag', 't_s'], ['t_abi'])
            V(0, lambda e, LR=LR: e.tensor_tensor(out=tt["t1"][:], in0=LR, in1=LR, op=ALU.mult), ['prm', 't_t1'], ['t_t1'])
            V(0, lambda e, LI=LI: e.tensor_tensor(out=tt["t2"][:], in0=LI, in1=LI, op=ALU.mult), ['prm', 't_t2'], ['t_t2'])
            V(0, lambda e: e.tensor_tensor(out=tt["den"][:], in0=tt["t1"][:], in1=tt["t2"][:], op=ALU.add), ['t_t1', 't_t2'], ['t_den'])
            V(0, lambda e: e.reciprocal(out=tt["den"][:], in_=tt["den"][:]), ['t_den'], ['t_den'])
            V(0, lambda e: e.tensor_scalar(out=tt["nr"][:], in0=tt["abr"][:], scalar1=-1.0, scalar2=None, op0=ALU.add), ['t_abr'], ['t_nr'])
            V(0, lambda e, LR=LR: e.tensor_tensor(out=tt["u1"][:], in0=tt["nr"][:], in1=LR, op=ALU.mult), ['t_nr', 'prm'], ['t_u1'])
            V(0, lambda e, LI=LI: e.tensor_tensor(out=tt["u2"][:], in0=tt["abi"][:], in1=LI, op=ALU.mult), ['t_abi', 'prm'], ['t_u2'])
            V(0, lambda e: e.tensor_tensor(out=tt["cr"][:], in0=tt["u1"][:], in1=tt["u2"][:], op=ALU.add), ['t_u1', 't_u2'], ['t_cr'])
            V(0, lambda e: e.tensor_tensor(out=tt["cr"][:], in0=tt["cr"][:], in1=tt["den"][:], op=ALU.mult), ['t_cr', 't_den'], ['t_cr'])
            V(0, lambda e, LR=LR: e.tensor_tensor(out=tt["u1"][:], in0=tt["abi"][:], in1=LR, op=ALU.mult), ['t_abi', 'prm', 't_u1'], ['t_u1'])
            V(0, lambda e, LI=LI: e.tensor_tensor(out=tt["u2"][:], in0=tt["nr"][:], in1=LI, op=ALU.mult), ['t_nr', 'prm', 't_u2'], ['t_u2'])
            V(0, lambda e: e.tensor_tensor(out=tt["ci"][:], in0=tt["u1"][:], in1=tt["u2"][:], op=ALU.subtract), ['t_u1', 't_u2'], ['t_ci'])
            V(0, lambda e: e.tensor_tensor(out=tt["ci"][:], in0=tt["ci"][:], in1=tt["den"][:], op=ALU.mult), ['t_ci', 't_den'], ['t_ci'])
            bc = lambda ap: ap.unsqueeze(2).broadcast_to([128, 12, 16])
            V(0, lambda e: e.tensor_tensor(out=tb1[:], in0=BRE, in1=bc(tt["cr"][:]), op=ALU.mult), ['prm', 't_cr'], ['tb1'])
            V(0, lambda e: e.tensor_tensor(out=tb2[:], in0=BIM, in1=bc(tt["ci"][:]), op=ALU.mult), ['prm', 't_ci'], ['tb2'])
            V(0, lambda e, d=d: e.tensor_tensor(out=Bb[d][0][:], in0=tb1[:], in1=tb2[:], op=ALU.subtract), ['tb1', 'tb2'], [('Bb', d)])
            V(0, lambda e: e.tensor_tensor(out=tb1[:], in0=BIM, in1=bc(tt["cr"][:]), op=ALU.mult), ['prm', 't_cr', 'tb1'], ['tb1'])
            V(0, lambda e: e.tensor_tensor(out=tb2[:], in0=BRE, in1=bc(tt["ci"][:]), op=ALU.mult), ['prm', 't_ci', 'tb2'], ['tb2'])
            V(0, lambda e, d=d: e.tensor_tensor(out=Bb[d][1][:], in0=tb1[:], in1=tb2[:], op=ALU.add), ['tb1', 'tb2', ('Bb', d)], [('Bb', d)])
            V(0, lambda e, d=d: e.tensor_copy(out=PR[d][:, :, 0], in_=tt["abr"][:]), ['t_abr'], [('PW', d)])
            V(0, lambda e, d=d: e.tensor_copy(out=PI[d][:, :, 0], in_=tt["abi"][:]), ['t_abi', ('PW', d)], [('PW', d)])
            for lev in range(11):
                V(0, lambda e, d=d, lev=lev: e.tensor_tensor(out=tt["t1"][:], in0=PR[d][:, :, lev], in1=PR[d][:, :, lev], op=ALU.mult), [('PW', d), 't_t1'], ['t_t1'])
                V(0, lambda e, d=d, lev=lev: e.tensor_tensor(out=tt["t2"][:], in0=PI[d][:, :, lev], in1=PI[d][:, :, lev], op=ALU.mult), [('PW', d), 't_t2'], ['t_t2'])
                V(0, lambda e, d=d, lev=lev: e.tensor_tensor(out=PR[d][:, :, lev + 1], in0=tt["t1"][:], in1=tt["t2"][:], op=ALU.subtract), ['t_t1', 't_t2', ('PW', d)], [('PW', d)])
                V(0, lambda e, d=d, lev=lev: e.scalar_tensor_tensor(out=PI[d][:, :, lev + 1], in0=PR[d][:, :, lev], scalar=2.0, in1=PI[d][:, :, lev], op0=ALU.mult, op1=ALU.mult), [('PW', d)], [('PW', d)])
            V(0, lambda e, d=d: e.tensor_scalar(out=NPI[d][:], in0=PI[d][:], scalar1=-1.0, scalar2=None, op0=ALU.mult), [('PW', d)], [('PW', d)])
        for ct in range(3):
            S.op('pool', lambda e, ct=ct: e.tensor_scalar(out=yS[:, ct, :], in0=uF[:, ct, :], scalar1=prm[:, 840 + ct:841 + ct], scalar2=None, op0=ALU.mult), reads=['uF', 'prm'], writes=['yS'])
        Wd_ = sb("Wide", (128, 128)); Bpad = [sb(f"Bpad{c}", (128, 128)) for c in range(2)]
        CW = [sb(f"CW{c}", (128, 128)) for c in range(2)]
        HA = [[sb(f"HA{d}{c}", (128, T)) for c in range(2)] for d in range(2)]
        HB = [[sb(f"HB{d}{c}", (128, T)) for c in range(2)] for d in range(2)]
        colmap = lambda d, n0: n0 if d == 0 else (n0 - TC if n0 >= TC else TL + n0)
        for j in range(NJ):
            ct = j // 4; cb = 32 * (j % 4)
            for c, CC in enumerate((CRE, CIM)):
                S.op('dve', lambda e, c=c: e.memset(CW[c][:], 0.0), writes=[('CW', c)])
                for gl in range(2):
                    S.op('dve', lambda e, c=c, gl=gl, CC=CC, j=j, cb=cb: e.tensor_scalar(out=CW[c][:, cb + 16 * gl:cb + 16 * gl + 16], in0=CC[:, j, :], scalar1=gm[gl], scalar2=(1.0 if c == 0 else -1.0), op0=ALU.mult, op1=ALU.mult),
                         reads=['prm', 'bmask', ('CW', c)], writes=[('CW', c)])
            for d in range(2):
                eng = 'dve'
                for c in range(2):
                    S.op('dve', lambda e: e.memset(Wd_[:], 0.0), writes=['Wide'])
                    for gl in range(2):
                        S.op('dve', lambda e, c=c, gl=gl, d=d, j=j, cb=cb: e.tensor_scalar(out=Wd_[:, cb + 16 * gl:cb + 16 * gl + 16], in0=Bb[d][c][:, j, :], scalar1=gm[gl], scalar2=None, op0=ALU.mult),
                             reads=[('Bb', d), 'bmask', 'Wide'], writes=['Wide'])
                    S.op('pe', lambda e: e.transpose(out=psf[:, 5, 0:128], in_=Wd_[:], identity=identf[:]), reads=['Wide', 'identf'], writes=[('psf', 5)])
                    S.op('act', lambda e, c=c: e.activation(out=Bpad[c][:], in_=psf[:, 5, 0:128], func=AF.Copy), reads=[('psf', 5)], writes=[('Bpad', c)])
                for ci_, (n0, nw) in enumerate(TOKCH):
                    col = colmap(d, n0)
                    for c in range(2):
                        pb = (2 * ci_ + c) % 4
                        S.op('pe', lambda e, c=c, pb=pb, n0=n0, nw=nw, ct=ct: e.matmul(psf[:, pb, 0:nw], lhsT=Bpad[c][:], rhs=uF[:, ct, n0:n0 + nw], start=True, stop=True), reads=[('Bpad', c), 'uF'], writes=[('psf', pb)])
                        S.op('act', lambda e, c=c, pb=pb, d=d, col=col, nw=nw: e.activation(out=HA[d][c][:, col:col + nw], in_=psf[:, pb, 0:nw], func=AF.Copy), reads=[('psf', pb)], writes=[('H', d, c, 0)])
            state = {d: [HA[d], HB[d], 0, 1] for d in range(2)}
            for lev in range(12):
                sh = 1 << lev
                for part in range(2):
                    for d in range(2):
                        src, dst, sk, dk = state[d]
                        pr = PR[d][:, j, lev:lev + 1]; pi_ = PI[d][:, j, lev:lev + 1]; npi = NPI[d][:, j, lev:lev + 1]
                        if d == 0:
                            o = slice(sh, T); i_ = slice(0, T - sh); pre = slice(0, sh)
                        else:
                            o = slice(0, T - sh); i_ = slice(sh, T); pre = slice(T - sh, T)
                        rk = [('H', d, 0, sk), ('H', d, 1, sk), ('PW', d)]
                        if part == 0:
                            S.op(eng, lambda e, src=src, dst=dst, o=o, i_=i_, pr=pr: e.scalar_tensor_tensor(out=dst[0][:, o], in0=src[0][:, i_], scalar=pr, in1=src[0][:, o], op0=ALU.mult, op1=ALU.add), reads=rk, writes=[('H', d, 0, dk)])
                            S.op(eng, lambda e, src=src, dst=dst, o=o, i_=i_, pr=pr: e.scalar_tensor_tensor(out=dst[1][:, o], in0=src[1][:, i_], scalar=pr, in1=src[1][:, o], op0=ALU.mult, op1=ALU.add), reads=rk, writes=[('H', d, 1, dk)])
                        else:
                            S.op(eng, lambda e, src=src, dst=dst, o=o, i_=i_, npi=npi: e.scalar_tensor_tensor(out=dst[0][:, o], in0=src[1][:, i_], scalar=npi, in1=dst[0][:, o], op0=ALU.mult, op1=ALU.add), reads=rk + [('H', d, 0, dk)], writes=[('H', d, 0, dk)])
                            S.op(eng, lambda e, src=src, dst=dst, o=o, i_=i_, pi_=pi_: e.scalar_tensor_tensor(out=dst[1][:, o], in0=src[0][:, i_], scalar=pi_, in1=dst[1][:, o], op0=ALU.mult, op1=ALU.add), reads=rk + [('H', d, 1, dk)], writes=[('H', d, 1, dk)])
                            for c in range(2):
                                S.op('act', lambda e, src=src, dst=dst, c=c, pre=pre: e.activation(out=dst[c][:, pre], in_=src[c][:, pre], func=AF.Copy), reads=[('H', d, c, sk), ('H', d, c, dk)], writes=[('H', d, c, dk)])
                for d in range(2):
                    src, dst, sk, dk = state[d]
                    state[d] = [dst, src, dk, sk]
            for ci_, (n0, nw) in enumerate(TOKCH):
                pb = ci_ % 2

                def mmr(e, pb=pb, n0=n0, nw=nw):
                    k = 0
                    for d in range(2):
                        col = colmap(d, n0)
                        for c in range(2):
                            ins = e.matmul(psf[:, pb, 0:nw], lhsT=CW[c][:], rhs=HA[d][c][:, col:col + nw], start=(k == 0), stop=(k == 3))
                            k += 1
                    return ins
                S.op('pe', mmr, reads=[('CW', 0), ('CW', 1)] + [('H', d, c, 0) for d in range(2) for c in range(2)], writes=[('psf', pb)])
                S.op('dve', lambda e, pb=pb, n0=n0, nw=nw, ct=ct: e.tensor_tensor(out=yS[:, ct, n0:n0 + nw], in0=yS[:, ct, n0:n0 + nw], in1=psf[:, pb, 0:nw], op=ALU.add), reads=[('psf', pb), 'yS'], writes=['yS'])
        x2 = HA[0][0]; inner = HA[0][1]; sgm = HB[0][0]
        yg = sb("yg", (128, 3, T), BF16); ygf = HB[0][1]
        wglu = sb("wglu", (128, 3, 384), BF16)
        S.dma('pool', wglu[:], dram(K, f's5_w_glu_{layer}').rearrange("(k p) n -> p k n", p=128), writes=['wglu'])
        for ct in range(3):
            y = yS[:, ct, :]
            S.op('dve', lambda e, y=y: e.tensor_tensor(out=x2[:], in0=y, in1=y, op=ALU.mult), reads=['yS', ('H', 0, 0, 0)], writes=[('H', 0, 0, 0)])
            S.op('dve', lambda e: e.tensor_scalar(out=x2[:], in0=x2[:], scalar1=0.044715, scalar2=1.0, op0=ALU.mult, op1=ALU.add), reads=[('H', 0, 0, 0)], writes=[('H', 0, 0, 0)])
            S.op('dve', lambda e, y=y: e.tensor_tensor(out=inner[:], in0=x2[:], in1=y, op=ALU.mult), reads=[('H', 0, 0, 0), 'yS', ('H', 0, 1, 0)], writes=[('H', 0, 1, 0)])
            S.op('act', lambda e: e.activation(out=sgm[:], in_=inner[:], func=AF.Sigmoid, scale=1.5957691216057308), reads=[('H', 0, 1, 0), ('H', 0, 0, 1)], writes=[('H', 0, 0, 1)])
            S.op('dve', lambda e, y=y, ct=ct: e.tensor_tensor(out=yS[:, ct, :], in0=y, in1=sgm[:], op=ALU.mult), reads=['yS', ('H', 0, 0, 1)], writes=['yS'])
            S.op('act', lambda e, ct=ct: e.activation(out=yg[:, ct, :], in_=yS[:, ct, :], func=AF.Copy), reads=['yS'], writes=['yg'])
        for m in range(3):
            for ci_, (n0, nw) in enumerate(TOKCH):
                pb = ci_ % 2

                def mmg(e, m=m, pb=pb, n0=n0, nw=nw):
                    for k in range(3):
                        ins = e.matmul(psf[:, pb, 0:nw], lhsT=wglu[:, k, m * 128:(m + 1) * 128], rhs=yg[:, k, n0:n0 + nw], start=(k == 0), stop=(k == 2))
                    return ins
                S.op('pe', mmg, reads=['wglu', 'yg'], writes=[('psf', pb)])
                S.op('act', lambda e, m=m, pb=pb, n0=n0, nw=nw: e.activation(out=ygf[:, n0:n0 + nw], in_=psf[:, pb, 0:nw], func=AF.Sigmoid, bias=prm[:, 843 + m:844 + m]), reads=[('psf', pb), 'prm', ('H', 0, 1, 1)], writes=[('H', 0, 1, 1)])
            S.op('dve', lambda e, m=m: e.tensor_tensor(out=HB[1][0][:], in0=yS[:, m, :], in1=ygf[:], op=ALU.mult), reads=['yS', ('H', 0, 1, 1), ('H', 1, 0, 1)], writes=[('H', 1, 0, 1)])
            S.op('act', lambda e, m=m: e.activation(out=HA[1][0][:].bitcast(BF16)[:, 0:T], in_=HB[1][0][:], func=AF.Copy), reads=[('H', 1, 0, 1), ('H', 1, 0, 0)], writes=[('H', 1, 0, 0)])
            S.dma('sp', yT_d[8 + m], HA[1][0][:].bitcast(BF16)[:, 0:T], reads=[('H', 1, 0, 0)], writes=[('yT_d', 8 + m)], sem='yTd')
        S.barrier()


def setup_common(K):
    nc, S, es = K.nc, K.S, K.es
    sb = lambda name, shape, dt=F32: es.enter_context(nc.sbuf_tensor(name, list(shape), dt))
    K.identf = sb("identf", (128, 128)); K.identb = sb("identb", (128, 128), BF16)
    K.ones_f = sb("ones_f", (128, 128)); K.eps_t = sb("eps_t", (128, 1))
    K.psf = es.enter_context(nc.psum_tensor("psf", [128, 6, 512], F32))
    K.psb = es.enter_context(nc.psum_tensor("psb", [128, 2, 1024], BF16))
    S.dma('sp', K.identf[:], dram(K, 'ident', (128, 128))[:, :], writes=['identf'])
    S.op('dve', lambda e: e.tensor_copy(out=K.identb[:], in_=K.identf[:]), reads=['identf'], writes=['identb'])
    S.op('dve', lambda e: e.memset(K.ones_f[:], 1.0), writes=['ones_f'])
    S.op('dve', lambda e: e.memset(K.eps_t[:], 1e-6), writes=['eps'])


PHASES = {}


def build_program(plan, kinds=None, dbg=False):
    nc = bass.Bass("TRN2", target_bir_lowering=False)
    K = Ctx()
    K.nc = nc; K.di = {}; K.kinds = kinds or {}; K.declared = {}; K.dbg = dbg
    with ExitStack() as es:
        K.es = es
        K.S = Sched(nc, es)
        setup_common(K)
        for (ph, layer) in plan:
            if ph == 'init':
                phase_init(K)
            else:
                PHASES[ph](K, layer)
        K.S.barrier()
        print("n_ops", K.S.n, flush=True)
    return nc, K


PHASES.update({'p0': phase_p0, 'p1': phase_p1, 'attn': phase_attn, 'ssd': phase_ssd, 'sc': phase_sc, 'merge': phase_merge, 'moe': phase_moe, 'final': phase_final, 's5': phase_s5})


def make_consts():
    half = 32
    inv = (np.float32(10000.0) ** (-np.arange(0, half, 2, dtype=np.float32) / np.float32(half))).astype(np.float32)
    tt = np.arange(TL)
    row = (tt // 64).astype(np.float32); col = (tt % 64).astype(np.float32)
    ar = (row[:, None] * inv).astype(np.float32); ac = (col[:, None] * inv).astype(np.float32)
    C = np.ones((T, 2, 2, 16), np.float32); Sg = np.zeros((T, 2, 2, 16), np.float32)
    for b, a in enumerate((ar, ac)):
        C[TC:, b, 0] = np.cos(a); C[TC:, b, 1] = np.cos(a)
        Sg[TC:, b, 0] = -np.sin(a); Sg[TC:, b, 1] = np.sin(a)
    ii = np.arange(128)
    triF = (ii[:, None] <= ii[None, :]).astype(np.float32); triB = (ii[:, None] >= ii[None, :]).astype(np.float32)
    bmask = np.zeros((128, 512), np.float32); bmask[0:64, 0:256] = 1.0; bmask[64:128, 256:512] = 1.0
    iota = np.tile(np.arange(256, dtype=np.float32)[None, :], (128, 1))
    return {"iota": iota, "triS": (triF - np.eye(128, dtype=np.float32)), "bmask": bmask, "ident": np.eye(128, dtype=np.float32), "ropeC": C.reshape(T, 64), "ropeS": Sg.reshape(T, 64),
            "triF": triF, "triB": triB, "negF": (triF - 1.0) * 30000.0, "negBm": (triB - 1.0) * 30000.0}


def make_in_maps(K, inputs, ncores, extra=None):
    consts = make_consts()
    maps = []
    for b in range(ncores):
        m = {}
        for name, kind in K.declared.items():
            if kind != "ExternalInput":
                continue
            if extra is not None and name in extra:
                m[name] = extra[name]
            elif name == 'x':
                m[name] = np.ascontiguousarray(inputs['x'][b])
            elif name == 'ctx':
                m[name] = np.ascontiguousarray(inputs['ctx'][b])
            elif name == 'cc':
                m[name] = np.ascontiguousarray(np.stack([inputs['c'][b], inputs['c_ctx']], 0))
            elif name in consts:
                m[name] = consts[name]
            elif name.startswith('s5p_'):
                l = int(name.rsplit('_', 1)[1])
                st = lambda a: np.ascontiguousarray(a.reshape(12, 2, 64).transpose(1, 2, 0).reshape(128, 12))
                cols = [st(inputs['s5_lambda_re'][l][d]) for d in range(2)] + [st(inputs['s5_lambda_im'][l][d]) for d in range(2)]
                cols += [np.repeat(inputs['s5_log_step'][l][d].reshape(12, 2).T[:, None, :], 64, axis=1).reshape(128, 12) for d in range(2)]
                for nm in ('s5_b_re', 's5_b_im'):
                    cols.append(inputs[nm][l].reshape(12, 2, 64, 16).transpose(1, 2, 0, 3).reshape(128, 192))
                for nm in ('s5_c_re', 's5_c_im'):
                    cols.append(inputs[nm][l].reshape(12, 2, 16, 64).transpose(1, 3, 0, 2).reshape(128, 192))
                cols.append(inputs['s5_d'][l].reshape(3, 128).T); cols.append(inputs['s5_b_glu'][l].reshape(3, 128).T)
                m[name] = np.ascontiguousarray(np.concatenate(cols, 1).astype(np.float32))
            elif name.startswith('ssdp_'):
                l = int(name.rsplit('_', 1)[1])
                m[name] = np.ascontiguousarray(np.concatenate([inputs['ssd_dt_bias'][l].ravel(), inputs['ssd_a_log'][l].ravel(), inputs['ssd_d'][l].ravel()])[None, :])
            elif name == 'final_norm_g':
                m[name] = np.ascontiguousarray(inputs[name])
            else:
                base, lay = name.rsplit('_', 1)
                m[name] = np.ascontiguousarray(inputs[base][int(lay)])
        maps.append(m)
    return maps


def full_plan():
    plan = [('init', 0)]
    for layer in range(DEPTH):
        plan += [(ph, layer) for ph in ('p0', 'p1', 'attn', 'ssd', 's5', 'sc', 'merge', 'moe')]
    plan.append(('final', 0))
    return plan


def kernel(**inputs):
    nc, K = build_program(full_plan(), kinds={'out': 'ExternalOutput', 'x_d': 'ExternalOutput'})
    in_maps = make_in_maps(K, inputs, 8)
    res = run_bass_kernel_spmd(nc, in_maps, core_ids=list(range(8)))
    return np.stack([r["out"] for r in res.results], 0)
```

```python
import os
import numpy as np
from contextlib import ExitStack
import ml_dtypes
import concourse.bass as bass
import concourse.mybir as mybir
from concourse.bass_utils import run_bass_kernel_spmd

F32 = mybir.dt.float32
BF16 = mybir.dt.bfloat16
AF = mybir.ActivationFunctionType
ALU = mybir.AluOpType
AX = mybir.AxisListType

D = 1024
TL = 2048
TC = 256
T = TL + TC
NT = T // 128
N_IN = 7688
DEPTH = 2
NE = 16
SAME_ENG_SYNC = os.environ.get("KSES", "1") == "1"
TOKCH = [(0, 256), (256, 512), (768, 512), (1280, 512), (1792, 512)]

PARAM_SHAPES = {
    'w_mod': (D, 6 * D), 'b_mod': (6 * D,), 'norm_mix_g': (D,), 'norm_ffn_g': (D,), 'w_in': (D, N_IN),
    'q_norm_g': (64,), 'k_norm_g': (64,), 'ssd_conv_w': (3, 768), 'ssd_conv_b': (768,), 'ssd_dt_bias': (2, 8),
    'ssd_a_log': (2, 8), 'ssd_d': (8,), 'ssd_norm_g': (512,), 's5_lambda_re': (2, 24, 64), 's5_lambda_im': (2, 24, 64),
    's5_log_step': (2, 24), 's5_b_re': (24, 64, 16), 's5_b_im': (24, 64, 16), 's5_c_re': (24, 16, 64), 's5_c_im': (24, 16, 64),
    's5_d': (384,), 's5_w_glu': (384, 384), 's5_b_glu': (384,), 'sc_conv_w': (3, 384), 'w_br_attn': (512, D), 'w_br_ssd': (512, D),
    'w_br_s5': (384, D), 'w_br_sc': (384, D), 'w_out': (D, D), 'w_router': (D, NE), 'w_exp_gate': (NE, D, D), 'w_exp_up': (NE, D, D),
    'w_exp_down': (NE, D, D),
}
SCRATCH = {
    'x_d': ((T, D), F32), 'modv_d': ((128, 128), F32), 'modrow_d': ((2, 48, 128), F32),
    'projT_d': ((T, 1288), F32), 'projF_d': ((18, 128, T), F32), 'hT_d': ((8, 128, T), BF16), 'yT_d': ((14, 128, T), BF16),
}


class Sched:
    def __init__(self, nc, es):
        self.nc = nc
        self.eng = {'pe': nc.tensor, 'act': nc.scalar, 'dve': nc.vector, 'pool': nc.gpsimd, 'sp': nc.sync}
        self.sem = {}
        self.cnt = {}
        for e in self.eng:
            self.sem[e] = es.enter_context(nc.semaphore("s_" + e))
            self.cnt[e] = 0
        self.es = es
        self.waited = {e: {} for e in self.eng}
        self.last_w = {}
        self.readers = {}
        self.n = 0
        self.limit = int(os.environ.get("KLIMIT", "1000000000"))

    def dsem(self, name):
        k = ('dma', name)
        if k not in self.sem:
            self.sem[k] = self.es.enter_context(self.nc.semaphore("d_" + name))
            self.cnt[k] = 0
        return k

    def _deps(self, reads, writes):
        deps = {}

        def add(d):
            if d is None:
                return
            k, v = d
            if deps.get(k, 0) < v:
                deps[k] = v
        for r in reads:
            add(self.last_w.get(r))
            if isinstance(r, tuple) and r[0] in ('psf', 'psb', 'psfO'):
                for d in self.readers.get(r, ()):
                    add(d)
        for w in writes:
            add(self.last_w.get(w))
            for d in self.readers.get(w, ()):
                add(d)
        return deps

    def _wait(self, e, deps):
        for k, v in deps.items():
            if k == e and (e == 'pe' or not SAME_ENG_SYNC):
                continue
            if self.waited[e].get(k, 0) >= v:
                continue
            self.eng[e].wait_ge(self.sem[k], v)
            self.waited[e][k] = v

    def _record(self, tag, reads, writes):
        for w in writes:
            self.last_w[w] = tag
            self.readers[w] = []
        for r in reads:
            if r in writes:
                continue
            self.readers.setdefault(r, []).append(tag)

    def op(self, e, fn, reads=(), writes=()):
        self.n += 1
        if self.n > self.limit:
            return None
        reads = list(reads); writes = list(writes)
        self._wait(e, self._deps(reads, writes))
        ins = fn(self.eng[e])
        self.cnt[e] += 1
        ins.then_inc(self.sem[e], 1)
        self._record((e, self.cnt[e]), reads, writes)
        return ins

    def dma(self, q, out, in_, reads=(), writes=(), sem='g', force=False, **kw):
        self.n += 1
        if self.n > self.limit and not force:
            return None
        reads = list(reads); writes = list(writes)
        k = self.dsem(sem)
        self._wait(q, self._deps(reads, writes))
        ins = self.eng[q].dma_start(out=out, in_=in_, **kw)
        self.cnt[k] += 16
        ins.then_inc(self.sem[k], 16)
        self._record((k, self.cnt[k]), reads, writes)
        return ins

    def barrier(self):
        snap = dict(self.cnt)
        for e in self.eng:
            for k, v in snap.items():
                if v > 0 and k != e and self.waited[e].get(k, 0) < v:
                    self.eng[e].wait_ge(self.sem[k], v)
                    self.waited[e][k] = v


class Ctx:
    pass


def dram(K, name, shape=None, dt=F32):
    if name not in K.di:
        if shape is None:
            if name in SCRATCH:
                shape, dt = SCRATCH[name]
            else:
                base, lay = name.rsplit('_', 1)
                shape = PARAM_SHAPES[base]
        kind = K.kinds.get(name, "Internal" if name in SCRATCH else "ExternalInput")
        K.di[name] = K.nc.dram_tensor(name, list(shape), dt, kind=kind).ap()
        K.declared[name] = kind
    return K.di[name]


def dump(K, name, ap, reads, dt=F32):
    shape = list(ap.shape)
    t = K.nc.dram_tensor("dbg_" + name, shape, dt, kind="ExternalOutput").ap()
    K.S.dma('sp', t, ap, reads=reads, writes=['dbgout'], sem='dbg', force=True)


def phase_init(K):
    S, nc = K.S, K.nc
    x_d = dram(K, 'x_d'); xin = dram(K, 'x', (TL, D)); cin = dram(K, 'ctx', (TC, D))
    with ExitStack() as pes:
        bufs = [pes.enter_context(nc.sbuf_tensor(f"init_b{j}", [128, D], F32)) for j in range(2)]
        for t in range(NT):
            j = t % 2
            src = cin[t * 128:(t + 1) * 128, :] if t < 2 else xin[(t - 2) * 128:(t - 1) * 128, :]
            S.dma('sp', bufs[j][:], src, writes=[('ib', j)], sem=f'ib{j}')
            S.dma('sp', x_d[t * 128:(t + 1) * 128, :], bufs[j][:], reads=[('ib', j)], writes=[('x_d', t)], sem=f'ib{j}')
        S.barrier()


def phase_p0(K, layer):
    S, nc, psf = K.S, K.nc, K.psf
    identf = K.identf
    modv_d = dram(K, 'modv_d'); modrow_d = dram(K, 'modrow_d')
    with ExitStack() as pes:
        sb = lambda name, shape, dt=F32: pes.enter_context(nc.sbuf_tensor(f"L{layer}_p0_{name}", list(shape), dt))
        modv = sb("modv", (128, 128))
        modFM = modv[:, 0:96].rearrange("p (w j) -> p w j", w=2)
        G1 = modv[:, 96:112].rearrange("p (w f) -> p w f", w=2); G2 = modv[:, 112:128].rearrange("p (w f) -> p w f", w=2)
        vecs = sb("vecs", (128, 64))
        cct = sb("cct", (2, 1024)); scs = sb("scs", (2, 1024)); scT = sb("scT", (128, 8, 2))
        vstage = sb("vstage", (64, 128))
        wm = [sb(f"wm{j}", (128, 8, 512)) for j in range(2)]
        mrow = sb("mrow", (48, 2, 128))
        S.dma('sp', cct[:], dram(K, 'cc', (2, D))[:, :], writes=['cct'])
        S.dma('sp', vstage[0:48, :], dram(K, f'b_mod_{layer}').rearrange("(j p) -> j p", p=128), writes=['vstage'])
        S.dma('sp', vstage[48:56, :], dram(K, f'norm_mix_g_{layer}').rearrange("(j p) -> j p", p=128), writes=['vstage'])
        S.dma('sp', vstage[56:64, :], dram(K, f'norm_ffn_g_{layer}').rearrange("(j p) -> j p", p=128), writes=['vstage'])
        S.op('act', lambda e: e.activation(out=scs[:], in_=cct[:], func=AF.Silu), reads=['cct'], writes=['scs'])

        def tr_sc(e):
            for k in range(8):
                ins = e.transpose(out=psf[:, 0, k * 2:(k + 1) * 2], in_=scs[:, k * 128:(k + 1) * 128], identity=identf[0:2, 0:2])
            return ins
        S.op('pe', tr_sc, reads=['scs', 'identf'], writes=[('psf', 0)])
        S.op('dve', lambda e: e.tensor_copy(out=scT[:].rearrange("p k w -> p (k w)"), in_=psf[:, 0, 0:16]), reads=[('psf', 0)], writes=['scT'])
        S.op('pe', lambda e: e.transpose(out=psf[:, 1, 0:64], in_=vstage[:, :], identity=identf[0:64, 0:64]), reads=['vstage', 'identf'], writes=[('psf', 1)])
        S.op('dve', lambda e: e.tensor_copy(out=vecs[:], in_=psf[:, 1, 0:64]), reads=[('psf', 1)], writes=['vecs'])
        wmv = dram(K, f'w_mod_{layer}').rearrange("(k p) n -> p k n", p=128)
        for ch in range(12):
            wt = wm[ch % 2]
            S.dma('sp', wt[:], wmv[:, :, ch * 512:(ch + 1) * 512], writes=[('wm', ch % 2)], sem=f'wm{ch % 2}')

            def mm(e, ch=ch, wt=wt):
                for jt in range(4):
                    j = ch * 4 + jt
                    for k in range(8):
                        ins = e.matmul(psf[:, 2, j * 2:(j + 1) * 2], lhsT=wt[:, k, jt * 128:(jt + 1) * 128], rhs=scT[:, k, :], start=(k == 0), stop=(k == 7))
                return ins
            S.op('pe', mm, reads=[('wm', ch % 2), 'scT'], writes=[('psf', 2)])
        for w in range(2):
            S.op('dve', lambda e, w=w: e.tensor_tensor(out=modFM[:, w, :], in0=psf[:, 2, 0:96].rearrange("p (j w) -> p j w", w=2)[:, :, w], in1=vecs[:, 0:48], op=ALU.add),
                 reads=[('psf', 2), 'vecs'], writes=['modv'])
        for w in range(2):
            S.op('dve', lambda e, w=w: e.scalar_tensor_tensor(out=G1[:, w, :], in0=modFM[:, w, 8:16], scalar=1.0, in1=vecs[:, 48:56], op0=ALU.add, op1=ALU.mult),
                 reads=['modv', 'vecs'], writes=['modv'])
            S.op('dve', lambda e, w=w: e.scalar_tensor_tensor(out=G2[:, w, :], in0=modFM[:, w, 32:40], scalar=1.0, in1=vecs[:, 56:64], op0=ALU.add, op1=ALU.mult),
                 reads=['modv', 'vecs'], writes=['modv'])
        S.dma('sp', modv_d[:, :], modv[:], reads=['modv'], writes=['modv_d'], sem='p0o')
        for w in range(2):
            S.op('pe', lambda e, w=w: e.transpose(out=psf[0:48, 3, w * 128:(w + 1) * 128], in_=modv[:, w * 48:(w + 1) * 48], identity=identf[:]), reads=['modv', 'identf'], writes=[('psf', 3)])
        S.op('dve', lambda e: e.tensor_copy(out=mrow[:].rearrange("j w p -> j (w p)"), in_=psf[0:48, 3, 0:256]), reads=[('psf', 3)], writes=['mrow'])
        for w in range(2):
            S.dma('sp', modrow_d[w], mrow[:, w, :], reads=['mrow'], writes=['modrow_d'], sem='p0o')
        S.barrier()


def load_modv(K, sb):
    modv = sb("modv", (128, 128))
    K.S.dma('sp', modv[:], dram(K, 'modv_d')[:, :], reads=['modv_d'], writes=['modv'])
    return modv


def phase_p1(K, layer):
    S, nc, psf, psb = K.S, K.nc, K.psf, K.psb
    identb, eps_t = K.identb, K.eps_t
    x_d = dram(K, 'x_d'); projT_d = dram(K, 'projT_d'); projF_d = dram(K, 'projF_d'); hT_d = dram(K, 'hT_d')
    with ExitStack() as pes:
        sb = lambda name, shape, dt=F32: pes.enter_context(nc.sbuf_tensor(f"L{layer}_p1_{name}", list(shape), dt))
        modv = load_modv(K, sb)
        modFM = modv[:, 0:96].rearrange("p (w j) -> p w j", w=2)
        G1 = modv[:, 96:112].rearrange("p (w f) -> p w f", w=2)
        hFM = sb("hFM", (128, 8, T), BF16)
        xt = [sb(f"xt{j}", (128, 1024)) for j in range(2)]
        sq = sb("sq", (128, 1024))
        xn = [sb(f"xn{j}", (128, 1024), BF16) for j in range(2)]
        ss = sb("ss", (128, 2)); rstd = sb("rstd", (128, 2))
        for t in range(NT):
            w = 1 if t < 2 else 0
            j = t % 2
            S.dma('sp', xt[j][:], x_d[t * 128:(t + 1) * 128, :], reads=[('x_d', t)], writes=[('xt', j)], sem=f'xt{j}')
            S.op('act', lambda e, j=j: e.activation(out=sq[:], in_=xt[j][:], func=AF.Square, accum_out=ss[:, j:j + 1]), reads=[('xt', j)], writes=['sq', ('ss', j)])
            S.op('act', lambda e, j=j: e.activation(out=rstd[:, j:j + 1], in_=ss[:, j:j + 1], func=AF.Sqrt, scale=1.0 / D, bias=eps_t[:]), reads=[('ss', j), 'eps'], writes=[('rstd', j)])
            S.op('dve', lambda e, j=j: e.reciprocal(out=rstd[:, j:j + 1], in_=rstd[:, j:j + 1]), reads=[('rstd', j)], writes=[('rstd', j)])
            S.op('dve', lambda e, j=j: e.tensor_scalar(out=xn[j][:], in0=xt[j][:], scalar1=rstd[:, j:j + 1], scalar2=None, op0=ALU.mult), reads=[('xt', j), ('rstd', j)], writes=[('xn', j)])

            def trx(e, j=j):
                for f in range(8):
                    ins = e.transpose(out=psb[:, j, f * 128:(f + 1) * 128], in_=xn[j][:, f * 128:(f + 1) * 128], identity=identb[:])
                return ins
            S.op('pe', trx, reads=[('xn', j), 'identb'], writes=[('psb', j)])
            for f in range(8):
                if f % 2 == 0:
                    S.op('act', lambda e, f=f, j=j, t=t, w=w: e.activation(out=hFM[:, f, t * 128:(t + 1) * 128], in_=psb[:, j, f * 128:(f + 1) * 128], func=AF.Identity,
                                                                          scale=G1[:, w, f:f + 1], bias=modFM[:, w, f:f + 1]),
                         reads=[('psb', j), 'modv'], writes=[('hFM', t)])
                else:
                    S.op('dve', lambda e, f=f, j=j, t=t, w=w: e.tensor_scalar(out=hFM[:, f, t * 128:(t + 1) * 128], in0=psb[:, j, f * 128:(f + 1) * 128],
                                                                             scalar1=G1[:, w, f:f + 1], scalar2=modFM[:, w, f:f + 1], op0=ALU.mult, op1=ALU.add),
                         reads=[('psb', j), 'modv'], writes=[('hFM', t)])
        for f in range(8):
            S.dma('sp', hT_d[f], hFM[:, f, :], reads=[('hFM', t) for t in range(NT)], writes=[('hT_d', f)], sem='spill')
        wv = dram(K, f'w_in_{layer}').rearrange("(k p) n -> p k n", p=128)
        wch = [sb(f"wch{j}", (128, 8, 512), BF16) for j in range(2)]
        stg = [sb(f"stg{j}", (128, 512)) for j in range(4)]
        wi = 0; si = 0; pbank = 0
        for (c0, wd, d0) in [(0, 512, 0), (512, 512, 512), (1024, 256, 1024), (2048, 8, 1280)]:
            wj = wi % 2; wi += 1
            S.dma('pool', wch[wj][:, :, 0:wd], wv[:, :, c0:c0 + wd], writes=[('wch', wj)], sem=f'wch{wj}')
            for t in range(NT):
                pb = pbank % 4; pbank += 1
                sj = si % 4; si += 1

                def mm(e, t=t, wj=wj, wd=wd, pb=pb):
                    for k in range(8):
                        ins = e.matmul(psf[:, pb, 0:wd], lhsT=hFM[:, k, t * 128:(t + 1) * 128], rhs=wch[wj][:, k, 0:wd], start=(k == 0), stop=(k == 7))
                    return ins
                S.op('pe', mm, reads=[('hFM', t), ('wch', wj)], writes=[('psf', pb)])
                if sj % 2 == 0:
                    S.op('act', lambda e, sj=sj, pb=pb, wd=wd: e.activation(out=stg[sj][:, 0:wd], in_=psf[:, pb, 0:wd], func=AF.Copy), reads=[('psf', pb)], writes=[('stg', sj)])
                else:
                    S.op('dve', lambda e, sj=sj, pb=pb, wd=wd: e.tensor_copy(out=stg[sj][:, 0:wd], in_=psf[:, pb, 0:wd]), reads=[('psf', pb)], writes=[('stg', sj)])
                S.dma('sp', projT_d[t * 128:(t + 1) * 128, d0:d0 + wd], stg[sj][:, 0:wd], reads=[('stg', sj)], writes=[('projT_d', t)], sem=f'st{sj}')
        srow = [sb(f"srow{j}", (128, T)) for j in range(2)]
        fm_tiles = [1280 + 128 * i for i in range(6)] + [2056 + 128 * i for i in range(3)] + [2440 + 128 * i for i in range(9)]
        tokch = [(0, 512), (512, 512), (1024, 512), (1536, 512), (2048, 256)]
        for grp in range(0, 18, 4):
            tiles = fm_tiles[grp:grp + 4]
            wj = wi % 2; wi += 1
            for ti, c0 in enumerate(tiles):
                S.dma('pool', wch[wj][:, :, ti * 128:(ti + 1) * 128], wv[:, :, c0:c0 + 128], writes=[('wch', wj)], sem=f'wch{wj}')
            for ti, c0 in enumerate(tiles):
                ft = grp + ti
                rj = ft % 2
                for (n0, nw) in tokch:
                    pb = pbank % 4; pbank += 1

                    def mm(e, ti=ti, wj=wj, n0=n0, nw=nw, pb=pb):
                        for k in range(8):
                            ins = e.matmul(psf[:, pb, 0:nw], lhsT=wch[wj][:, k, ti * 128:(ti + 1) * 128], rhs=hFM[:, k, n0:n0 + nw], start=(k == 0), stop=(k == 7))
                        return ins
                    S.op('pe', mm, reads=[('hFM', t) for t in range(n0 // 128, (n0 + nw) // 128)] + [('wch', wj)], writes=[('psf', pb)])
                    if pb % 2 == 0:
                        S.op('act', lambda e, rj=rj, pb=pb, n0=n0, nw=nw: e.activation(out=srow[rj][:, n0:n0 + nw], in_=psf[:, pb, 0:nw], func=AF.Copy), reads=[('psf', pb)], writes=[('srow', rj)])
                    else:
                        S.op('dve', lambda e, rj=rj, pb=pb, n0=n0, nw=nw: e.tensor_copy(out=srow[rj][:, n0:n0 + nw], in_=psf[:, pb, 0:nw]), reads=[('psf', pb)], writes=[('srow', rj)])
                S.dma('sp', projF_d[ft], srow[rj][:], reads=[('srow', rj)], writes=[('projF_d', ft)], sem=f'sr{rj}')
        S.barrier()


def phase_attn(K, layer):
    S, nc, psf, psb = K.S, K.nc, K.psf, K.psb
    identb, eps_t = K.identb, K.eps_t
    projT_d = dram(K, 'projT_d'); yT_d = dram(K, 'yT_d')
    with ExitStack() as pes:
        sb = lambda name, shape, dt=F32: pes.enter_context(nc.sbuf_tensor(f"L{layer}_p2_{name}", list(shape), dt))
        qT = sb("qT", (128, 4, T), BF16)
        kTp = [sb(f"kTp{g}", (128, T), BF16) for g in range(2)]
        Vp = sb("Vp", (128, NT, 2, 66), BF16)
        ropeC = sb("ropeC", (128, NT, 64)); ropeS = sb("ropeS", (128, NT, 64))
        gfull = sb("gfull", (128, 10, 64))
        negB = sb("negB", (128, 1))
        qkv = [sb(f"qkv{j}", (128, 768)) for j in range(2)]
        sq2 = sb("sq2", (128, 640)); ssq = sb("ssq", (128, 10)); rs = sb("rs", (128, 10))
        qn = sb("qn", (128, 640)); t1 = sb("t1", (128, 640)); t2 = sb("t2", (128, 640))
        qr = sb("qr", (128, 512), BF16)
        kz = [sb(f"kz{g}", (128, 128), BF16) for g in range(2)]
        PT = [sb(f"PT{j}", (128, 512), BF16) for j in range(2)]
        ya = sb("ya", (128, 4, 512), BF16)
        rc = sb("rc", (128, 4))
        yaTs = sb("yaTs", (128, 4, 512), BF16)
        S.dma('sp', ropeC[:], dram(K, 'ropeC', (T, 64)).rearrange("(t p) d -> p t d", p=128), writes=['ropeC'])
        S.dma('sp', ropeS[:], dram(K, 'ropeS', (T, 64)).rearrange("(t p) d -> p t d", p=128), writes=['ropeS'])
        qg = dram(K, f'q_norm_g_{layer}').rearrange("(o d) -> o d", o=1); kg = dram(K, f'k_norm_g_{layer}').rearrange("(o d) -> o d", o=1)
        for h in range(10):
            S.dma('sp', gfull[:, h, :], (qg if h < 8 else kg).partition_broadcast(128), writes=['gfull'])
        S.op('dve', lambda e: e.memset(negB[:], -12.0), writes=['negB'])
        S.op('dve', lambda e: e.memset(kz[0][:], 0.0), writes=['kz'])
        S.op('dve', lambda e: e.memset(kz[1][:], 0.0), writes=['kz'])
        S.op('dve', lambda e: e.memset(Vp[:], 1.0), writes=['Vp'])
        v3 = lambda ap: ap.rearrange("p (h d) -> p h d", d=64)
        for t in range(NT):
            j = t % 2
            S.dma('sp', qkv[j][:], projT_d[t * 128:(t + 1) * 128, 0:768], reads=[('projT_d', t)], writes=[('qkv', j)], sem=f'qkv{j}')
            S.op('dve', lambda e, j=j: e.tensor_tensor(out=sq2[:], in0=qkv[j][:, 0:640], in1=qkv[j][:, 0:640], op=ALU.mult), reads=[('qkv', j)], writes=['sq2'])
            S.op('dve', lambda e: e.tensor_reduce(out=ssq[:], in_=v3(sq2[:]), axis=AX.X, op=ALU.add), reads=['sq2'], writes=['ssq'])
            S.op('act', lambda e: e.activation(out=rs[:], in_=ssq[:], func=AF.Sqrt, scale=1.0 / 64, bias=eps_t[:]), reads=['ssq', 'eps'], writes=['rs'])
            S.op('dve', lambda e: e.reciprocal(out=rs[:], in_=rs[:]), reads=['rs'], writes=['rs'])
            S.op('dve', lambda e, j=j: e.tensor_tensor(out=v3(qn[:]), in0=v3(qkv[j][:, 0:640]), in1=rs[:].unsqueeze(2).broadcast_to([128, 10, 64]), op=ALU.mult), reads=[('qkv', j), 'rs'], writes=['qn'])
            S.op('dve', lambda e: e.tensor_tensor(out=qn[:], in0=qn[:], in1=gfull[:].rearrange("p h d -> p (h d)"), op=ALU.mult), reads=['qn', 'gfull'], writes=['qn'])
            S.op('dve', lambda e, t=t: e.tensor_tensor(out=v3(t1[:]), in0=v3(qn[:]), in1=ropeC[:, t, :].unsqueeze(1).broadcast_to([128, 10, 64]), op=ALU.mult), reads=['qn', 'ropeC'], writes=['t1'])
            v5 = lambda ap: ap.rearrange("p (h b f j) -> p h b f j", h=10, b=2, f=2)
            for hf in range(2):
                S.op('dve', lambda e, t=t, hf=hf: e.tensor_tensor(
                    out=v5(t2[:])[:, :, :, hf, :], in0=v5(qn[:])[:, :, :, 1 - hf, :],
                    in1=ropeS[:, t, :].rearrange("p (b f j) -> p b f j", b=2, f=2)[:, :, hf, :].unsqueeze(1).broadcast_to([128, 10, 2, 16]), op=ALU.mult),
                    reads=['qn', 'ropeS'], writes=['t2'])
            pr = lambda ap: ap.rearrange("p (g j d) -> p j g d", g=2, j=4)
            S.op('dve', lambda e: e.tensor_tensor(out=qr[:, 0:512].rearrange("p (j g d) -> p j g d", j=4, g=2), in0=pr(t1[:, 0:512]), in1=pr(t2[:, 0:512]), op=ALU.add), reads=['t1', 't2'], writes=['qr'])
            for g in range(2):
                S.op('dve', lambda e, g=g: e.tensor_tensor(out=kz[g][:, g * 64:(g + 1) * 64], in0=t1[:, 512 + g * 64:576 + g * 64], in1=t2[:, 512 + g * 64:576 + g * 64], op=ALU.add), reads=['t1', 't2', 'kz'], writes=['kz'])
            if K.dbg and t == 2:
                dump(K, "qkv", qkv[j][:], [('qkv', j)]); dump(K, "qn", qn[:], ['qn']); dump(K, "qr", qr[:], ['qr'], BF16)

            def trq(e):
                for jj in range(4):
                    e.transpose(out=psb[:, 0, jj * 128:(jj + 1) * 128], in_=qr[:, jj * 128:(jj + 1) * 128], identity=identb[:])
                e.transpose(out=psb[:, 0, 512:640], in_=kz[0][:], identity=identb[:])
                return e.transpose(out=psb[:, 0, 640:768], in_=kz[1][:], identity=identb[:])
            S.op('pe', trq, reads=['qr', 'kz', 'identb'], writes=[('psb', 0)])
            S.op('act', lambda e, t=t: e.activation(out=qT[:, :, t * 128:(t + 1) * 128], in_=psb[:, 0, 0:512].rearrange("p (j q) -> p j q", j=4), func=AF.Copy), reads=[('psb', 0)], writes=[('qT', t)])
            S.op('dve', lambda e, t=t: e.tensor_scalar(out=kTp[0][:, t * 128:(t + 1) * 128], in0=psb[:, 0, 512:640], scalar1=1.0, scalar2=None, op0=ALU.mult), reads=[('psb', 0)], writes=[('kT', t)])
            S.op('dve', lambda e, t=t: e.tensor_scalar(out=kTp[1][:, t * 128:(t + 1) * 128], in0=psb[:, 0, 640:768], scalar1=1.0, scalar2=None, op0=ALU.mult), reads=[('psb', 0)], writes=[('kT', t)])
            S.op('dve', lambda e, t=t, j=j: e.tensor_copy(out=Vp[:, t, :, 0:64], in_=qkv[j][:, 640:768].rearrange("p (g d) -> p g d", g=2)), reads=[('qkv', j), 'Vp'], writes=[('Vp', t)])
        chunks = [(0, 2)] + [(2 + 4 * i, 4) for i in range(4)]
        if os.environ.get("KCUT") == "1":
            dump(K, "qT", qT[:, 0, :], [('qT', t) for t in range(NT)], BF16); dump(K, "kT0", kTp[0][:], [('kT', t) for t in range(NT)], BF16)
            chunks = []
        it = 0
        for (qt0, nq) in chunks:
            kts = list(range(2)) if qt0 == 0 else list(range(NT))
            nw = nq * 128
            for h in range(8):
                g, jj = h // 4, h % 4
                for ki, kt in enumerate(kts):
                    sbk = it % 2; it += 1
                    S.op('pe', lambda e, sbk=sbk, g=g, jj=jj, kt=kt, qt0=qt0, nw=nw: e.matmul(psf[:, sbk, 0:nw], lhsT=kTp[g][:, kt * 128:(kt + 1) * 128], rhs=qT[:, jj, qt0 * 128:qt0 * 128 + nw], start=True, stop=True),
                         reads=[('kT', kt)] + [('qT', qt0 + i) for i in range(nq)], writes=[('psf', sbk)])
                    S.op('act', lambda e, sbk=sbk, nw=nw: e.activation(out=PT[sbk][:, 0:nw], in_=psf[:, sbk, 0:nw], func=AF.Exp, scale=0.125, bias=negB[:]), reads=[('psf', sbk), 'negB'], writes=[('PT', sbk)])

                    def pv(e, sbk=sbk, nq=nq, kt=kt, g=g, ki=ki, last=(ki == len(kts) - 1)):
                        for i in range(nq):
                            ins = e.matmul(psf[:, 2 + i, 0:66], lhsT=PT[sbk][:, i * 128:(i + 1) * 128], rhs=Vp[:, kt, g, :], start=(ki == 0), stop=last)
                        return ins
                    S.op('pe', pv, reads=[('PT', sbk), ('Vp', kt), 'Vp'], writes=[('psfO', 0)])
                    if K.dbg and qt0 == 2 and h == 0 and ki == 0:
                        dump(K, "PT", PT[sbk][:], [('PT', sbk)], BF16)
                S.op('dve', lambda e, nq=nq: e.reciprocal(out=rc[:, 0:nq], in_=psf[:, 2:2 + nq, 64]), reads=[('psfO', 0)], writes=['rc'])
                S.op('dve', lambda e, nq=nq, h=h: e.tensor_tensor(out=ya[:, 0:nq, h * 64:(h + 1) * 64], in0=psf[:, 2:2 + nq, 0:64],
                                                                in1=rc[:, 0:nq].unsqueeze(2).broadcast_to([128, nq, 64]), op=ALU.mult), reads=[('psfO', 0), 'rc'], writes=['ya'])
            if K.dbg and qt0 == 2:
                dump(K, "ya", ya[:], ['ya'], BF16); dump(K, "rc", rc[:], ['rc'])
            for i in range(nq):
                def trya(e, i=i):
                    for c in range(4):
                        ins = e.transpose(out=psb[:, 1, c * 128:(c + 1) * 128], in_=ya[:, i, c * 128:(c + 1) * 128], identity=identb[:])
                    return ins
                S.op('pe', trya, reads=['ya', 'identb'], writes=[('psb', 1)])
                S.op('act', lambda e, i=i: e.activation(out=yaTs[:, :, i * 128:(i + 1) * 128], in_=psb[:, 1, 0:512].rearrange("p (c q) -> p c q", c=4), func=AF.Copy), reads=[('psb', 1)], writes=['yaTs'])
            for c in range(4):
                S.dma('sp', yT_d[c, :, qt0 * 128:qt0 * 128 + nw], yaTs[:, c, 0:nw], reads=['yaTs'], writes=[('yT_d', c)], sem='yTd')
        S.barrier()


def phase_ssd(K, layer):
    S, nc, psf, psb = K.S, K.nc, K.psf, K.psb
    identb, identf, eps_t, ones_f = K.identb, K.identf, K.eps_t, K.ones_f
    projT_d = dram(K, 'projT_d'); projF_d = dram(K, 'projF_d'); yT_d = dram(K, 'yT_d')
    with ExitStack() as pes:
        sb = lambda name, shape, dt=F32: pes.enter_context(nc.sbuf_tensor(f"L{layer}_p3_{name}", list(shape), dt))
        tri = [sb(f"tri{d}", (128, 128)) for d in range(2)]
        negm = [sb(f"negm{d}", (128, 128)) for d in range(2)]
        gmask = sb("gmask", (128, 2)); bmask = sb("bmask", (128, 512))
        cst = sb("cst", (32, 128)); cwT = sb("cwT", (128, 32))
        xin = [sb(f"xin{j}", (128, T)) for j in range(2)]
        acc = sb("acc", (128, T))
        xcF = sb("xcF", (128, T), BF16)
        BTp = [sb(f"BTp{g}", (128, T), BF16) for g in range(2)]
        CT = sb("CT", (128, T), BF16)
        xTM = sb("xTM", (128, NT, 512), BF16)
        BTM = sb("BTM", (128, NT, 128), BF16)
        dtw = sb("dtw", (128, NT, 72)); dtd = sb("dtd", (128, NT, 8)); adt = sb("adt", (128, NT, 8))
        prow = sb("prow", (128, 40)); arow = sb("arow", (128, 8)); grow = sb("grow", (128, 512))
        xd = sb("xd", (128, NT, 512), BF16)
        yacc = sb("yacc", (128, NT, 512))
        rhsA = sb("rhsA", (128, 8, 128)); acol = sb("acol", (128, 8))
        diff = sb("diff", (128, 8, 128)); LT = sb("LT", (128, 8, 128)); MT = sb("MT", (128, 8, 128), BF16)
        Erow = sb("Erow", (128, 8, 128)); CTs = sb("CTs", (128, 8, 128), BF16)
        dte = sb("dte", (128, 8)); atot = sb("atot", (128, 8)); xdd = sb("xdd", (128, 512), BF16)
        ST = sb("ST", (128, 512)); STm = sb("STm", (128, 512), BF16); cst2 = sb("cst2", (128, 512))
        zt = sb("zt", (128, 512)); yz = sb("yz", (128, 512)); ysq = sb("ysq", (128, 512)); yss = sb("yss", (128, 1))
        ybn = sb("ybn", (128, 512), BF16); ybTs = sb("ybTs", (128, 4, 128), BF16)
        for d, nm in enumerate(("triF", "triB")):
            S.dma('sp', tri[d][:], dram(K, nm, (128, 128))[:, :], writes=['tri'])
        for d, nm in enumerate(("negF", "negBm")):
            S.dma('sp', negm[d][:], dram(K, nm, (128, 128))[:, :], writes=['negm'])
        S.dma('sp', bmask[:], dram(K, 'bmask', (128, 512))[:, :], writes=['bmask'])
        S.op('dve', lambda e: e.tensor_copy(out=gmask[:, 0:1], in_=bmask[:, 0:1]), reads=['bmask'], writes=['gmask'])
        S.op('dve', lambda e: e.tensor_copy(out=gmask[:, 1:2], in_=bmask[:, 256:257]), reads=['bmask', 'gmask'], writes=['gmask'])
        S.op('dve', lambda e: e.memset(cst[:], 0.0), writes=['cst'])
        S.dma('sp', cst[0:18, :], dram(K, f'ssd_conv_w_{layer}').rearrange("k (c p) -> (k c) p", p=128), reads=['cst'], writes=['cst'])
        S.dma('sp', cst[18:24, :], dram(K, f'ssd_conv_b_{layer}').rearrange("(c p) -> c p", p=128), reads=['cst'], writes=['cst'])
        S.op('pe', lambda e: e.transpose(out=psf[:, 0, 0:32], in_=cst[:, :], identity=identf[0:32, 0:32]), reads=['cst', 'identf'], writes=[('psf', 0)])
        S.op('dve', lambda e: e.tensor_copy(out=cwT[:], in_=psf[:, 0, 0:32]), reads=[('psf', 0)], writes=['cwT'])
        S.dma('sp', prow[:], dram(K, f'ssdp_{layer}', (1, 40)).partition_broadcast(128), writes=['prow'])
        S.dma('sp', grow[:], dram(K, f'ssd_norm_g_{layer}').rearrange("(o n) -> o n", o=1).partition_broadcast(128), writes=['grow'])
        S.dma('sp', dtw[:], projT_d[:, 1216:1288].rearrange("(t p) h -> p t h", p=128), reads=[('projT_d', t) for t in range(NT)], writes=['dtw'])
        segs = [(0, TC), (TC, T)]
        for c in range(6):
            j = c % 2
            S.dma('sp', xin[j][:], projF_d[c], reads=[('projF_d', c)], writes=[('xin', j)], sem=f'xin{j}')
            w0 = cwT[:, c:c + 1]; w1 = cwT[:, 6 + c:7 + c]; w2 = cwT[:, 12 + c:13 + c]; bb = cwT[:, 18 + c:19 + c]
            S.op('dve', lambda e, j=j, w1=w1: e.tensor_scalar(out=acc[:], in0=xin[j][:], scalar1=w1, scalar2=None, op0=ALU.mult), reads=[('xin', j), 'cwT'], writes=['acc'])
            for (s0, s1) in segs:
                S.op('dve', lambda e, j=j, w0=w0, s0=s0, s1=s1: e.scalar_tensor_tensor(out=acc[:, s0 + 1:s1], in0=xin[j][:, s0:s1 - 1], scalar=w0, in1=acc[:, s0 + 1:s1], op0=ALU.mult, op1=ALU.add),
                     reads=[('xin', j), 'acc'], writes=['acc'])
                S.op('dve', lambda e, j=j, w2=w2, s0=s0, s1=s1: e.scalar_tensor_tensor(out=acc[:, s0:s1 - 1], in0=xin[j][:, s0 + 1:s1], scalar=w2, in1=acc[:, s0:s1 - 1], op0=ALU.mult, op1=ALU.add),
                     reads=[('xin', j), 'acc'], writes=['acc'])
            if c < 5:
                S.op('act', lambda e, bb=bb: e.activation(out=xcF[:], in_=acc[:], func=AF.Silu, bias=bb), reads=['acc', 'cwT'], writes=['xcF'])
                if c == 4:
                    for g in range(2):
                        S.op('dve', lambda e, g=g: e.tensor_scalar(out=BTp[g][:], in0=xcF[:], scalar1=gmask[:, g:g + 1], scalar2=None, op0=ALU.mult), reads=['xcF', 'gmask'], writes=['BTp'])
                for t in range(NT):
                    S.op('pe', lambda e, t=t: e.transpose(out=psb[:, t % 2, 0:128], in_=xcF[:, t * 128:(t + 1) * 128], identity=identb[:]), reads=['xcF', 'identb'], writes=[('psb', t % 2)])
                    dst = xTM[:, t, c * 128:(c + 1) * 128] if c < 4 else BTM[:, t, :]
                    if t % 2:
                        S.op('dve', lambda e, t=t, dst=dst: e.tensor_scalar(out=dst, in0=psb[:, t % 2, 0:128], scalar1=1.0, scalar2=None, op0=ALU.mult), reads=[('psb', t % 2)], writes=['xTM'])
                    else:
                        S.op('act', lambda e, t=t, dst=dst: e.activation(out=dst, in_=psb[:, t % 2, 0:128], func=AF.Copy), reads=[('psb', t % 2)], writes=['xTM'])
            else:
                S.op('act', lambda e, bb=bb: e.activation(out=CT[:], in_=acc[:], func=AF.Silu, bias=bb), reads=['acc', 'cwT'], writes=['CT'])
        h3 = lambda ap: ap.rearrange("p (h q) -> p h q", h=8)
        for t in range(NT):
            S.op('dve', lambda e, t=t: e.tensor_tensor(out=h3(yacc[:, t, :]), in0=h3(xTM[:, t, :]), in1=prow[:, 32:40].unsqueeze(2).broadcast_to([128, 8, 64]), op=ALU.mult), reads=['xTM', 'prow'], writes=[('yacc', t)])
        for d in range(2):
            S.op('act', lambda e, d=d: e.activation(out=arow[:], in_=prow[:, 16 + 8 * d:24 + 8 * d], func=AF.Exp), reads=['prow'], writes=['arow'])
            S.op('dve', lambda e, d=d: e.tensor_tensor(out=dtd[:], in0=dtw[:, :, 64:72], in1=prow[:, 8 * d:8 * d + 8].unsqueeze(1).broadcast_to([128, NT, 8]), op=ALU.add), reads=['dtw', 'prow'], writes=['dtd'])
            S.op('act', lambda e: e.activation(out=dtd[:], in_=dtd[:], func=AF.Exp), reads=['dtd'], writes=['dtd'])
            S.op('act', lambda e: e.activation(out=dtd[:], in_=dtd[:], func=AF.Ln, bias=1.0), reads=['dtd'], writes=['dtd'])
            S.op('dve', lambda e: e.scalar_tensor_tensor(out=adt[:], in0=dtd[:], scalar=-1.0, in1=arow[:].unsqueeze(1).broadcast_to([128, NT, 8]), op0=ALU.mult, op1=ALU.mult), reads=['dtd', 'arow'], writes=['adt'])
            for t in range(NT):
                S.op('dve', lambda e, t=t: e.tensor_tensor(out=h3(xd[:, t, :]), in0=h3(xTM[:, t, :]), in1=dtd[:, t, :].unsqueeze(2).broadcast_to([128, 8, 64]), op=ALU.mult), reads=['xTM', 'dtd'], writes=['xd'])
            S.op('dve', lambda e: e.memset(ST[:], 0.0), writes=['ST'])
            S.op('dve', lambda e: e.memset(STm[:], 0.0), writes=['STm'])
            order = list(range(NT)) if d == 0 else [1, 0] + list(range(NT - 1, 1, -1))
            last = 127 if d == 0 else 0
            for c in order:
                cs = slice(c * 128, (c + 1) * 128)
                S.op('dve', lambda e, c=c, d=d: e.tensor_tensor(out=rhsA[:], in0=tri[d][:].unsqueeze(1).broadcast_to([128, 8, 128]), in1=adt[:, c, :].unsqueeze(2).broadcast_to([128, 8, 128]), op=ALU.mult),
                     reads=['tri', 'adt'], writes=['rhsA'])

                def mmA(e, c=c, d=d):
                    e.matmul(psf[:, 0, :], lhsT=ones_f[:], rhs=rhsA[:, 0:4, :].rearrange("p h l -> p (h l)"), start=True, stop=True)
                    e.matmul(psf[:, 1, :], lhsT=ones_f[:], rhs=rhsA[:, 4:8, :].rearrange("p h l -> p (h l)"), start=True, stop=True)
                    e.matmul(psf[:, 2, 0:8], lhsT=tri[d][:], rhs=adt[:, c, :], start=True, stop=True)
                    for g in range(2):
                        ins = e.matmul(psf[:, 3, g * 128:(g + 1) * 128], lhsT=BTp[g][:, c * 128:(c + 1) * 128], rhs=CT[:, c * 128:(c + 1) * 128], start=True, stop=True)
                    return ins
                S.op('pe', mmA, reads=['rhsA', 'ones_f', 'tri', 'adt', 'BTp', 'CT'], writes=[('psf', 0), ('psf', 1), ('psf', 2), ('psf', 3)])
                arow_ps = psf[:, 0:2, :].rearrange("p a (h l) -> p (a h) l", h=4)
                P01 = [('psf', 0), ('psf', 1)]
                S.op('dve', lambda e: e.tensor_copy(out=acol[:], in_=psf[:, 2, 0:8]), reads=[('psf', 2)], writes=['acol'])
                S.op('dve', lambda e, arow_ps=arow_ps: e.tensor_tensor(out=diff[:], in0=arow_ps, in1=acol[:].unsqueeze(2).broadcast_to([128, 8, 128]), op=ALU.subtract), reads=P01 + ['acol'], writes=['diff'])
                S.op('dve', lambda e, arow_ps=arow_ps, last=last: e.tensor_tensor(out=dte[:], in0=arow_ps[:, :, last], in1=acol[:], op=ALU.subtract), reads=P01 + ['acol'], writes=['dte'])
                S.op('dve', lambda e, d=d: e.tensor_tensor(out=diff[:], in0=diff[:], in1=negm[d][:].unsqueeze(1).broadcast_to([128, 8, 128]), op=ALU.add), reads=['diff', 'negm'], writes=['diff'])
                S.op('act', lambda e, arow_ps=arow_ps: e.activation(out=Erow[:], in_=arow_ps, func=AF.Exp), reads=P01, writes=['Erow'])
                S.op('act', lambda e, arow_ps=arow_ps, last=last: e.activation(out=atot[:], in_=arow_ps[:, :, last], func=AF.Exp), reads=P01, writes=['atot'])
                S.op('act', lambda e: e.activation(out=LT[:], in_=diff[:], func=AF.Exp), reads=['diff'], writes=['LT'])
                S.op('act', lambda e: e.activation(out=dte[:], in_=dte[:], func=AF.Exp), reads=['dte'], writes=['dte'])
                S.op('dve', lambda e: e.tensor_tensor(out=MT[:].rearrange("p (g r) l -> p g r l", g=2), in0=LT[:].rearrange("p (g r) l -> p g r l", g=2),
                                                      in1=psf[:, 3, 0:256].rearrange("p (g l) -> p g l", g=2).unsqueeze(2).broadcast_to([128, 2, 4, 128]), op=ALU.mult), reads=['LT', ('psf', 3)], writes=['MT'])
                S.op('dve', lambda e, cs=cs: e.tensor_tensor(out=CTs[:], in0=Erow[:], in1=CT[:, cs].unsqueeze(1).broadcast_to([128, 8, 128]), op=ALU.mult), reads=['Erow', 'CT'], writes=['CTs'])
                S.op('dve', lambda e, c=c: e.tensor_tensor(out=h3(xdd[:]), in0=h3(xd[:, c, :]), in1=dte[:].unsqueeze(2).broadcast_to([128, 8, 64]), op=ALU.mult),
                     reads=['xd', 'dte'], writes=['xdd'])

                def mmY(e, c=c):
                    for h in range(8):
                        e.matmul(psf[:, 4, h * 64:(h + 1) * 64], lhsT=MT[:, h, :], rhs=xd[:, c, h * 64:(h + 1) * 64], start=True, stop=False)
                        e.matmul(psf[:, 4, h * 64:(h + 1) * 64], lhsT=CTs[:, h, :], rhs=STm[:, h * 64:(h + 1) * 64], start=False, stop=True)
                    return e.matmul(psf[:, 5, :], lhsT=BTM[:, c, :], rhs=xdd[:], start=True, stop=True)
                S.op('pe', mmY, reads=['MT', 'xd', 'CTs', 'STm', 'xTM', 'xdd'], writes=[('psf', 4), ('psf', 5)])
                S.op('dve', lambda e, c=c: e.tensor_tensor(out=yacc[:, c, :], in0=yacc[:, c, :], in1=psf[:, 4, :], op=ALU.add), reads=[('yacc', c), ('psf', 4)], writes=[('yacc', c)])
                S.op('dve', lambda e: e.tensor_tensor(out=cst2[:], in0=psf[:, 5, :], in1=bmask[:], op=ALU.mult), reads=[('psf', 5), 'bmask'], writes=['cst2'])
                S.op('dve', lambda e: e.tensor_tensor(out=h3(ST[:]), in0=h3(ST[:]), in1=atot[:].unsqueeze(2).broadcast_to([128, 8, 64]), op=ALU.mult), reads=['ST', 'atot'], writes=['ST'])
                S.op('dve', lambda e: e.tensor_tensor(out=ST[:], in0=ST[:], in1=cst2[:], op=ALU.add), reads=['ST', 'cst2'], writes=['ST'])
                S.op('dve', lambda e: e.tensor_copy(out=STm[:], in_=ST[:]), reads=['ST'], writes=['STm'])
        for t in range(NT):
            S.dma('sp', zt[:], projT_d[t * 128:(t + 1) * 128, 768:1280], reads=[('projT_d', t)], writes=['zt'], sem='zt')
            S.op('act', lambda e: e.activation(out=zt[:], in_=zt[:], func=AF.Silu), reads=['zt'], writes=['zt'])
            S.op('dve', lambda e, t=t: e.tensor_tensor(out=yz[:], in0=yacc[:, t, :], in1=zt[:], op=ALU.mult), reads=[('yacc', t), 'zt'], writes=['yz'])
            S.op('act', lambda e: e.activation(out=ysq[:], in_=yz[:], func=AF.Square, accum_out=yss[:]), reads=['yz'], writes=['ysq', 'yss'])
            S.op('act', lambda e: e.activation(out=yss[:], in_=yss[:], func=AF.Sqrt, scale=1.0 / 512, bias=eps_t[:]), reads=['yss', 'eps'], writes=['yss'])
            S.op('dve', lambda e: e.reciprocal(out=yss[:], in_=yss[:]), reads=['yss'], writes=['yss'])
            S.op('dve', lambda e: e.scalar_tensor_tensor(out=ybn[:], in0=yz[:], scalar=yss[:, 0:1], in1=grow[:], op0=ALU.mult, op1=ALU.mult), reads=['yz', 'yss', 'grow'], writes=['ybn'])

            def tryb(e):
                for c in range(4):
                    ins = e.transpose(out=psb[:, 0, c * 128:(c + 1) * 128], in_=ybn[:, c * 128:(c + 1) * 128], identity=identb[:])
                return ins
            S.op('pe', tryb, reads=['ybn', 'identb'], writes=[('psb', 0)])
            S.op('act', lambda e: e.activation(out=ybTs[:], in_=psb[:, 0, 0:512].rearrange("p (c q) -> p c q", c=4), func=AF.Copy), reads=[('psb', 0)], writes=['ybTs'])
            for c in range(4):
                S.dma('sp', yT_d[4 + c, :, t * 128:(t + 1) * 128], ybTs[:, c, :], reads=['ybTs'], writes=[('yT_d', 4 + c)], sem='yTd')
        S.barrier()


def phase_sc(K, layer):
    S, nc, psf = K.S, K.nc, K.psf
    identf = K.identf
    projF_d = dram(K, 'projF_d'); yT_d = dram(K, 'yT_d')
    with ExitStack() as pes:
        sb = lambda name, shape, dt=F32: pes.enter_context(nc.sbuf_tensor(f"L{layer}_p5_{name}", list(shape), dt))
        cst = sb("cst", (32, 128)); cwT = sb("cwT", (128, 32))
        tb = [sb(f"tb{j}", (128, T)) for j in range(2)]
        tg = [sb(f"tg{j}", (128, T)) for j in range(2)]
        th = [sb(f"th{j}", (128, T)) for j in range(2)]
        pr = sb("pr", (128, T)); acc = sb("acc", (128, T)); yd = sb("yd", (128, T), BF16)
        S.op('dve', lambda e: e.memset(cst[:], 0.0), writes=['cst'])
        S.dma('sp', cst[0:9, :], dram(K, f'sc_conv_w_{layer}').rearrange("k (c p) -> (k c) p", p=128), reads=['cst'], writes=['cst'])
        S.op('pe', lambda e: e.transpose(out=psf[:, 0, 0:32], in_=cst[:, :], identity=identf[0:32, 0:32]), reads=['cst', 'identf'], writes=[('psf', 0)])
        S.op('dve', lambda e: e.tensor_copy(out=cwT[:], in_=psf[:, 0, 0:32]), reads=[('psf', 0)], writes=['cwT'])
        segs = [(0, TC), (TC, T)]
        for c in range(3):
            j = c % 2
            S.dma('sp', tb[j][:], projF_d[9 + c], reads=[('projF_d', 9 + c)], writes=[('tb', j)], sem=f'scb{j}')
            S.dma('sp', tg[j][:], projF_d[12 + c], reads=[('projF_d', 12 + c)], writes=[('tg', j)], sem=f'scb{j}')
            S.dma('sp', th[j][:], projF_d[15 + c], reads=[('projF_d', 15 + c)], writes=[('th', j)], sem=f'scb{j}')
            w0 = cwT[:, c:c + 1]; w1 = cwT[:, 3 + c:4 + c]; w2 = cwT[:, 6 + c:7 + c]
            S.op('dve', lambda e, j=j: e.tensor_tensor(out=pr[:], in0=tg[j][:], in1=th[j][:], op=ALU.mult), reads=[('tg', j), ('th', j)], writes=['pr'])
            S.op('dve', lambda e, w1=w1: e.tensor_scalar(out=acc[:], in0=pr[:], scalar1=w1, scalar2=None, op0=ALU.mult), reads=['pr', 'cwT'], writes=['acc'])
            for (s0, s1) in segs:
                S.op('dve', lambda e, w0=w0, s0=s0, s1=s1: e.scalar_tensor_tensor(out=acc[:, s0 + 1:s1], in0=pr[:, s0:s1 - 1], scalar=w0, in1=acc[:, s0 + 1:s1], op0=ALU.mult, op1=ALU.add), reads=['pr', 'acc'], writes=['acc'])
                S.op('dve', lambda e, w2=w2, s0=s0, s1=s1: e.scalar_tensor_tensor(out=acc[:, s0:s1 - 1], in0=pr[:, s0 + 1:s1], scalar=w2, in1=acc[:, s0:s1 - 1], op0=ALU.mult, op1=ALU.add), reads=['pr', 'acc'], writes=['acc'])
            S.op('dve', lambda e, j=j: e.tensor_tensor(out=yd[:], in0=acc[:], in1=tb[j][:], op=ALU.mult), reads=['acc', ('tb', j)], writes=['yd'])
            S.dma('sp', yT_d[11 + c], yd[:], reads=['yd'], writes=[('yT_d', 11 + c)], sem='yTd')
        S.barrier()


def phase_merge(K, layer):
    S, nc, psf = K.S, K.nc, K.psf
    hT_d = dram(K, 'hT_d'); yT_d = dram(K, 'yT_d'); modrow_d = dram(K, 'modrow_d'); x_d = dram(K, 'x_d')
    xo_d = dram(K, 'xo_d', (T, D)) if K.kinds.get('xo_d') else x_d
    with ExitStack() as pes:
        sb = lambda name, shape, dt=F32: pes.enter_context(nc.sbuf_tensor(f"L{layer}_p6_{name}", list(shape), dt))
        wg = sb("wg", (128, 8, 4096), BF16)
        wbr = sb("wbr", (128, 14, 1024), BF16)
        wout = sb("wout", (128, 8, 1024), BF16)
        grow = sb("grow", (128, 2, 1024))
        hT = sb("hT", (128, 8, 512), BF16); yT = sb("yT", (128, 14, 512), BF16)
        mT = sb("mT", (128, 8, 512), BF16)
        gs = [sb(f"gs{j}", (128, 512)) for j in range(2)]
        macc = sb("macc", (128, 512)); mtmp = sb("mtmp", (128, 512))
        xt = [sb(f"xt{j}", (128, 1024)) for j in range(2)]
        ytmp = sb("ytmp", (128, 1024))
        wv = dram(K, f'w_in_{layer}').rearrange("(k p) n -> p k n", p=128)
        for i in range(8):
            S.dma('pool', wg[:, :, i * 512:(i + 1) * 512], wv[:, :, 3592 + i * 512:3592 + (i + 1) * 512], writes=['wg'], sem='mw')
        k0 = 0
        for nm, nk in (('w_br_attn', 4), ('w_br_ssd', 4), ('w_br_s5', 3), ('w_br_sc', 3)):
            S.dma('pool', wbr[:, k0:k0 + nk, :], dram(K, f'{nm}_{layer}').rearrange("(k p) n -> p k n", p=128), writes=['wbr'], sem='mw')
            k0 += nk
        S.dma('pool', wout[:], dram(K, f'w_out_{layer}').rearrange("(k p) n -> p k n", p=128), writes=['wout'], sem='mw')
        for w in range(2):
            S.dma('sp', grow[:, w, :], modrow_d[w:w + 1, 16:24, :].rearrange("o j p -> o (j p)").partition_broadcast(128), reads=['modrow_d'], writes=['grow'])
        kr = [(0, 4), (4, 8), (8, 11), (11, 14)]
        pi = 0
        for (n0, nw) in TOKCH:
            S.dma('sp', hT[:, :, 0:nw], hT_d[:, :, n0:n0 + nw].rearrange("k p n -> p k n"), reads=[('hT_d', f) for f in range(8)], writes=['hT'], sem='mh')
            S.dma('sp', yT[:, :, 0:nw], yT_d[:, :, n0:n0 + nw].rearrange("k p n -> p k n"), reads=[('yT_d', f) for f in range(14)], writes=['yT'], sem='mh')
            for f in range(8):
                for b in range(4):
                    pg = pi % 2; pi += 1

                    def mmg(e, f=f, b=b, pg=pg, nw=nw):
                        for k in range(8):
                            ins = e.matmul(psf[:, pg, 0:nw], lhsT=wg[:, k, b * 1024 + f * 128:b * 1024 + (f + 1) * 128], rhs=hT[:, k, 0:nw], start=(k == 0), stop=(k == 7))
                        return ins
                    S.op('pe', mmg, reads=['wg', 'hT'], writes=[('psf', pg)])
                    S.op('act', lambda e, pg=pg, nw=nw: e.activation(out=gs[pg][:, 0:nw], in_=psf[:, pg, 0:nw], func=AF.Sigmoid), reads=[('psf', pg)], writes=[('gs', pg)])

                    def mmb(e, f=f, b=b, pg=pg, nw=nw):
                        ks = list(range(*kr[b]))
                        for k in ks:
                            ins = e.matmul(psf[:, 2 + pg, 0:nw], lhsT=wbr[:, k, f * 128:(f + 1) * 128], rhs=yT[:, k, 0:nw], start=(k == ks[0]), stop=(k == ks[-1]))
                        return ins
                    S.op('pe', mmb, reads=['wbr', 'yT'], writes=[('psf', 2 + pg)])
                    if b == 0:
                        S.op('dve', lambda e, pg=pg, nw=nw: e.tensor_tensor(out=macc[:, 0:nw], in0=psf[:, 2 + pg, 0:nw], in1=gs[pg][:, 0:nw], op=ALU.mult), reads=[('psf', 2 + pg), ('gs', pg)], writes=['macc'])
                    else:
                        S.op('dve', lambda e, pg=pg, nw=nw: e.tensor_tensor(out=mtmp[:, 0:nw], in0=psf[:, 2 + pg, 0:nw], in1=gs[pg][:, 0:nw], op=ALU.mult), reads=[('psf', 2 + pg), ('gs', pg)], writes=['mtmp'])
                        if b < 3:
                            S.op('dve', lambda e, nw=nw: e.tensor_tensor(out=macc[:, 0:nw], in0=macc[:, 0:nw], in1=mtmp[:, 0:nw], op=ALU.add), reads=['macc', 'mtmp'], writes=['macc'])
                        else:
                            S.op('dve', lambda e, nw=nw, f=f: e.tensor_tensor(out=mT[:, f, 0:nw], in0=macc[:, 0:nw], in1=mtmp[:, 0:nw], op=ALU.add), reads=['macc', 'mtmp'], writes=['mT'])
            for i in range(nw // 128):
                t = n0 // 128 + i
                w = 1 if t < 2 else 0
                j = t % 2
                S.dma('sp', xt[j][:], x_d[t * 128:(t + 1) * 128, :], reads=[('x_d', t)], writes=[('xt', j)], sem=f'mx{j}')

                def mmo(e, i=i):
                    for hh in range(2):
                        for k in range(8):
                            ins = e.matmul(psf[:, 4 + hh, :], lhsT=mT[:, k, i * 128:(i + 1) * 128], rhs=wout[:, k, hh * 512:(hh + 1) * 512], start=(k == 0), stop=(k == 7))
                    return ins
                S.op('pe', mmo, reads=['mT', 'wout'], writes=[('psf', 4), ('psf', 5)])
                S.op('dve', lambda e, w=w: e.tensor_tensor(out=ytmp[:], in0=psf[:, 4:6, :].rearrange("p a b -> p (a b)"), in1=grow[:, w, :], op=ALU.mult), reads=[('psf', 4), ('psf', 5), 'grow'], writes=['ytmp'])
                S.op('dve', lambda e, j=j: e.tensor_tensor(out=xt[j][:], in0=xt[j][:], in1=ytmp[:], op=ALU.add), reads=[('xt', j), 'ytmp'], writes=[('xt', j)])
                S.dma('sp', xo_d[t * 128:(t + 1) * 128, :], xt[j][:], reads=[('xt', j)], writes=[('x_d', t), ('xo_d', t)], sem=f'mx{j}')
        S.barrier()


def phase_moe(K, layer):
    S, nc, psf, psb = K.S, K.nc, K.psf, K.psb
    identf, identb, eps_t, ones_f = K.identf, K.identb, K.eps_t, K.ones_f
    NER = int(os.environ.get("KNE", str(NE)))
    x_d = dram(K, 'x_d'); modrow_d = dram(K, 'modrow_d')
    xo_d = dram(K, 'xo_d', (T, D)) if K.kinds.get('xo_d') else x_d
    wgd = dram(K, f'w_exp_gate_{layer}', (NER, D, D)); wud = dram(K, f'w_exp_up_{layer}', (NER, D, D)); wdd = dram(K, f'w_exp_down_{layer}', (NER, D, D))
    with ExitStack() as pes:
        sb = lambda name, shape, dt=F32: pes.enter_context(nc.sbuf_tensor(f"L{layer}_p7_{name}", list(shape), dt))
        modv = load_modv(K, sb)
        S2 = lambda w, f: modv[:, w * 48 + 24 + f:w * 48 + 25 + f]
        G2 = lambda w, f: modv[:, 112 + w * 8 + f:113 + w * 8 + f]
        xnb = sb("xnb", (128, NT, 1024), BF16)
        macc = sb("macc", (128, NT, 1024))
        aff = sb("aff", (128, NT, 16)); mask = sb("mask", (128, NT, 16)); affm = sb("affm", (128, NT, 16)); pos = sb("pos", (128, NT, 16))
        iota = sb("iota", (128, 256)); triS = sb("triS", (128, 128)); wr = sb("wr", (128, 8, 16))
        S.dma('sp', iota[:], dram(K, 'iota', (128, 256))[:, :], writes=['iota'])
        S.dma('sp', triS[:], dram(K, 'triS', (128, 128))[:, :], writes=['triS'])
        S.dma('sp', wr[:], dram(K, f'w_router_{layer}').rearrange("(k p) e -> p k e", p=128), writes=['wr'])
        S.op('pool', lambda e: e.memset(macc[:], 0.0), writes=['macc'])
        mats = []
        for ei in range(NER):
            mats += [(wgd, ei), (wud, ei), (wdd, ei)]
        wstate = {'next': 0}

        def issue_weights(upto):
            while wstate['next'] < min(upto, len(mats)):
                mi = wstate['next']; wstate['next'] += 1
                src, ei = mats[mi]
                v = src[ei].rearrange("(k p) n -> p k n", p=128)
                for hh in range(2):
                    S.dma('pool', Wb[mi % 4][:, hh * 4:(hh + 1) * 4, :], v[:, hh * 4:(hh + 1) * 4, :], writes=[('Wb', mi % 4)], sem=f'wb{mi % 4}')
        with ExitStack() as aes:
            sa = lambda name, shape, dt=F32: aes.enter_context(nc.sbuf_tensor(f"L{layer}_p7a_{name}", list(shape), dt))
            xt = [sa(f"xt{j}", (128, 1024)) for j in range(2)]
            sq = sa("sq", (128, 1024)); xnf = sa("xnf", (128, 1024))
            ss = sa("ss", (128, 1)); rstd = sa("rstd", (128, 1))
            hT2 = sa("hT2", (128, 8, 128))
            mx = sa("mx", (128, 1)); ssum = sa("ssum", (128, 1))
            affT = sa("affT", (16, T)); wk = sa("wk", (16, TL)); m8 = sa("m8", (16, 8))
            thr = sa("thr", (16, 2)); diag16 = sa("diag16", (16, 16)); throw = sa("throw", (128, 2, 16))
            for t in range(NT):
                w = 1 if t < 2 else 0
                j = t % 2
                S.dma('sp', xt[j][:], x_d[t * 128:(t + 1) * 128, :], reads=[('x_d', t)], writes=[('xt', j)], sem=f'mxt{j}')
                S.op('act', lambda e, j=j: e.activation(out=sq[:], in_=xt[j][:], func=AF.Square, accum_out=ss[:]), reads=[('xt', j)], writes=['sq', 'ss'])
                S.op('act', lambda e: e.activation(out=rstd[:], in_=ss[:], func=AF.Sqrt, scale=1.0 / D, bias=eps_t[:]), reads=['ss', 'eps'], writes=['rstd'])
                S.op('dve', lambda e: e.reciprocal(out=rstd[:], in_=rstd[:]), reads=['rstd'], writes=['rstd'])
                S.op('dve', lambda e, j=j: e.tensor_scalar(out=xnf[:], in0=xt[j][:], scalar1=rstd[:, 0:1], scalar2=None, op0=ALU.mult), reads=[('xt', j), 'rstd'], writes=['xnf'])
                S.op('act', lambda e, t=t: e.activation(out=xnb[:, t, :], in_=xnf[:], func=AF.Copy), reads=['xnf'], writes=[('xnb', t)])

                def trr(e):
                    for f in range(8):
                        ins = e.transpose(out=psf[:, f // 4, (f % 4) * 128:(f % 4 + 1) * 128], in_=xnf[:, f * 128:(f + 1) * 128], identity=identf[:])
                    return ins
                S.op('pe', trr, reads=['xnf', 'identf'], writes=[('psf', 0), ('psf', 1)])
                for f in range(8):
                    S.op('dve' if f % 2 else 'act',
                         (lambda e, f=f, w=w: e.tensor_scalar(out=hT2[:, f, :], in0=psf[:, f // 4, (f % 4) * 128:(f % 4 + 1) * 128], scalar1=G2(w, f), scalar2=S2(w, f), op0=ALU.mult, op1=ALU.add)) if f % 2 else
                         (lambda e, f=f, w=w: e.activation(out=hT2[:, f, :], in_=psf[:, f // 4, (f % 4) * 128:(f % 4 + 1) * 128], func=AF.Identity, scale=G2(w, f), bias=S2(w, f))),
                         reads=[('psf', f // 4), 'modv'], writes=['hT2'])

                def mmr(e):
                    for k in range(8):
                        ins = e.matmul(psf[:, 2, 0:16], lhsT=hT2[:, k, :], rhs=wr[:, k, :], start=(k == 0), stop=(k == 7))
                    return ins
                S.op('pe', mmr, reads=['hT2', 'wr'], writes=[('psf', 2)])
                S.op('dve', lambda e: e.tensor_reduce(out=mx[:], in_=psf[:, 2, 0:16], axis=AX.X, op=ALU.max), reads=[('psf', 2)], writes=['mx'])
                S.op('dve', lambda e: e.tensor_scalar(out=mx[:], in0=mx[:], scalar1=-1.0, scalar2=None, op0=ALU.mult), reads=['mx'], writes=['mx'])
                S.op('act', lambda e, t=t: e.activation(out=aff[:, t, :], in_=psf[:, 2, 0:16], func=AF.Exp, bias=mx[:], accum_out=ssum[:]), reads=[('psf', 2), 'mx'], writes=[('aff', t), 'ssum'])
                S.op('dve', lambda e: e.reciprocal(out=ssum[:], in_=ssum[:]), reads=['ssum'], writes=['ssum'])
                S.op('dve', lambda e, t=t: e.tensor_scalar(out=aff[:, t, :], in0=aff[:, t, :], scalar1=ssum[:, 0:1], scalar2=None, op0=ALU.mult), reads=[('aff', t), 'ssum'], writes=[('aff', t)])
            for g0 in range(0, NT, 4):
                tl = list(range(g0, min(g0 + 4, NT)))

                def tra(e, tl=tl):
                    for i, t in enumerate(tl):
                        ins = e.transpose(out=psf[0:16, 3, i * 128:(i + 1) * 128], in_=aff[:, t, :], identity=identf[:])
                    return ins
                S.op('pe', tra, reads=[('aff', t) for t in tl] + ['identf'], writes=[('psf', 3)])
                S.op('dve', lambda e, g0=g0, n=len(tl): e.tensor_copy(out=affT[:, g0 * 128:(g0 + n) * 128], in_=psf[0:16, 3, 0:n * 128]), reads=[('psf', 3)], writes=['affT'])
            for seg, (c0, c1, nit) in enumerate([(TC, T, 32), (0, TC, 4)]):
                n = c1 - c0
                S.op('dve', lambda e, c0=c0, c1=c1, n=n: e.tensor_copy(out=wk[:, 0:n], in_=affT[:, c0:c1]), reads=['affT', 'wk'], writes=['wk'])
                for it in range(nit):
                    S.op('dve', lambda e, n=n: e.max(out=m8[:], in_=wk[:, 0:n]), reads=['wk'], writes=['m8'])
                    if it < nit - 1:
                        S.op('dve', lambda e, n=n: e.match_replace(out=wk[:, 0:n], in_to_replace=m8[:], in_values=wk[:, 0:n], imm_value=-1.0), reads=['wk', 'm8'], writes=['wk'])
                S.op('dve', lambda e, seg=seg: e.tensor_copy(out=thr[:, seg:seg + 1], in_=m8[:, 7:8]), reads=['m8'], writes=['thr'])
                S.op('dve', lambda e, seg=seg: e.tensor_scalar(out=diag16[:], in0=identf[0:16, 0:16], scalar1=thr[:, seg:seg + 1], scalar2=None, op0=ALU.mult), reads=['thr', 'identf'], writes=['diag16'])
                S.op('pe', lambda e: e.matmul(psf[:, 3, 0:16], lhsT=ones_f[0:16, :], rhs=diag16[:], start=True, stop=True), reads=['diag16', 'ones_f'], writes=[('psf', 3)])
                S.op('dve', lambda e, seg=seg: e.tensor_copy(out=throw[:, seg, :], in_=psf[:, 3, 0:16]), reads=[('psf', 3)], writes=['throw'])
            for t in range(NT):
                seg = 1 if t < 2 else 0
                S.op('dve', lambda e, t=t, seg=seg: e.tensor_tensor(out=mask[:, t, :], in0=aff[:, t, :], in1=throw[:, seg, :], op=ALU.is_ge), reads=[('aff', t), 'throw'], writes=[('mask', t)])
                S.op('dve', lambda e, t=t: e.tensor_tensor(out=affm[:, t, :], in0=aff[:, t, :], in1=mask[:, t, :], op=ALU.mult), reads=[('aff', t), ('mask', t)], writes=[('affm', t)])
            for t in range(NT):
                prev = list(range(0, t)) if t < 2 else list(range(2, t))

                def mmp(e, t=t, prev=prev):
                    ins = e.matmul(psf[:, t % 2, 0:16], lhsT=triS[:], rhs=mask[:, t, :], start=True, stop=(len(prev) == 0))
                    for i, tp in enumerate(prev):
                        ins = e.matmul(psf[:, t % 2, 0:16], lhsT=ones_f[:], rhs=mask[:, tp, :], start=False, stop=(i == len(prev) - 1))
                    return ins
                S.op('pe', mmp, reads=[('mask', tp) for tp in prev + [t]] + ['triS', 'ones_f'], writes=[('psf', t % 2)])
                S.op('dve', lambda e, t=t: e.tensor_copy(out=pos[:, t, :], in_=psf[:, t % 2, 0:16]), reads=[('psf', t % 2)], writes=[('pos', t)])
            S.barrier()
        des = ExitStack()
        sb_outer = sb
        sb = lambda name, shape, dt=F32: des.enter_context(nc.sbuf_tensor(f"L{layer}_p7d_{name}", list(shape), dt))
        Wb = [sb(f"Wb{j}", (128, 8, 1024), BF16) for j in range(4)]
        issue_weights(4)
        Soh = sb("Soh", (128, NT, 256), BF16)
        SWb = [sb(f"SW{q}", (128, 256), BF16) for q in range(2)]; SWTb = [sb(f"SWT{q}", (128, 2, 128), BF16) for q in range(2)]
        xgT = sb("xgT", (128, 8, 288), BF16); hidT = sb("hidT", (128, 8, 288), BF16)
        sg = sb("sg", (128, 288))
        ye = [sb(f"ye{j}", (128, 1024), BF16) for j in range(3)]
        pcnt = 0
        for ei in range(NER):
            Wg, Wu, Wd = Wb[(ei * 3) % 4], Wb[(ei * 3 + 1) % 4], Wb[(ei * 3 + 2) % 4]
            kWg, kWu, kWd = ('Wb', (ei * 3) % 4), ('Wb', (ei * 3 + 1) % 4), ('Wb', (ei * 3 + 2) % 4)
            for t in range(NT):
                ns = 32 if t < 2 else 256
                S.op('dve', lambda e, t=t, ns=ns, ei=ei: e.tensor_scalar(out=Soh[:, t, 0:ns], in0=iota[:, 0:ns], scalar1=pos[:, t, ei:ei + 1], scalar2=mask[:, t, ei:ei + 1], op0=ALU.is_equal, op1=ALU.mult),
                     reads=['iota', ('pos', t), ('mask', t)], writes=[('Soh', t)])
            for f in range(8):
                pb = pcnt % 2; pcnt += 1

                def mmg(e, f=f, pb=pb):
                    for i, t in enumerate(range(2, NT)):
                        ins = e.matmul(psf[:, pb, 0:256], lhsT=xnb[:, t, f * 128:(f + 1) * 128], rhs=Soh[:, t, :], start=(i == 0), stop=(i == NT - 3))
                    for i, t in enumerate(range(2)):
                        ins = e.matmul(psf[:, pb, 256:288], lhsT=xnb[:, t, f * 128:(f + 1) * 128], rhs=Soh[:, t, 0:32], start=(i == 0), stop=(i == 1))
                    return ins
                S.op('pe', mmg, reads=[('xnb', t) for t in range(NT)] + [('Soh', t) for t in range(NT)], writes=[('psf', pb)])
                S.op('act', lambda e, f=f, pb=pb: e.activation(out=xgT[:, f, 0:256], in_=psf[:, pb, 0:256], func=AF.Identity, scale=G2(0, f), bias=S2(0, f)), reads=[('psf', pb), 'modv'], writes=['xgT'])
                S.op('act', lambda e, f=f, pb=pb: e.activation(out=xgT[:, f, 256:288], in_=psf[:, pb, 256:288], func=AF.Identity, scale=G2(1, f), bias=S2(1, f)), reads=[('psf', pb), 'modv'], writes=['xgT'])
            for m in range(8):
                pb = 2 * (m % 2)

                def mmf(e, m=m, pb=pb):
                    for k in range(8):
                        e.matmul(psf[:, pb, 0:288], lhsT=Wg[:, k, m * 128:(m + 1) * 128], rhs=xgT[:, k, :], start=(k == 0), stop=(k == 7))
                    for k in range(8):
                        ins = e.matmul(psf[:, pb + 1, 0:288], lhsT=Wu[:, k, m * 128:(m + 1) * 128], rhs=xgT[:, k, :], start=(k == 0), stop=(k == 7))
                    return ins
                S.op('pe', mmf, reads=[kWg, kWu, 'xgT'], writes=[('psf', pb), ('psf', pb + 1)])
                S.op('act', lambda e, pb=pb: e.activation(out=sg[:], in_=psf[:, pb, 0:288], func=AF.Silu), reads=[('psf', pb)], writes=['sg'])
                S.op('dve', lambda e, m=m, pb=pb: e.tensor_tensor(out=hidT[:, m, :], in0=psf[:, pb + 1, 0:288], in1=sg[:], op=ALU.mult), reads=[('psf', pb + 1), 'sg'], writes=['hidT'])
            issue_weights(ei * 3 + 6)
            for sidx, (s0, sn) in enumerate([(0, 128), (128, 128), (256, 32)]):
                def mmd(e, s0=s0, sn=sn):
                    for hh in range(2):
                        for m in range(8):
                            ins = e.matmul(psf[0:sn, 4 + hh, :], lhsT=hidT[:, m, s0:s0 + sn], rhs=Wd[:, m, hh * 512:(hh + 1) * 512], start=(m == 0), stop=(m == 7))
                    return ins
                S.op('pe', mmd, reads=['hidT', kWd], writes=[('psf', 4), ('psf', 5)])
                S.op('act', lambda e, sidx=sidx, sn=sn: e.activation(out=ye[sidx][0:sn, :], in_=psf[0:sn, 4:6, :].rearrange("p a b -> p (a b)"), func=AF.Copy), reads=[('psf', 4), ('psf', 5)], writes=[('ye', sidx)])
            issue_weights(ei * 3 + 7)
            for t in range(NT):
                ns = 32 if t < 2 else 256
                j = t % 2
                pb = 2 * (t % 2)
                SW = SWb[j]; SWT = SWTb[j]
                S.op('dve', lambda e, t=t, ns=ns, ei=ei, SW=SW: e.tensor_scalar(out=SW[:, 0:ns], in0=iota[:, 0:ns], scalar1=pos[:, t, ei:ei + 1], scalar2=affm[:, t, ei:ei + 1], op0=ALU.is_equal, op1=ALU.mult),
                     reads=['iota', ('pos', t), ('affm', t)], writes=[('SW', j)])
                if t >= 2:
                    def trs(e, j=j, SW=SW):
                        e.transpose(out=psb[:, j, 0:128], in_=SW[:, 0:128], identity=identb[:])
                        return e.transpose(out=psb[:, j, 128:256], in_=SW[:, 128:256], identity=identb[:])
                    S.op('pe', trs, reads=[('SW', j), 'identb'], writes=[('psb', j)])
                    S.op('act', lambda e, j=j, SWT=SWT: e.activation(out=SWT[:].rearrange("p s q -> p (s q)"), in_=psb[:, j, 0:256], func=AF.Copy), reads=[('psb', j)], writes=[('SWT', j)])

                    def mms(e, pb=pb, SWT=SWT):
                        for hh in range(2):
                            for s_ in range(2):
                                ins = e.matmul(psf[:, pb + hh, :], lhsT=SWT[:, s_, :], rhs=ye[s_][:, hh * 512:(hh + 1) * 512], start=(s_ == 0), stop=(s_ == 1))
                        return ins
                    S.op('pe', mms, reads=[('SWT', j), ('ye', 0), ('ye', 1)], writes=[('psf', pb), ('psf', pb + 1)])
                else:
                    S.op('pe', lambda e, j=j, SW=SW: e.transpose(out=psb[0:32, j, 0:128], in_=SW[:, 0:32], identity=identb[:]), reads=[('SW', j), 'identb'], writes=[('psb', j)])
                    S.op('act', lambda e, j=j, SWT=SWT: e.activation(out=SWT[0:32, 0, :], in_=psb[0:32, j, 0:128], func=AF.Copy), reads=[('psb', j)], writes=[('SWT', j)])

                    def mms(e, pb=pb, SWT=SWT):
                        for hh in range(2):
                            ins = e.matmul(psf[:, pb + hh, :], lhsT=SWT[0:32, 0, :], rhs=ye[2][0:32, hh * 512:(hh + 1) * 512], start=True, stop=True)
                        return ins
                    S.op('pe', mms, reads=[('SWT', j), ('ye', 2)], writes=[('psf', pb), ('psf', pb + 1)])
                S.op('dve', lambda e, t=t, pb=pb: e.tensor_tensor(out=macc[:, t, :], in0=macc[:, t, :], in1=psf[:, pb:pb + 2, :].rearrange("p a b -> p (a b)"), op=ALU.add), reads=[('psf', pb), ('psf', pb + 1), 'macc'], writes=['macc'])
        S.barrier()
        des.close()
        sb = sb_outer
        grow = sb("grow", (128, 2, 1024))
        for w in range(2):
            S.dma('sp', grow[:, w, :], modrow_d[w:w + 1, 40:48, :].rearrange("o j p -> o (j p)").partition_broadcast(128), reads=['modrow_d'], writes=['grow'])
        xr = [sb(f"xr{j}", (128, 1024)) for j in range(2)]
        for t in range(NT):
            w = 1 if t < 2 else 0
            j = t % 2
            S.dma('sp', xr[j][:], x_d[t * 128:(t + 1) * 128, :], reads=[('x_d', t)], writes=[('xr', j)], sem=f'mxr{j}')
            S.op('dve', lambda e, t=t, w=w: e.tensor_tensor(out=macc[:, t, :], in0=macc[:, t, :], in1=grow[:, w, :], op=ALU.mult), reads=['macc', 'grow'], writes=['macc'])
            S.op('dve', lambda e, t=t, j=j: e.tensor_tensor(out=xr[j][:], in0=xr[j][:], in1=macc[:, t, :], op=ALU.add), reads=['macc', ('xr', j)], writes=[('xr', j)])
            S.dma('sp', xo_d[t * 128:(t + 1) * 128, :], xr[j][:], reads=[('xr', j)], writes=[('x_d', t), ('xo_d', t)], sem=f'mxr{j}')
        S.barrier()


def phase_final(K, layer):
    S, nc = K.S, K.nc
    eps_t = K.eps_t
    x_d = dram(K, 'x_d'); out = dram(K, 'out', (TL, D))
    with ExitStack() as pes:
        sb = lambda name, shape, dt=F32: pes.enter_context(nc.sbuf_tensor(f"fin_{name}", list(shape), dt))
        grow = sb("grow", (128, 1024))
        xt = [sb(f"xt{j}", (128, 1024)) for j in range(2)]
        sq = sb("sq", (128, 1024)); ss = sb("ss", (128, 1)); rstd = sb("rstd", (128, 1))
        S.dma('sp', grow[:], dram(K, 'final_norm_g', (D,)).rearrange("(o n) -> o n", o=1).partition_broadcast(128), writes=['grow'])
        for t in range(2, NT):
            j = t % 2
            S.dma('sp', xt[j][:], x_d[t * 128:(t + 1) * 128, :], reads=[('x_d', t)], writes=[('xt', j)], sem=f'fx{j}')
            S.op('act', lambda e, j=j: e.activation(out=sq[:], in_=xt[j][:], func=AF.Square, accum_out=ss[:]), reads=[('xt', j)], writes=['sq', 'ss'])
            S.op('act', lambda e: e.activation(out=rstd[:], in_=ss[:], func=AF.Sqrt, scale=1.0 / D, bias=eps_t[:]), reads=['ss', 'eps'], writes=['rstd'])
            S.op('dve', lambda e: e.reciprocal(out=rstd[:], in_=rstd[:]), reads=['rstd'], writes=['rstd'])
            S.op('dve', lambda e, j=j: e.scalar_tensor_tensor(out=xt[j][:], in0=xt[j][:], scalar=rstd[:, 0:1], in1=grow[:], op0=ALU.mult, op1=ALU.mult), reads=[('xt', j), 'rstd', 'grow'], writes=[('xt', j)])
            S.dma('sp', out[(t - 2) * 128:(t - 1) * 128, :], xt[j][:], reads=[('xt', j)], writes=[('out', t)], sem=f'fx{j}')
        S.barrier()


def phase_s5(K, layer):
    S, nc, psf = K.S, K.nc, K.psf
    identf = K.identf
    projF_d = dram(K, 'projF_d'); yT_d = dram(K, 'yT_d')
    NJ = int(os.environ.get("KNJ", "12"))
    with ExitStack() as pes:
        sb = lambda name, shape, dt=F32: pes.enter_context(nc.sbuf_tensor(f"L{layer}_p4_{name}", list(shape), dt))
        prm = sb("prm", (128, 846)); bmask = sb("bmask", (128, 512)); halfpi = sb("halfpi", (128, 1))
        uF = sb("uF", (128, 3, T)); yS = sb("yS", (128, 3, T))
        S.dma('sp', prm[:], dram(K, f's5p_{layer}', (128, 846))[:, :], writes=['prm'])
        S.dma('sp', bmask[:], dram(K, 'bmask', (128, 512))[:, :], writes=['bmask'])
        for ct in range(3):
            S.dma('sp', uF[:, ct, :], projF_d[6 + ct], reads=[('projF_d', 6 + ct)], writes=['uF'])
        S.op('dve', lambda e: e.memset(halfpi[:], float(np.pi / 2)), writes=['halfpi'])
        gm = [bmask[:, 0:1], bmask[:, 256:257]]
        BRE = prm[:, 72:264].rearrange("p (j i) -> p j i", j=12); BIM = prm[:, 264:456].rearrange("p (j i) -> p j i", j=12)
        CRE = prm[:, 456:648].rearrange("p (j i) -> p j i", j=12); CIM = prm[:, 648:840].rearrange("p (j i) -> p j i", j=12)
        Bb = [[sb(f"Bb{d}{c}", (128, 12, 16)) for c in range(2)] for d in range(2)]
        PR = [sb(f"PR{d}", (128, 12, 12)) for d in range(2)]; PI = [sb(f"PI{d}", (128, 12, 12)) for d in range(2)]; NPI = [sb(f"NPI{d}", (128, 12, 12)) for d in range(2)]
        tt = {n: sb("t_" + n, (128, 12)) for n in ("step", "a", "mag", "ang", "s", "c", "t1", "t2", "abr", "abi", "den", "nr", "cr", "ci", "u1", "u2")}
        tb1 = sb("tb1", (128, 12, 16)); tb2 = sb("tb2", (128, 12, 16))
        V = lambda e, f, r, w: S.op('dve', f, reads=r, writes=w)
        for d in range(2):
            LR = prm[:, 12 * d:12 * d + 12]; LI = prm[:, 24 + 12 * d:36 + 12 * d]; LS = prm[:, 48 + 12 * d:60 + 12 * d]
            S.op('act', lambda e, LS=LS: e.activation(out=tt["step"][:], in_=LS, func=AF.Exp), reads=['prm'], writes=['t_step'])
            V(0, lambda e, LR=LR: e.tensor_tensor(out=tt["a"][:], in0=LR, in1=tt["step"][:], op=ALU.mult), ['prm', 't_step'], ['t_a'])
            V(0, lambda e, LI=LI: e.tensor_tensor(out=tt["ang"][:], in0=LI, in1=tt["step"][:], op=ALU.mult), ['prm', 't_step'], ['t_ang'])
            S.op('act', lambda e: e.activation(out=tt["mag"][:], in_=tt["a"][:], func=AF.Exp), reads=['t_a'], writes=['t_mag'])
            S.op('act', lambda e: e.activation(out=tt["s"][:], in_=tt["ang"][:], func=AF.Sin, scale=1.0 / 32), reads=['t_ang'], writes=['t_s'])
            S.op('act', lambda e: e.activation(out=tt["c"][:], in_=tt["ang"][:], func=AF.Sin, scale=1.0 / 32, bias=halfpi[:]), reads=['t_ang', 'halfpi'], writes=['t_c'])
            for it in range(5):
                V(0, lambda e: e.tensor_tensor(out=tt["t1"][:], in0=tt["c"][:], in1=tt["c"][:], op=ALU.mult), ['t_c'], ['t_t1'])
                V(0, lambda e: e.tensor_tensor(out=tt["t2"][:], in0=tt["s"][:], in1=tt["s"][:], op=ALU.mult), ['t_s'], ['t_t2'])
                V(0, lambda e: e.scalar_tensor_tensor(out=tt["s"][:], in0=tt["c"][:], scalar=2.0, in1=tt["s"][:], op0=ALU.mult, op1=ALU.mult), ['t_c', 't_s'], ['t_s'])
                V(0, lambda e: e.tensor_tensor(out=tt["c"][:], in0=tt["t1"][:], in1=tt["t2"][:], op=ALU.subtract), ['t_t1', 't_t2', 't_s'], ['t_c'])
            V(0, lambda e: e.tensor_tensor(out=tt["abr"][:], in0=tt["mag"][:], in1=tt["c"][:], op=ALU.mult), ['t_mag', 't_c'], ['t_abr'])
            V(0, lambda e: e.tensor_tensor(out=tt["abi"][:], in0=tt["mag"][:], in1=tt["s"][:], op=ALU.mult), ['t_mag', 't_s'], ['t_abi'])
            V(0, lambda e, LR=LR: e.tensor_tensor(out=tt["t1"][:], in0=LR, in1=LR, op=ALU.mult), ['prm', 't_t1'], ['t_t1'])
            V(0, lambda e, LI=LI: e.tensor_tensor(out=tt["t2"][:], in0=LI, in1=LI, op=ALU.mult), ['prm', 't_t2'], ['t_t2'])
            V(0, lambda e: e.tensor_tensor(out=tt["den"][:], in0=tt["t1"][:], in1=tt["t2"][:], op=ALU.add), ['t_t1', 't_t2'], ['t_den'])
            V(0, lambda e: e.reciprocal(out=tt["den"][:], in_=tt["den"][:]), ['t_den'], ['t_den'])
            V(0, lambda e: e.tensor_scalar(out=tt["nr"][:], in0=tt["abr"][:], scalar1=-1.0, scalar2=None, op0=ALU.add), ['t_abr'], ['t_nr'])
            V(0, lambda e, LR=LR: e.tensor_tensor(out=tt["u1"][:], in0=tt["nr"][:], in1=LR, op=ALU.mult), ['t_nr', 'prm'], ['t_u1'])
            V(0, lambda e, LI=LI: e.tensor_tensor(out=tt["u2"][:], in0=tt["abi"][:], in1=LI, op=ALU.mult), ['t_abi', 'prm'], ['t_u2'])
            V(0, lambda e: e.tensor_tensor(out=tt["cr"][:], in0=tt["u1"][:], in1=tt["u2"][:], op=ALU.add), ['t_u1', 't_u2'], ['t_cr'])
            V(0, lambda e: e.tensor_tensor(out=tt["cr"][:], in0=tt["cr"][:], in1=tt["den"][:], op=ALU.mult), ['t_cr', 't_den'], ['t_cr'])
            V(0, lambda e, LR=LR: e.tensor_tensor(out=tt["u1"][:], in0=tt["abi"][:], in1=LR, op=ALU.mult), ['t_abi', 'prm', 't_u1'], ['t_u1'])
            V(0, lambda e, LI=LI: e.tensor_tensor(out=tt["u2"][:], in0=tt["nr"][:], in1=LI, op=ALU.mult), ['t_nr', 'prm', 't_u2'], ['t_u2'])
            V(0, lambda e: e.tensor_tensor(out=tt["ci"][:], in0=tt["u1"][:], in1=tt["u2"][:], op=ALU.subtract), ['t_u1', 't_u2'], ['t_ci'])
            V(0, lambda e: e.tensor_tensor(out=tt["ci"][:], in0=tt["ci"][:], in1=tt["den"][:], op=ALU.mult), ['t_ci', 't_den'], ['t_ci'])
            bc = lambda ap: ap.unsqueeze(2).broadcast_to([128, 12, 16])
            V(0, lambda e: e.tensor_tensor(out=tb1[:], in0=BRE, in1=bc(tt["cr"][:]), op=ALU.mult), ['prm', 't_cr'], ['tb1'])
            V(0, lambda e: e.tensor_tensor(out=tb2[:], in0=BIM, in1=bc(tt["ci"][:]), op=ALU.mult), ['prm', 't_ci'], ['tb2'])
            V(0, lambda e, d=d: e.tensor_tensor(out=Bb[d][0][:], in0=tb1[:], in1=tb2[:], op=ALU.subtract), ['tb1', 'tb2'], [('Bb', d)])
            V(0, lambda e: e.tensor_tensor(out=tb1[:], in0=BIM, in1=bc(tt["cr"][:]), op=ALU.mult), ['prm', 't_cr', 'tb1'], ['tb1'])
            V(0, lambda e: e.tensor_tensor(out=tb2[:], in0=BRE, in1=bc(tt["ci"][:]), op=ALU.mult), ['prm', 't_ci', 'tb2'], ['tb2'])
            V(0, lambda e, d=d: e.tensor_tensor(out=Bb[d][1][:], in0=tb1[:], in1=tb2[:], op=ALU.add), ['tb1', 'tb2', ('Bb', d)], [('Bb', d)])
            V(0, lambda e, d=d: e.tensor_copy(out=PR[d][:, :, 0], in_=tt["abr"][:]), ['t_abr'], [('PW', d)])
            V(0, lambda e, d=d: e.tensor_copy(out=PI[d][:, :, 0], in_=tt["abi"][:]), ['t_abi', ('PW', d)], [('PW', d)])
            for lev in range(11):
                V(0, lambda e, d=d, lev=lev: e.tensor_tensor(out=tt["t1"][:], in0=PR[d][:, :, lev], in1=PR[d][:, :, lev], op=ALU.mult), [('PW', d), 't_t1'], ['t_t1'])
                V(0, lambda e, d=d, lev=lev: e.tensor_tensor(out=tt["t2"][:], in0=PI[d][:, :, lev], in1=PI[d][:, :, lev], op=ALU.mult), [('PW', d), 't_t2'], ['t_t2'])
                V(0, lambda e, d=d, lev=lev: e.tensor_tensor(out=PR[d][:, :, lev + 1], in0=tt["t1"][:], in1=tt["t2"][:], op=ALU.subtract), ['t_t1', 't_t2', ('PW', d)], [('PW', d)])
                V(0, lambda e, d=d, lev=lev: e.scalar_tensor_tensor(out=PI[d][:, :, lev + 1], in0=PR[d][:, :, lev], scalar=2.0, in1=PI[d][:, :, lev], op0=ALU.mult, op1=ALU.mult), [('PW', d)], [('PW', d)])
            V(0, lambda e, d=d: e.tensor_scalar(out=NPI[d][:], in0=PI[d][:], scalar1=-1.0, scalar2=None, op0=ALU.mult), [('PW', d)], [('PW', d)])
        for ct in range(3):
            S.op('pool', lambda e, ct=ct: e.tensor_scalar(out=yS[:, ct, :], in0=uF[:, ct, :], scalar1=prm[:, 840 + ct:841 + ct], scalar2=None, op0=ALU.mult), reads=['uF', 'prm'], writes=['yS'])
        Wd_ = sb("Wide", (128, 128)); Bpad = [sb(f"Bpad{c}", (128, 128)) for c in range(2)]
        CW = [sb(f"CW{c}", (128, 128)) for c in range(2)]
        HA = [[sb(f"HA{d}{c}", (128, T)) for c in range(2)] for d in range(2)]
        HB = [[sb(f"HB{d}{c}", (128, T)) for c in range(2)] for d in range(2)]
        colmap = lambda d, n0: n0 if d == 0 else (n0 - TC if n0 >= TC else TL + n0)
        for j in range(NJ):
            ct = j // 4; cb = 32 * (j % 4)
            for c, CC in enumerate((CRE, CIM)):
                S.op('dve', lambda e, c=c: e.memset(CW[c][:], 0.0), writes=[('CW', c)])
                for gl in range(2):
                    S.op('dve', lambda e, c=c, gl=gl, CC=CC, j=j, cb=cb: e.tensor_scalar(out=CW[c][:, cb + 16 * gl:cb + 16 * gl + 16], in0=CC[:, j, :], scalar1=gm[gl], scalar2=(1.0 if c == 0 else -1.0), op0=ALU.mult, op1=ALU.mult),
                         reads=['prm', 'bmask', ('CW', c)], writes=[('CW', c)])
            for d in range(2):
                eng = 'dve'
                for c in range(2):
                    S.op('dve', lambda e: e.memset(Wd_[:], 0.0), writes=['Wide'])
                    for gl in range(2):
                        S.op('dve', lambda e, c=c, gl=gl, d=d, j=j, cb=cb: e.tensor_scalar(out=Wd_[:, cb + 16 * gl:cb + 16 * gl + 16], in0=Bb[d][c][:, j, :], scalar1=gm[gl], scalar2=None, op0=ALU.mult),
                             reads=[('Bb', d), 'bmask', 'Wide'], writes=['Wide'])
                    S.op('pe', lambda e: e.transpose(out=psf[:, 5, 0:128], in_=Wd_[:], identity=identf[:]), reads=['Wide', 'identf'], writes=[('psf', 5)])
                    S.op('act', lambda e, c=c: e.activation(out=Bpad[c][:], in_=psf[:, 5, 0:128], func=AF.Copy), reads=[('psf', 5)], writes=[('Bpad', c)])
                for ci_, (n0, nw) in enumerate(TOKCH):
                    col = colmap(d, n0)
                    for c in range(2):
                        pb = (2 * ci_ + c) % 4
                        S.op('pe', lambda e, c=c, pb=pb, n0=n0, nw=nw, ct=ct: e.matmul(psf[:, pb, 0:nw], lhsT=Bpad[c][:], rhs=uF[:, ct, n0:n0 + nw], start=True, stop=True), reads=[('Bpad', c), 'uF'], writes=[('psf', pb)])
                        S.op('act', lambda e, c=c, pb=pb, d=d, col=col, nw=nw: e.activation(out=HA[d][c][:, col:col + nw], in_=psf[:, pb, 0:nw], func=AF.Copy), reads=[('psf', pb)], writes=[('H', d, c, 0)])
            state = {d: [HA[d], HB[d], 0, 1] for d in range(2)}
            for lev in range(12):
                sh = 1 << lev
                for part in range(2):
                    for d in range(2):
                        src, dst, sk, dk = state[d]
                        pr = PR[d][:, j, lev:lev + 1]; pi_ = PI[d][:, j, lev:lev + 1]; npi = NPI[d][:, j, lev:lev + 1]
                        if d == 0:
                            o = slice(sh, T); i_ = slice(0, T - sh); pre = slice(0, sh)
                        else:
                            o = slice(0, T - sh); i_ = slice(sh, T); pre = slice(T - sh, T)
                        rk = [('H', d, 0, sk), ('H', d, 1, sk), ('PW', d)]
                        if part == 0:
                            S.op(eng, lambda e, src=src, dst=dst, o=o, i_=i_, pr=pr: e.scalar_tensor_tensor(out=dst[0][:, o], in0=src[0][:, i_], scalar=pr, in1=src[0][:, o], op0=ALU.mult, op1=ALU.add), reads=rk, writes=[('H', d, 0, dk)])
                            S.op(eng, lambda e, src=src, dst=dst, o=o, i_=i_, pr=pr: e.scalar_tensor_tensor(out=dst[1][:, o], in0=src[1][:, i_], scalar=pr, in1=src[1][:, o], op0=ALU.mult, op1=ALU.add), reads=rk, writes=[('H', d, 1, dk)])
                        else:
                            S.op(eng, lambda e, src=src, dst=dst, o=o, i_=i_, npi=npi: e.scalar_tensor_tensor(out=dst[0][:, o], in0=src[1][:, i_], scalar=npi, in1=dst[0][:, o], op0=ALU.mult, op1=ALU.add), reads=rk + [('H', d, 0, dk)], writes=[('H', d, 0, dk)])
                            S.op(eng, lambda e, src=src, dst=dst, o=o, i_=i_, pi_=pi_: e.scalar_tensor_tensor(out=dst[1][:, o], in0=src[0][:, i_], scalar=pi_, in1=dst[1][:, o], op0=ALU.mult, op1=ALU.add), reads=rk + [('H', d, 1, dk)], writes=[('H', d, 1, dk)])
                            for c in range(2):
                                S.op('act', lambda e, src=src, dst=dst, c=c, pre=pre: e.activation(out=dst[c][:, pre], in_=src[c][:, pre], func=AF.Copy), reads=[('H', d, c, sk), ('H', d, c, dk)], writes=[('H', d, c, dk)])
                for d in range(2):
                    src, dst, sk, dk = state[d]
                    state[d] = [dst, src, dk, sk]
            for ci_, (n0, nw) in enumerate(TOKCH):
                pb = ci_ % 2

                def mmr(e, pb=pb, n0=n0, nw=nw):
                    k = 0
                    for d in range(2):
                        col = colmap(d, n0)
                        for c in range(2):
                            ins = e.matmul(psf[:, pb, 0:nw], lhsT=CW[c][:], rhs=HA[d][c][:, col:col + nw], start=(k == 0), stop=(k == 3))
                            k += 1
                    return ins
                S.op('pe', mmr, reads=[('CW', 0), ('CW', 1)] + [('H', d, c, 0) for d in range(2) for c in range(2)], writes=[('psf', pb)])
                S.op('dve', lambda e, pb=pb, n0=n0, nw=nw, ct=ct: e.tensor_tensor(out=yS[:, ct, n0:n0 + nw], in0=yS[:, ct, n0:n0 + nw], in1=psf[:, pb, 0:nw], op=ALU.add), reads=[('psf', pb), 'yS'], writes=['yS'])
        x2 = HA[0][0]; inner = HA[0][1]; sgm = HB[0][0]
        yg = sb("yg", (128, 3, T), BF16); ygf = HB[0][1]
        wglu = sb("wglu", (128, 3, 384), BF16)
        S.dma('pool', wglu[:], dram(K, f's5_w_glu_{layer}').rearrange("(k p) n -> p k n", p=128), writes=['wglu'])
        for ct in range(3):
            y = yS[:, ct, :]
            S.op('dve', lambda e, y=y: e.tensor_tensor(out=x2[:], in0=y, in1=y, op=ALU.mult), reads=['yS', ('H', 0, 0, 0)], writes=[('H', 0, 0, 0)])
            S.op('dve', lambda e: e.tensor_scalar(out=x2[:], in0=x2[:], scalar1=0.044715, scalar2=1.0, op0=ALU.mult, op1=ALU.add), reads=[('H', 0, 0, 0)], writes=[('H', 0, 0, 0)])
            S.op('dve', lambda e, y=y: e.tensor_tensor(out=inner[:], in0=x2[:], in1=y, op=ALU.mult), reads=[('H', 0, 0, 0), 'yS', ('H', 0, 1, 0)], writes=[('H', 0, 1, 0)])
            S.op('act', lambda e: e.activation(out=sgm[:], in_=inner[:], func=AF.Sigmoid, scale=1.5957691216057308), reads=[('H', 0, 1, 0), ('H', 0, 0, 1)], writes=[('H', 0, 0, 1)])
            S.op('dve', lambda e, y=y, ct=ct: e.tensor_tensor(out=yS[:, ct, :], in0=y, in1=sgm[:], op=ALU.mult), reads=['yS', ('H', 0, 0, 1)], writes=['yS'])
            S.op('act', lambda e, ct=ct: e.activation(out=yg[:, ct, :], in_=yS[:, ct, :], func=AF.Copy), reads=['yS'], writes=['yg'])
        for m in range(3):
            for ci_, (n0, nw) in enumerate(TOKCH):
                pb = ci_ % 2

                def mmg(e, m=m, pb=pb, n0=n0, nw=nw):
                    for k in range(3):
                        ins = e.matmul(psf[:, pb, 0:nw], lhsT=wglu[:, k, m * 128:(m + 1) * 128], rhs=yg[:, k, n0:n0 + nw], start=(k == 0), stop=(k == 2))
                    return ins
                S.op('pe', mmg, reads=['wglu', 'yg'], writes=[('psf', pb)])
                S.op('act', lambda e, m=m, pb=pb, n0=n0, nw=nw: e.activation(out=ygf[:, n0:n0 + nw], in_=psf[:, pb, 0:nw], func=AF.Sigmoid, bias=prm[:, 843 + m:844 + m]), reads=[('psf', pb), 'prm', ('H', 0, 1, 1)], writes=[('H', 0, 1, 1)])
            S.op('dve', lambda e, m=m: e.tensor_tensor(out=HB[1][0][:], in0=yS[:, m, :], in1=ygf[:], op=ALU.mult), reads=['yS', ('H', 0, 1, 1), ('H', 1, 0, 1)], writes=[('H', 1, 0, 1)])
            S.op('act', lambda e, m=m: e.activation(out=HA[1][0][:].bitcast(BF16)[:, 0:T], in_=HB[1][0][:], func=AF.Copy), reads=[('H', 1, 0, 1), ('H', 1, 0, 0)], writes=[('H', 1, 0, 0)])
            S.dma('sp', yT_d[8 + m], HA[1][0][:].bitcast(BF16)[:, 0:T], reads=[('H', 1, 0, 0)], writes=[('yT_d', 8 + m)], sem='yTd')
        S.barrier()


def setup_common(K):
    nc, S, es = K.nc, K.S, K.es
    sb = lambda name, shape, dt=F32: es.enter_context(nc.sbuf_tensor(name, list(shape), dt))
    K.identf = sb("identf", (128, 128)); K.identb = sb("identb", (128, 128), BF16)
    K.ones_f = sb("ones_f", (128, 128)); K.eps_t = sb("eps_t", (128, 1))
    K.psf = es.enter_context(nc.psum_tensor("psf", [128, 6, 512], F32))
    K.psb = es.enter_context(nc.psum_tensor("psb", [128, 2, 1024], BF16))
    S.dma('sp', K.identf[:], dram(K, 'ident', (128, 128))[:, :], writes=['identf'])
    S.op('dve', lambda e: e.tensor_copy(out=K.identb[:], in_=K.identf[:]), reads=['identf'], writes=['identb'])
    S.op('dve', lambda e: e.memset(K.ones_f[:], 1.0), writes=['ones_f'])
    S.op('dve', lambda e: e.memset(K.eps_t[:], 1e-6), writes=['eps'])


PHASES = {}


def build_program(plan, kinds=None, dbg=False):
    nc = bass.Bass("TRN2", target_bir_lowering=False)
    K = Ctx()
    K.nc = nc; K.di = {}; K.kinds = kinds or {}; K.declared = {}; K.dbg = dbg
    with ExitStack() as es:
        K.es = es
        K.S = Sched(nc, es)
        setup_common(K)
        for (ph, layer) in plan:
            if ph == 'init':
                phase_init(K)
            else:
                PHASES[ph](K, layer)
        K.S.barrier()
        print("n_ops", K.S.n, flush=True)
    return nc, K


PHASES.update({'p0': phase_p0, 'p1': phase_p1, 'attn': phase_attn, 'ssd': phase_ssd, 'sc': phase_sc, 'merge': phase_merge, 'moe': phase_moe, 'final': phase_final, 's5': phase_s5})


def make_consts():
    half = 32
    inv = (np.float32(10000.0) ** (-np.arange(0, half, 2, dtype=np.float32) / np.float32(half))).astype(np.float32)
    tt = np.arange(TL)
    row = (tt // 64).astype(np.float32); col = (tt % 64).astype(np.float32)
    ar = (row[:, None] * inv).astype(np.float32); ac = (col[:, None] * inv).astype(np.float32)
    C = np.ones((T, 2, 2, 16), np.float32); Sg = np.zeros((T, 2, 2, 16), np.float32)
    for b, a in enumerate((ar, ac)):
        C[TC:, b, 0] = np.cos(a); C[TC:, b, 1] = np.cos(a)
        Sg[TC:, b, 0] = -np.sin(a); Sg[TC:, b, 1] = np.sin(a)
    ii = np.arange(128)
    triF = (ii[:, None] <= ii[None, :]).astype(np.float32); triB = (ii[:, None] >= ii[None, :]).astype(np.float32)
    bmask = np.zeros((128, 512), np.float32); bmask[0:64, 0:256] = 1.0; bmask[64:128, 256:512] = 1.0
    iota = np.tile(np.arange(256, dtype=np.float32)[None, :], (128, 1))
    return {"iota": iota, "triS": (triF - np.eye(128, dtype=np.float32)), "bmask": bmask, "ident": np.eye(128, dtype=np.float32), "ropeC": C.reshape(T, 64), "ropeS": Sg.reshape(T, 64),
            "triF": triF, "triB": triB, "negF": (triF - 1.0) * 30000.0, "negBm": (triB - 1.0) * 30000.0}


def make_in_maps(K, inputs, ncores, extra=None):
    consts = make_consts()
    maps = []
    for b in range(ncores):
        m = {}
        for name, kind in K.declared.items():
            if kind != "ExternalInput":
                continue
            if extra is not None and name in extra:
                m[name] = extra[name]
            elif name == 'x':
                m[name] = np.ascontiguousarray(inputs['x'][b])
            elif name == 'ctx':
                m[name] = np.ascontiguousarray(inputs['ctx'][b])
            elif name == 'cc':
                m[name] = np.ascontiguousarray(np.stack([inputs['c'][b], inputs['c_ctx']], 0))
            elif name in consts:
                m[name] = consts[name]
            elif name.startswith('s5p_'):
                l = int(name.rsplit('_', 1)[1])
                st = lambda a: np.ascontiguousarray(a.reshape(12, 2, 64).transpose(1, 2, 0).reshape(128, 12))
                cols = [st(inputs['s5_lambda_re'][l][d]) for d in range(2)] + [st(inputs['s5_lambda_im'][l][d]) for d in range(2)]
                cols += [np.repeat(inputs['s5_log_step'][l][d].reshape(12, 2).T[:, None, :], 64, axis=1).reshape(128, 12) for d in range(2)]
                for nm in ('s5_b_re', 's5_b_im'):
                    cols.append(inputs[nm][l].reshape(12, 2, 64, 16).transpose(1, 2, 0, 3).reshape(128, 192))
                for nm in ('s5_c_re', 's5_c_im'):
                    cols.append(inputs[nm][l].reshape(12, 2, 16, 64).transpose(1, 3, 0, 2).reshape(128, 192))
                cols.append(inputs['s5_d'][l].reshape(3, 128).T); cols.append(inputs['s5_b_glu'][l].reshape(3, 128).T)
                m[name] = np.ascontiguousarray(np.concatenate(cols, 1).astype(np.float32))
            elif name.startswith('ssdp_'):
                l = int(name.rsplit('_', 1)[1])
                m[name] = np.ascontiguousarray(np.concatenate([inputs['ssd_dt_bias'][l].ravel(), inputs['ssd_a_log'][l].ravel(), inputs['ssd_d'][l].ravel()])[None, :])
            elif name == 'final_norm_g':
                m[name] = np.ascontiguousarray(inputs[name])
            else:
                base, lay = name.rsplit('_', 1)
                m[name] = np.ascontiguousarray(inputs[base][int(lay)])
        maps.append(m)
    return maps


def full_plan():
    plan = [('init', 0)]
    for layer in range(DEPTH):
        plan += [(ph, layer) for ph in ('p0', 'p1', 'attn', 'ssd', 's5', 'sc', 'merge', 'moe')]
    plan.append(('final', 0))
    return plan


def kernel(**inputs):
    nc, K = build_program(full_plan(), kinds={'out': 'ExternalOutput', 'x_d': 'ExternalOutput'})
    in_maps = make_in_maps(K, inputs, 8)
    res = run_bass_kernel_spmd(nc, in_maps, core_ids=list(range(8)))
    return np.stack([r["out"] for r in res.results], 0)
```
